# Optimizing a Trainium2 kernel written in Bass

```python
import math
import jax, jax.numpy as jnp
from jax import lax
import numpy as np

D_MODEL = 1024
BATCH = 8
SEQ = 4096
DEPTH = 1

GLA_HEADS = 4
GLA_DK = D_MODEL // 2
GLA_DV = D_MODEL
GLA_GATE_RANK = 16
GLA_TAU = 16.0
GLA_CHUNK = 64
DSA_HEADS = 8
DSA_HEAD_DIM = 128
DSA_LATENT = 256
IDX_HEADS = 8
IDX_DIM = 64
DSA_TOPK = 256
Q_BLOCK = 128
PEER_HEADS = 8
PEER_NKEYS = 128
PEER_NEXPERTS = PEER_NKEYS * PEER_NKEYS
PEER_DKEY = 256
PEER_TOPK = 16
PEER_TOKEN_BLOCK = 128
NORM_EPS = 1e-6

IN_SIZES = (GLA_DK, GLA_DK, GLA_DV, GLA_DV, GLA_GATE_RANK,
            DSA_HEADS * DSA_HEAD_DIM, DSA_LATENT, IDX_HEADS * IDX_DIM, IDX_DIM, IDX_HEADS,
            D_MODEL, D_MODEL)
IN_TOTAL = sum(IN_SIZES)

kernel_name = "hybrid_gla_dsa_peer_block"


def _rmsnorm(x, g):
    x32 = x.astype(jnp.float32)
    y = x32 * lax.rsqrt(jnp.mean(x32 * x32, axis=-1, keepdims=True) + NORM_EPS)
    return (y * g.astype(jnp.float32)).astype(x.dtype)


def _alibi_slopes(n_heads):
    return jnp.exp2(-8.0 * jnp.arange(1, n_heads + 1, dtype=jnp.float32) / n_heads)


def _gla_chunked(q, k, v, log_alpha):
    B, H, S, dk = q.shape
    dv = v.shape[-1]
    C = GLA_CHUNK
    n = S // C
    q, k, v, la = [t.astype(jnp.float32).reshape(B, H, n, C, t.shape[-1]) for t in (q, k, v, log_alpha)]
    b = jnp.cumsum(la, axis=3)
    b_last = b[:, :, :, -1:, :]
    q_dec = q * jnp.exp(b)
    k_inv = k * jnp.exp(-b)
    k_end = k * jnp.exp(b_last - b)
    causal = jnp.tril(jnp.ones((C, C), dtype=bool))
    attn = jnp.where(causal, jnp.einsum('bhnid,bhnjd->bhnij', q_dec, k_inv), 0.0)
    o_intra = jnp.einsum('bhnij,bhnjv->bhniv', attn, v)

    def step(state, inp):
        q_c, k_c, v_c, decay_c = inp
        o_c = jnp.einsum('bhid,bhdv->bhiv', q_c, state)
        state = decay_c[..., None] * state + jnp.einsum('bhjd,bhjv->bhdv', k_c, v_c)
        return state, o_c

    xs = (jnp.moveaxis(q_dec, 2, 0), jnp.moveaxis(k_end, 2, 0), jnp.moveaxis(v, 2, 0),
          jnp.moveaxis(jnp.exp(b_last[:, :, :, 0, :]), 2, 0))
    state0 = jnp.zeros((B, H, dk, dv), jnp.float32)
    _, o_inter = lax.scan(step, state0, xs)
    o = o_intra + jnp.moveaxis(o_inter, 0, 2)
    return o.reshape(B, H, S, dv)


def _gla_branch(q, k, v, r, g_low, w_gate_up, b_gate, norm_g):
    B, S, _ = q.shape
    dkh = GLA_DK // GLA_HEADS
    dvh = GLA_DV // GLA_HEADS
    gate_logit = (g_low @ w_gate_up + b_gate).astype(jnp.float32)
    log_alpha = jax.nn.log_sigmoid(gate_logit) / GLA_TAU

    def heads(t, d):
        return t.reshape(B, S, GLA_HEADS, d).transpose(0, 2, 1, 3)

    o = _gla_chunked(heads(q, dkh) * (dkh ** -0.5), heads(k, dkh), heads(v, dvh), heads(log_alpha, dkh))
    o = o.transpose(0, 2, 1, 3)
    o = _rmsnorm(o, norm_g.reshape(GLA_HEADS, dvh)).reshape(B, S, GLA_DV)
    return o.astype(r.dtype) * jax.nn.silu(r)


def _dsa_branch(q, kv_raw, iq, ik, iw, kv_norm_g, w_uk, w_uv):
    B, S, _ = q.shape
    n_blocks = S // Q_BLOCK
    k_sel = min(DSA_TOPK, S // 4)
    c_kv = _rmsnorm(kv_raw, kv_norm_g)
    slopes = _alibi_slopes(DSA_HEADS)
    key_pos = jnp.arange(S, dtype=jnp.int32)

    def to_blocks(t):
        return t.reshape((B, n_blocks, Q_BLOCK) + t.shape[2:]).swapaxes(0, 1)

    q_b = to_blocks(q.reshape(B, S, DSA_HEADS, DSA_HEAD_DIM))
    iq_b = to_blocks(iq.reshape(B, S, IDX_HEADS, IDX_DIM))
    iw_b = to_blocks(iw)

    def block(inp):
        qb, iqb, iwb, blk = inp
        t = blk * Q_BLOCK + jnp.arange(Q_BLOCK, dtype=jnp.int32)
        rel = jax.nn.relu(jnp.einsum('bqhd,bsd->bqhs', iqb, ik).astype(jnp.float32) * (IDX_DIM ** -0.5))
        score = jnp.einsum('bqh,bqhs->bqs', iwb.astype(jnp.float32) * (IDX_HEADS ** -0.5), rel)
        score = jnp.where(key_pos[None, None, :] <= t[None, :, None], score, -jnp.inf)
        _, idx = lax.top_k(score, k_sel)
        kv_sel = jax.vmap(lambda cb, ib: cb[ib])(c_kv, idx)
        q_lat = jnp.einsum('bqhd,hdc->bqhc', qb, w_uk)
        logits = jnp.einsum('bqhc,bqkc->bqhk', q_lat, kv_sel).astype(jnp.float32) * (DSA_HEAD_DIM ** -0.5)
        dist = t[None, :, None] - idx
        logits = logits - slopes[None, None, :, None] * dist[:, :, None, :].astype(jnp.float32)
        logits = jnp.where((dist >= 0)[:, :, None, :], logits, -jnp.inf)
        p = jax.nn.softmax(logits, axis=-1).astype(kv_sel.dtype)
        o_lat = jnp.einsum('bqhk,bqkc->bqhc', p, kv_sel)
        o = jnp.einsum('bqhc,hcd->bqhd', o_lat, w_uv)
        return o.reshape(B, Q_BLOCK, DSA_HEADS * DSA_HEAD_DIM)

    out = lax.map(block, (q_b, iq_b, iw_b, jnp.arange(n_blocks, dtype=jnp.int32)))
    return out.swapaxes(0, 1).reshape(B, S, DSA_HEADS * DSA_HEAD_DIM)


def _peer(h, w_q, sub_keys_1, sub_keys_2, expert_u, expert_v):
    B, S, D = h.shape
    half = PEER_DKEY // 2
    q = (h @ w_q).reshape(B, S, PEER_HEADS, PEER_DKEY)
    s1 = jnp.einsum('bshd,nd->bshn', q[..., :half], sub_keys_1).astype(jnp.float32)
    s2 = jnp.einsum('bshd,nd->bshn', q[..., half:], sub_keys_2).astype(jnp.float32)
    v1, i1 = lax.top_k(s1, PEER_TOPK)
    v2, i2 = lax.top_k(s2, PEER_TOPK)
    cand = (v1[..., :, None] + v2[..., None, :]).reshape(B, S, PEER_HEADS, PEER_TOPK * PEER_TOPK)
    best, ci = lax.top_k(cand, PEER_TOPK)
    ia = jnp.take_along_axis(i1, ci // PEER_TOPK, axis=-1)
    ib = jnp.take_along_axis(i2, ci % PEER_TOPK, axis=-1)
    expert_idx = ia * PEER_NKEYS + ib
    gates = jax.nn.softmax(best, axis=-1)
    n_blocks = (B * S) // PEER_TOKEN_BLOCK
    h_b = h.reshape(n_blocks, PEER_TOKEN_BLOCK, D)
    i_b = expert_idx.reshape(n_blocks, PEER_TOKEN_BLOCK, PEER_HEADS, PEER_TOPK)
    g_b = gates.reshape(n_blocks, PEER_TOKEN_BLOCK, PEER_HEADS, PEER_TOPK)

    def block(inp):
        hx, ix, gx = inp
        u = expert_u[ix]
        v = expert_v[ix]
        a = jax.nn.gelu(jnp.einsum('td,thkd->thk', hx, u).astype(jnp.float32), approximate=False) * gx
        return jnp.einsum('thk,thkd->td', a.astype(hx.dtype), v)

    return lax.map(block, (h_b, i_b, g_b)).reshape(B, S, D)


def setup_inputs(seed: int = 0) -> dict:
    key = jax.random.key(seed)
    ks = jax.random.split(key, 24)
    D = D_MODEL

    def nrm(k, shape, scale):
        return jax.random.normal(k, shape, jnp.float32) * scale

    def gain(k, shape):
        return 1.0 + 0.1 * jax.random.normal(k, shape, jnp.float32)

    return {
        "x": nrm(ks[0], (BATCH, SEQ, D), 1.0),
        "c": nrm(ks[1], (BATCH, D), 1.0),
        "w_ada": nrm(ks[2], (DEPTH, D, 6 * D), 0.5 * D ** -0.5),
        "b_ada": nrm(ks[3], (DEPTH, 6 * D), 0.02),
        "norm1_g": gain(ks[4], (DEPTH, D)),
        "w_in": nrm(ks[5], (DEPTH, D, IN_TOTAL), D ** -0.5),
        "gla_w_gate_up": nrm(ks[6], (DEPTH, GLA_GATE_RANK, GLA_DK), GLA_GATE_RANK ** -0.5),
        "gla_b_gate": nrm(ks[7], (DEPTH, GLA_DK), 0.1),
        "gla_norm_g": gain(ks[8], (DEPTH, GLA_DV)),
        "dsa_kv_norm_g": gain(ks[9], (DEPTH, DSA_LATENT)),
        "dsa_w_uk": nrm(ks[10], (DEPTH, DSA_HEADS, DSA_HEAD_DIM, DSA_LATENT), DSA_HEAD_DIM ** -0.5),
        "dsa_w_uv": nrm(ks[11], (DEPTH, DSA_HEADS, DSA_LATENT, DSA_HEAD_DIM), DSA_LATENT ** -0.5),
        "w_branch_a": nrm(ks[12], (DEPTH, GLA_DV, D), GLA_DV ** -0.5),
        "w_branch_b": nrm(ks[13], (DEPTH, DSA_HEADS * DSA_HEAD_DIM, D), (DSA_HEADS * DSA_HEAD_DIM) ** -0.5),
        "w_out": nrm(ks[14], (DEPTH, D, D), D ** -0.5),
        "norm2_g": gain(ks[15], (DEPTH, D)),
        "peer_w_q": nrm(ks[16], (DEPTH, D, PEER_HEADS * PEER_DKEY), D ** -0.5),
        "peer_sub_keys_1": nrm(ks[17], (DEPTH, PEER_NKEYS, PEER_DKEY // 2), (PEER_DKEY // 2) ** -0.5),
        "peer_sub_keys_2": nrm(ks[18], (DEPTH, PEER_NKEYS, PEER_DKEY // 2), (PEER_DKEY // 2) ** -0.5),
        "peer_u": nrm(ks[19], (DEPTH, PEER_NEXPERTS, D), D ** -0.5),
        "peer_v": nrm(ks[20], (DEPTH, PEER_NEXPERTS, D), PEER_HEADS ** -0.5),
        "final_norm_g": gain(ks[21], (D,)),
    }


def reference(x, c, w_ada, b_ada, norm1_g, w_in, gla_w_gate_up, gla_b_gate, gla_norm_g,
              dsa_kv_norm_g, dsa_w_uk, dsa_w_uv, w_branch_a, w_branch_b, w_out, norm2_g,
              peer_w_q, peer_sub_keys_1, peer_sub_keys_2, peer_u, peer_v, final_norm_g):
    split_points = np.cumsum(IN_SIZES)[:-1].tolist()
    for l in range(DEPTH):
        ada = jax.nn.silu(c) @ w_ada[l] + b_ada[l]
        shift1, scale1, gate1, shift2, scale2, gate2 = jnp.split(ada[:, None, :], 6, axis=-1)

        h = _rmsnorm(x, norm1_g[l]) * (1.0 + scale1) + shift1
        proj = h @ w_in[l]
        (gq, gk, gv, gr, glow, dq, dkv, iq, ik, iw, za, zb) = jnp.split(proj, split_points, axis=-1)
        ya = _gla_branch(gq, gk, gv, gr, glow, gla_w_gate_up[l], gla_b_gate[l], gla_norm_g[l]) @ w_branch_a[l]
        yb = _dsa_branch(dq, dkv, iq, ik, iw, dsa_kv_norm_g[l], dsa_w_uk[l], dsa_w_uv[l]) @ w_branch_b[l]
        y = (jax.nn.sigmoid(za) * ya + jax.nn.sigmoid(zb) * yb) @ w_out[l]
        x = x + gate1 * y

        h2 = _rmsnorm(x, norm2_g[l]) * (1.0 + scale2) + shift2
        x = x + gate2 * _peer(h2, peer_w_q[l], peer_sub_keys_1[l], peer_sub_keys_2[l], peer_u[l], peer_v[l])
    return _rmsnorm(x, final_norm_g)
```

```python
import os
import numpy as np
from contextlib import ExitStack
import concourse.bass as bass
import concourse.mybir as mybir
from concourse.bass_utils import run_bass_kernel_spmd

F32 = mybir.dt.float32
BF16 = mybir.dt.bfloat16
U32 = mybir.dt.uint32
AF = mybir.ActivationFunctionType
ALU = mybir.AluOpType

D = 1024
S = 4096
NT = S // 128
IN_TOTAL = 7000
OFF = dict(gq=0, gk=512, gv=1024, gr=2048, glow=3072, dq=3088, dkv=4112, iq=4368, ik=4880,
           iw=4944, za=4952, zb=5976)
NEG = -1.0e30


class Trk:
    __slots__ = ("w", "r", "multi")

    def __init__(self, multi=False):
        self.w = {}
        self.r = {}
        self.multi = multi


class Stream:
    def __init__(self, sem, sid):
        self.sem = sem
        self.n = 0
        self.sid = sid
        self.maxwait = 0


class Eng:
    def __init__(self, name, e, sem, sid):
        self.name = name
        self.e = e
        self.sem = sem
        self.sid = sid
        self.n = 0
        self.seen = {}


class KB:
    def __init__(self, nc, es):
        self.nc = nc
        self.es = es
        self.nsid = 0
        self.E = {}
        for name, e in (("pe", nc.tensor), ("act", nc.scalar), ("dve", nc.vector),
                        ("pool", nc.gpsimd), ("sp", nc.sync)):
            sem = es.enter_context(nc.semaphore("sem_" + name))
            self.E[name] = Eng(name, e, sem, self.nsid)
            self.nsid += 1
        self.ninst = 0
        self.streams = []
        self.sid2stream = {}

    def stream(self, name):
        sem = self.es.enter_context(self.nc.semaphore("st_" + name))
        s = Stream(sem, self.nsid)
        self.nsid += 1
        self.streams.append(s)
        self.sid2stream[s.sid] = s
        return s

    def op(self, en, fn, reads=(), writes=(), dma=None):
        e = self.E[en]
        need = {}

        def add(evd, skip_own):
            for k, (sem, val) in evd.items():
                if skip_own and k == e.sid:
                    continue
                if need.get(k, (None, 0))[1] < val:
                    need[k] = (sem, val)

        own = (en == "pe" and dma is None)
        for t in reads:
            add(t.w, own)
        for t in writes:
            if not t.multi:
                add(t.w, own)
            add(t.r, own)
        for k, (sem, val) in need.items():
            stt = self.sid2stream.get(k)
            if stt is not None:
                val = stt.n
                stt.maxwait = max(stt.maxwait, val)
            if e.seen.get(k, 0) >= val:
                continue
            e.e.wait_ge(sem, val)
            e.seen[k] = val
            self.ninst += 1
        if dma is not None and dma.maxwait > e.seen.get(dma.sid, 0):
            e.e.wait_ge(dma.sem, dma.maxwait)
            e.seen[dma.sid] = dma.maxwait
            self.ninst += 1
        inst = fn(e.e)
        self.ninst += 1
        if dma is None:
            e.n += 1
            inst.then_inc(e.sem, 1)
            ev = (e.sem, e.n)
            key = e.sid
        else:
            dma.n += 16
            inst.then_inc(dma.sem, 16)
            ev = (dma.sem, dma.n)
            key = dma.sid
        for t in reads:
            if t.r.get(key, (None, 0))[1] < ev[1]:
                t.r[key] = ev
        for t in writes:
            if t.multi:
                if t.w.get(key, (None, 0))[1] < ev[1]:
                    t.w[key] = ev
            else:
                t.w = {key: ev}
                t.r = {}
        return ev

    def barrier(self):
        for e in self.E.values():
            for o in self.E.values():
                if o is e or o.n == 0 or e.seen.get(o.sid, 0) >= o.n:
                    continue
                e.e.wait_ge(o.sem, o.n)
                e.seen[o.sid] = o.n
                self.ninst += 1
            for stt in self.streams:
                if stt.n == 0 or e.seen.get(stt.sid, 0) >= stt.n:
                    continue
                e.e.wait_ge(stt.sem, stt.n)
                e.seen[stt.sid] = stt.n
                stt.maxwait = stt.n
                self.ninst += 1

    def wait_all(self, en, trks):
        e = self.E[en]
        for t in trks:
            for k, (sem, val) in t.w.items():
                stt = self.sid2stream.get(k)
                if stt is not None:
                    val = stt.n
                    stt.maxwait = max(stt.maxwait, val)
                if e.seen.get(k, 0) >= val:
                    continue
                e.e.wait_ge(sem, val)
                e.seen[k] = val


def make_consts():
    p = np.arange(128)
    ident = np.eye(128, dtype=np.float32)
    lt = (p[:, None] <= p[None, :]).astype(np.float32)
    ut = (p[:, None] > p[None, :]).astype(np.float32)
    diag = np.where(p[None, :] <= p[:, None], 0.0, NEG).astype(np.float32)
    slopes = np.exp2(-8.0 * np.arange(1, 9, dtype=np.float32) / 8).astype(np.float32)
    dl = np.arange(32)
    bias = slopes[None, :, None] * (p[:, None, None] - 127.0 + 128.0 * (dl[None, None, :] - 31.0))
    bias = bias.astype(np.float32).reshape(128, 256)
    iota16 = np.tile(np.arange(16, dtype=np.float32)[None, None, :], (128, 16, 1)).reshape(128, 256)
    pos1 = np.tile(np.arange(1, 129, dtype=np.float32)[None, :], (128, 1))
    slopes8 = np.tile(slopes[None, :], (128, 1)).astype(np.float32)
    esel = np.zeros((8, 8, 128), np.float32)
    for h in range(8):
        esel[h, h, :] = 1.0
    posr = np.ones((10, 32, 128), np.float32)
    posr[8, :, :] = (p - 127.0)[None, :]
    posr[9, :, :] = (128.0 * (dl - 31.0))[:, None]
    slopeR = np.zeros((10, 8, 128), np.float32)
    slopeR[8:10] = slopes[None, :, None]
    basej = np.tile((128.0 * np.arange(NT, dtype=np.float32))[None, :], (128, 1))
    return dict(c_posr=posr.reshape(10, 4096), c_slopeR=slopeR.reshape(10, 1024), c_basej=basej,
                c_ident=ident, c_lt=lt, c_ut=ut, c_diag=diag, c_bias=bias, c_iota16=iota16,
                c_pos1=pos1, c_slopes8=slopes8, c_esel=esel.reshape(8, 1024))


def build(stages=("A", "B", "C", "D", "E"), debug=(), scr_in=()):
    nc = bass.Bass("TRN2", target_bir_lowering=False)

    def din(name, shape, dt=F32):
        return nc.dram_tensor(name, list(shape), dt, kind="ExternalInput").ap()

    def dscr(name, shape, dt=BF16):
        kind = "ExternalOutput" if name in debug else ("ExternalInput" if name in scr_in else "Internal")
        return nc.dram_tensor(name, list(shape), dt, kind=kind).ap()

    x_d = din("x", [S, D])
    c_d = din("c", [1, D])
    wada_d = din("w_ada", [D, 6 * D])
    bada_d = din("b_ada", [1, 6 * D])
    g1_d = din("norm1_g", [1, D])
    win_d = din("w_in", [D, IN_TOTAL])
    wgu_d = din("gla_w_gate_up", [16, 512])
    bgate_d = din("gla_b_gate", [1, 512])
    gng_d = din("gla_norm_g", [1, D])
    kvg_d = din("dsa_kv_norm_g", [1, 256])
    wuk_d = din("dsa_w_uk", [8, 128, 256])
    wuv_d = din("dsa_w_uv", [8, 256, 128])
    wba_d = din("w_branch_a", [D, D])
    wbb_d = din("w_branch_b", [D, D])
    wout_d = din("w_out", [D, D])
    g2_d = din("norm2_g", [1, D])
    wq_d = din("peer_w_q", [D, 2048])
    sk1_d = din("peer_sub_keys_1", [128, 128])
    sk2_d = din("peer_sub_keys_2", [128, 128])
    pu_d = din("peer_u", [16384, D])
    pv_d = din("peer_v", [16384, D])
    fg_d = din("final_norm_g", [1, D])
    cst = {k: din(k, v.shape) for k, v in make_consts().items()}

    out_d = nc.dram_tensor("out", [S, D], F32, kind="ExternalOutput").ap()

    ada_s = dscr("ada_s", [1, 6 * D], F32)
    qT_s = dscr("qT_s", [512, S])
    kT_s = dscr("kT_s", [512, S])
    glT_s = dscr("glT_s", [16, S])
    dqT_s = dscr("dqT_s", [1024, S])
    iqT_s = dscr("iqT_s", [512, S])
    ikT_s = dscr("ikT_s", [64, S])
    k_s = dscr("k_s", [S, 512])
    v_s = dscr("v_s", [S, 1024])
    sr_s = dscr("sr_s", [S, 1024])
    dkv_s = dscr("dkv_s", [S, 256])
    iw_s = dscr("iw_s", [S, 8], F32)
    sza_s = dscr("sza_s", [S, 1024])
    szb_s = dscr("szb_s", [S, 1024])
    yag_s = dscr("yag_s", [S, 1024])
    x1_s = dscr("x1_s", [S, D], F32)
    uv_s = dscr("uv_s", [16384, 2 * D])

    with ExitStack() as es:
        kb = KB(nc, es)
        op = kb.op
        es.enter_context(nc.allow_non_contiguous_dma(reason="small strided setup loads"))
        es.enter_context(nc.allow_low_precision(reason="bf16 matmul operands, fp32 accumulation"))

        def sb(name, shape, dt=F32, st=None):
            return (st or es).enter_context(nc.sbuf_tensor(name, list(shape), dt))

        ps = es.enter_context(nc.psum_tensor("ps", [128, 8, 512], F32))
        psT = [Trk() for _ in range(8)]

        ld_c = kb.stream("ldc")
        cT = Trk(multi=True)
        ident_f = sb("ident_f", [128, 128])
        lt_f = sb("lt_f", [128, 128])
        ut_f = sb("ut_f", [128, 128])
        diag_f = sb("diag_f", [128, 128])
        bias_t = sb("bias_t", [128, 256])
        iota16 = sb("iota16", [128, 256])
        for t, nm in ((ident_f, "c_ident"), (lt_f, "c_lt"), (ut_f, "c_ut"), (diag_f, "c_diag"),
                      (bias_t, "c_bias"), (iota16, "c_iota16")):
            op("sp", lambda e, t=t, nm=nm: e.dma_start(out=t[:], in_=cst[nm][:, :]), writes=[cT], dma=ld_c)
        ident_b = sb("ident_b", [128, 128], BF16)
        lt_b = sb("lt_b", [128, 128], BF16)
        eps_t = sb("eps_t", [128, 1])
        one_t = sb("one_t", [128, 1])
        ones_row = sb("ones_row", [1, 128], BF16)
        op("dve", lambda e: e.tensor_copy(out=ident_b[:], in_=ident_f[:]), reads=[cT], writes=[cT])
        op("dve", lambda e: e.tensor_copy(out=lt_b[:], in_=lt_f[:]), reads=[cT], writes=[cT])
        op("dve", lambda e: e.memset(eps_t[:], 1e-6), writes=[cT])
        op("dve", lambda e: e.memset(one_t[:], 1.0), writes=[cT])
        op("dve", lambda e: e.memset(ones_row[:], 1.0), writes=[cT])

        adaT = Trk()
        t_adas = Trk()
        ld_ada = kb.stream("ldada")
        SH1, A1, G1, SH2, A2, G2 = [slice(i * D, (i + 1) * D) for i in range(6)]

        def load_ada(st, name, sls):
            t = sb(name, [128, len(sls) * D], st=st)
            outs = []
            for n, sl in enumerate(sls):
                op("sp", lambda e, n=n, sl=sl: e.dma_start(out=t[:, n * D:(n + 1) * D],
                                                         in_=ada_s[0:1, sl].to_broadcast([128, D])),
                   reads=[t_adas], writes=[adaT], dma=ld_ada)
                outs.append(slice(n * D, (n + 1) * D))
            return t, outs

        if "A" in stages:
            with ExitStack() as st:
                cTt = sb("cTt", [128, 8], st=st)
                scT = sb("scT", [128, 8], st=st)
                wada = [sb(f"wada{i}", [128, 8, 512], st=st) for i in range(2)]
                wadaT = [Trk() for _ in range(2)]
                ld_w = [kb.stream(f"ldwada{i}") for i in range(2)]
                arow = sb("arow", [1, 6 * D], st=st)
                brow = sb("brow", [1, 6 * D], st=st)
                grow = sb("grow", [1, 2 * D], st=st)
                t_c, t_sc, t_arow, t_brow = Trk(), Trk(), Trk(), Trk()
                ld_a = kb.stream("lda")
                op("sp", lambda e: e.dma_start(out=cTt[:], in_=c_d.rearrange("o (kc p) -> p (o kc)", p=128)),
                   writes=[t_c], dma=ld_a)
                op("sp", lambda e: e.dma_start(out=brow[:], in_=bada_d[:, :]), writes=[t_brow], dma=ld_a)
                op("sp", lambda e: e.dma_start(out=grow[0:1, 0:D], in_=g1_d[:, :]), writes=[t_brow], dma=ld_a)
                op("sp", lambda e: e.dma_start(out=grow[0:1, D:2 * D], in_=g2_d[:, :]), writes=[t_brow], dma=ld_a)
                op("act", lambda e: e.activation(out=scT[:], in_=cTt[:], func=AF.Silu), reads=[t_c], writes=[t_sc])
                wv = wada_d.rearrange("(kc p) n -> p kc n", p=128)

                def load_wada(cg):
                    i = cg % 2
                    op("sp", lambda e: e.dma_start(out=wada[i][:], in_=wv[:, :, cg * 512:(cg + 1) * 512]),
                       writes=[wadaT[i]], dma=ld_w[i])
                load_wada(0)
                for cg in range(12):
                    if cg + 1 < 12:
                        load_wada(cg + 1)
                    i = cg % 2
                    b = cg % 2
                    for kc in range(8):
                        op("pe", lambda e, kc=kc: e.matmul(ps[0:1, b, :], lhsT=scT[:, kc:kc + 1], rhs=wada[i][:, kc, :],
                                                           start=(kc == 0), stop=(kc == 7)),
                           reads=[t_sc, wadaT[i]], writes=[psT[b]])
                    op("dve", lambda e: e.tensor_tensor(out=arow[0:1, cg * 512:(cg + 1) * 512], in0=ps[0:1, b, :],
                                                        in1=brow[0:1, cg * 512:(cg + 1) * 512], op=ALU.add),
                       reads=[psT[b], t_brow], writes=[t_arow])
                for (sl, gsl) in ((A1, slice(0, D)), (A2, slice(D, 2 * D))):
                    op("dve", lambda e, sl=sl, gsl=gsl: e.scalar_tensor_tensor(
                        out=arow[0:1, sl], in0=arow[0:1, sl], scalar=1.0, in1=grow[0:1, gsl],
                        op0=ALU.add, op1=ALU.mult), reads=[t_arow, t_brow], writes=[t_arow])
                op("sp", lambda e: e.dma_start(out=ada_s[:, :], in_=arow[:]), reads=[t_arow], writes=[t_adas], dma=ld_a)

        stBC = ExitStack()
        uv_t = Trk(multi=True)
        CVR = 2
        cv32 = [sb(f"cv32_{i}", [128, CVR, D], st=stBC) for i in range(2)]
        cv16 = [sb(f"cv16_{i}", [128, CVR, D], BF16, st=stBC) for i in range(2)]
        cv32_t = [Trk() for _ in range(2)]
        cv16_t = [Trk() for _ in range(2)]
        ld_cv = [kb.stream(f"ldcv{i}") for i in range(2)]
        st_cv = [kb.stream(f"stcv{i}") for i in range(2)]
        uv_v = uv_s.rearrange("(p a) (t d) -> p a t d", a=128, t=2)
        cvs = {"n": 0, "pending": None}
        NCV = 2 * (128 // CVR)

        def conv_store():
            pnd = cvs["pending"]
            if pnd is not None:
                k, t, c = pnd
                op("sp", lambda e: e.dma_start(out=uv_v[:, c * CVR:(c + 1) * CVR, t, :], in_=cv16[k][:]),
                   reads=[cv16_t[k]], writes=[uv_t], dma=st_cv[k])
                cvs["pending"] = None

        def conv_step():
            n = cvs["n"]
            if n >= NCV:
                conv_store()
                return
            cvs["n"] += 1
            k = n % 2
            t, c = n % 2, n // 2
            tab = pu_d if t == 0 else pv_d
            src_v = tab.rearrange("(p a) d -> p a d", a=128)[:, c * CVR:(c + 1) * CVR, :]
            op("sp", lambda e: e.dma_start(out=cv32[k][:], in_=src_v), writes=[cv32_t[k]], dma=ld_cv[k])
            conv_store()
            op("pool", lambda e: e.tensor_copy(out=cv16[k][:], in_=cv32[k][:]), reads=[cv32_t[k]], writes=[cv16_t[k]])
            cvs["pending"] = (k, t, c)

        kb.barrier()
        if "B" in stages:
            with ExitStack() as st:
                adab, (A1, SH1) = load_ada(st, "adaB", [A1, SH1])
                hT = sb("hT", [128, 8, S], BF16, st=st)
                hT_t = [Trk() for _ in range(NT)]
                xt = [sb(f"xt{i}", [128, D], st=st) for i in range(2)]
                xt_t = [Trk() for _ in range(2)]
                ld_x = [kb.stream(f"ldx{i}") for i in range(2)]
                junk = sb("junk", [128, D], BF16, st=st)
                t_junk = Trk()
                tmp = sb("tmpB", [128, D], st=st)
                t_tmp = Trk()
                hb = sb("hb", [128, D], BF16, st=st)
                t_hb = Trk()
                ss = sb("ssB", [128, 4], st=st)
                t_ss = Trk()
                x_v = x_d.rearrange("(t p) d -> t p d", p=128)

                def load_x(tt):
                    i = tt % 2
                    op("sp", lambda e: e.dma_start(out=xt[i][:], in_=x_v[tt]), writes=[xt_t[i]], dma=ld_x[i])
                load_x(0)
                for tt in range(NT):
                    if tt + 1 < NT:
                        load_x(tt + 1)
                    i = tt % 2
                    op("act", lambda e: e.activation(out=junk[:], in_=xt[i][:], func=AF.Square, accum_out=ss[:, 0:1]),
                       reads=[xt_t[i]], writes=[t_junk, t_ss])
                    op("act", lambda e: e.activation(out=ss[:, 1:2], in_=ss[:, 0:1], func=AF.Sqrt, bias=eps_t[:],
                                                     scale=1.0 / D), reads=[t_ss, cT], writes=[t_ss])
                    op("dve", lambda e: e.reciprocal(out=ss[:, 2:3], in_=ss[:, 1:2]), reads=[t_ss], writes=[t_ss])
                    op("dve", lambda e: e.scalar_tensor_tensor(out=tmp[:], in0=xt[i][:], scalar=ss[:, 2:3],
                                                               in1=adab[:, A1], op0=ALU.mult, op1=ALU.mult),
                       reads=[xt_t[i], t_ss, adaT], writes=[t_tmp])
                    op("dve", lambda e: e.tensor_tensor(out=hb[:], in0=tmp[:], in1=adab[:, SH1], op=ALU.add),
                       reads=[t_tmp, adaT], writes=[t_hb])
                    pb = 6 + (tt % 2)
                    pbv = ps[:, pb, :].bitcast(BF16)
                    for kc in range(8):
                        op("pe", lambda e, kc=kc: e.transpose(out=pbv[:, kc * 128:(kc + 1) * 128],
                                                              in_=hb[:, kc * 128:(kc + 1) * 128], identity=ident_b[:]),
                           reads=[t_hb, cT], writes=[psT[pb]])
                    op("act", lambda e: e.activation(out=hT[:, :, tt * 128:(tt + 1) * 128],
                                                     in_=pbv.rearrange("p (k t) -> p k t", k=8), func=AF.Copy),
                       reads=[psT[pb]], writes=[hT_t[tt]])

                wt = [sb(f"wt{i}", [128, 8, 512], BF16, st=st) for i in range(2)]
                wt_t = [Trk() for _ in range(2)]
                ld_wt = [kb.stream(f"ldwt{i}") for i in range(2)]
                stf = [sb(f"stf{i}", [128, S], BF16, st=st) for i in range(2)]
                stf_t = [Trk(multi=True) for _ in range(2)]
                st_f = [kb.stream(f"stf{i}") for i in range(2)]
                stt_ = [sb(f"stt{i}", [128, 4, 512], BF16, st=st) for i in range(2)]
                stt_t = [Trk(multi=True) for _ in range(2)]
                st_t = [kb.stream(f"stt{i}") for i in range(2)]
                sti = sb("sti", [128, NT, 8], st=st)
                sti_t = Trk()
                win_v = win_d.rearrange("(kc p) n -> p kc n", p=128)
                scr_t = Trk(multi=True)
                self_scr = scr_t

                blocks = []
                for nm, dst, n in (("gq", qT_s, 512), ("gk", kT_s, 512), ("glow", glT_s, 16), ("dq", dqT_s, 1024),
                                   ("iq", iqT_s, 512), ("ik", ikT_s, 64)):
                    for c0 in range(0, n, 128):
                        w = min(128, n - c0)
                        blocks.append(("F", OFF[nm] + c0, w, dst, c0, None))
                for nm, dst, n, fn in (("gk", k_s, 512, AF.Copy), ("gv", v_s, 1024, AF.Copy), ("gr", sr_s, 1024, AF.Silu),
                                       ("dkv", dkv_s, 256, AF.Copy), ("iw", iw_s, 8, AF.Copy),
                                       ("za", sza_s, 1024, AF.Sigmoid), ("zb", szb_s, 1024, AF.Sigmoid)):
                    for c0 in range(0, n, 512):
                        w = min(512, n - c0)
                        blocks.append(("T", OFF[nm] + c0, w, dst, c0, fn))

                wt32 = [sb(f"wt32_{i}", [128, 8, 512], F32, st=st) for i in range(2)]
                wt32_t = [Trk() for _ in range(2)]

                def load_w(bi):
                    kind, col, w, dst, c0, fn = blocks[bi]
                    i = bi % 2
                    op("sp", lambda e: e.dma_start(out=wt32[i][:, :, 0:w], in_=win_v[:, :, col:col + w]),
                       writes=[wt32_t[i]], dma=ld_wt[i])
                    op("pool", lambda e: e.tensor_copy(out=wt[i][:, :, 0:w], in_=wt32[i][:, :, 0:w]),
                       reads=[wt32_t[i]], writes=[wt_t[i]])
                load_w(0)
                nf = 0
                ntb = 0
                evac = 0
                for bi, (kind, col, w, dst, c0, fn) in enumerate(blocks):
                    if bi + 1 < len(blocks):
                        load_w(bi + 1)
                    if "E" in stages:
                        conv_step()
                        conv_step()
                    i = bi % 2
                    if kind == "F":
                        si = nf % 2
                        nf += 1
                        for tg in range(8):
                            pb = tg % 6
                            for kc in range(8):
                                op("pe", lambda e, kc=kc: e.matmul(ps[0:w, pb, :], lhsT=wt[i][:, kc, 0:w],
                                                                   rhs=hT[:, kc, tg * 512:(tg + 1) * 512],
                                                                   start=(kc == 0), stop=(kc == 7)),
                                   reads=[wt_t[i]] + hT_t[tg * 4:(tg + 1) * 4], writes=[psT[pb]])
                            en = "act" if evac % 2 == 0 else "dve"
                            evac += 1
                            if en == "act":
                                op("act", lambda e: e.activation(out=stf[si][0:w, tg * 512:(tg + 1) * 512],
                                                                 in_=ps[0:w, pb, :], func=AF.Copy),
                                   reads=[psT[pb]], writes=[stf_t[si]])
                            else:
                                op("dve", lambda e: e.tensor_copy(out=stf[si][0:w, tg * 512:(tg + 1) * 512],
                                                                  in_=ps[0:w, pb, :]),
                                   reads=[psT[pb]], writes=[stf_t[si]])
                        op("sp", lambda e: e.dma_start(out=dst[c0:c0 + w, :], in_=stf[si][0:w, :]),
                           reads=[stf_t[si]], writes=[scr_t], dma=st_f[si])
                    else:
                        is_iw = (dst is iw_s)
                        for tt in range(NT):
                            pb = tt % 6
                            for kc in range(8):
                                op("pe", lambda e, kc=kc: e.matmul(ps[:, pb, 0:w], lhsT=hT[:, kc, tt * 128:(tt + 1) * 128],
                                                                   rhs=wt[i][:, kc, 0:w],
                                                                   start=(kc == 0), stop=(kc == 7)),
                                   reads=[wt_t[i], hT_t[tt]], writes=[psT[pb]])
                            if is_iw:
                                op("dve", lambda e: e.tensor_copy(out=sti[:, tt, :], in_=ps[:, pb, 0:w]),
                                   reads=[psT[pb]], writes=[sti_t])
                                continue
                            si = ntb % 2
                            a = tt % 4
                            if fn == AF.Copy and evac % 2 == 1:
                                op("dve", lambda e: e.tensor_copy(out=stt_[si][:, a, 0:w], in_=ps[:, pb, 0:w]),
                                   reads=[psT[pb]], writes=[stt_t[si]])
                            else:
                                op("act", lambda e: e.activation(out=stt_[si][:, a, 0:w], in_=ps[:, pb, 0:w], func=fn),
                                   reads=[psT[pb]], writes=[stt_t[si]])
                            evac += 1
                            if a == 3:
                                r0 = (tt - 3) * 128
                                op("sp", lambda e: e.dma_start(
                                    out=dst[r0:r0 + 512, c0:c0 + w].rearrange("(a p) n -> p a n", p=128),
                                    in_=stt_[si][:, :, 0:w]), reads=[stt_t[si]], writes=[scr_t], dma=st_t[si])
                                ntb += 1
                        if is_iw:
                            op("sp", lambda e: e.dma_start(out=iw_s.rearrange("(t p) n -> p t n", p=128), in_=sti[:]),
                               reads=[sti_t], writes=[scr_t], dma=st_t[0])
        else:
            scr_t = Trk(multi=True)

        kb.barrier()
        yag_t = Trk(multi=True)
        if "C" in stages:
            with ExitStack() as st:
                stg = [sb(f"stgC{i}", [128, 8, 512], F32, st=st) for i in range(2)]
                stg_t = [Trk() for _ in range(2)]
                ld_stg = [kb.stream(f"ldstgC{i}") for i in range(2)]
                wgu = sb("wgu", [16, 512], BF16, st=st)
                bgr = sb("bgr", [1, 512], BF16, st=st)
                gngb = sb("gngb", [128, D], st=st)
                wba = sb("wba", [128, 8, D], BF16, st=st)
                wC = Trk(multi=True)
                op("sp", lambda e: e.dma_start(out=stg[0][0:16, 0, :], in_=wgu_d[:, :]), writes=[stg_t[0]], dma=ld_stg[0])
                op("pool", lambda e: e.tensor_copy(out=wgu[:], in_=stg[0][0:16, 0, :]), reads=[stg_t[0]], writes=[wC])
                op("sp", lambda e: e.dma_start(out=stg[1][0:1, 0, :], in_=bgate_d[:, :]), writes=[stg_t[1]], dma=ld_stg[1])
                op("pool", lambda e: e.tensor_copy(out=bgr[:], in_=stg[1][0:1, 0, :]), reads=[stg_t[1]], writes=[wC])
                op("sp", lambda e: e.dma_start(out=gngb[:], in_=gng_d[0:1, :].to_broadcast([128, D])), writes=[wC], dma=ld_stg[0])
                wba_v = wba_d.rearrange("(kc p) n -> p kc n", p=128)
                for half in range(2):
                    op("sp", lambda e, half=half: e.dma_start(out=stg[half][:], in_=wba_v[:, :, half * 512:(half + 1) * 512]),
                       writes=[stg_t[half]], dma=ld_stg[half])
                    op("pool", lambda e, half=half: e.tensor_copy(out=wba[:, :, half * 512:(half + 1) * 512], in_=stg[half][:]),
                       reads=[stg_t[half]], writes=[wC])

                qTc = [sb(f"qTc{i}", [128, 4, 128], BF16, st=st) for i in range(2)]
                kTc = [sb(f"kTc{i}", [128, 4, 128], BF16, st=st) for i in range(2)]
                ktok = [sb(f"ktok{i}", [128, 512], BF16, st=st) for i in range(2)]
                vc = [sb(f"vc{i}", [128, D], BF16, st=st) for i in range(2)]
                glc = [sb(f"glc{i}", [16, 128], BF16, st=st) for i in range(2)]
                src = [sb(f"src{i}", [128, D], BF16, st=st) for i in range(2)]
                szac = [sb(f"szac{i}", [128, D], BF16, st=st) for i in range(2)]
                in_t = [Trk(multi=True) for _ in range(2)]
                ld_C = [kb.stream(f"ldC{i}") for i in range(2)]
                qT_v = qT_s.rearrange("(h d) t -> d h t", d=128)
                kT_v = kT_s.rearrange("(h d) t -> d h t", d=128)

                def load_c(c):
                    i = c % 2
                    cs = slice(c * 128, (c + 1) * 128)
                    for dst, srcap in ((qTc[i][:], qT_v[:, :, cs]), (kTc[i][:], kT_v[:, :, cs]), (ktok[i][:], k_s[cs, :]),
                                       (vc[i][:], v_s[cs, :]), (glc[i][:], glT_s[:, cs]), (src[i][:], sr_s[cs, :]),
                                       (szac[i][:], sza_s[cs, :])):
                        op("sp", lambda e, dst=dst, srcap=srcap: e.dma_start(out=dst, in_=srcap),
                           reads=[scr_t], writes=[in_t[i]], dma=ld_C[i])

                sp_t = sb("sp_t", [128, 512], st=st)
                eke = sb("eke", [128, 512], st=st)
                kend = sb("kend", [128, 512], BF16, st=st)
                eq4 = sb("eq4", [128, 4, 128], st=st)
                ek4 = sb("ek4", [128, 4, 128], st=st)
                qd4 = sb("qd4", [128, 4, 128], BF16, st=st)
                ki4 = sb("ki4", [128, 4, 128], BF16, st=st)
                at4 = sb("at4", [128, 4, 128], BF16, st=st)
                S_f = sb("S_f", [128, 4, 256], st=st)
                S_b = sb("S_b", [128, 4, 256], BF16, st=st)
                ss4 = sb("ss4", [128, 12], st=st)
                junkC = sb("junkC", [128, 256], BF16, st=st)
                tmpC = sb("tmpC", [128, D], st=st)
                ga = sb("ga", [128, D], BF16, st=st)
                gaT = sb("gaT", [128, 8, 128], BF16, st=st)
                yag = [sb(f"yag{i}", [128, D], BF16, st=st) for i in range(2)]
                yag_bt = [Trk() for _ in range(2)]
                st_y = [kb.stream(f"sty{i}") for i in range(2)]
                (t_sp, t_eke, t_kend, t_eq, t_ek, t_qd, t_ki, t_at, t_ss4, t_junk, t_tmp, t_ga, t_gaT) = [Trk() for _ in range(13)]
                t_S = [Trk() for _ in range(4)]
                t_Sb = [Trk() for _ in range(4)]
                t_psC = t_psD = psT[2]
                op("dve", lambda e: e.memset(S_f[:], 0.0), writes=t_S)
                op("dve", lambda e: e.memset(S_b[:], 0.0), writes=t_Sb)

                load_c(0)
                for c in range(NT):
                    i = c % 2
                    if c + 1 < NT:
                        load_c(c + 1)
                    conv_step()
                    conv_step()
                    it = [in_t[i]]
                    op("pe", lambda e: e.matmul(ps[:, 0, :], lhsT=glc[i][0:16, :], rhs=wgu[0:16, :], start=True, stop=False),
                       reads=it + [wC], writes=[psT[0]])
                    op("pe", lambda e: e.matmul(ps[:, 0, :], lhsT=ones_row[0:1, :], rhs=bgr[0:1, :], start=False, stop=True),
                       reads=[cT, wC], writes=[psT[0]])
                    op("act", lambda e: e.activation(out=sp_t[:], in_=ps[:, 0, :], func=AF.Exp, scale=-1.0),
                       reads=[psT[0]], writes=[t_sp])
                    op("act", lambda e: e.activation(out=sp_t[:], in_=sp_t[:], func=AF.Ln, bias=one_t[:]),
                       reads=[t_sp, cT], writes=[t_sp])
                    op("pe", lambda e: e.matmul(ps[:, 1, :], lhsT=ut_f[:], rhs=sp_t[:], start=True, stop=True),
                       reads=[cT, t_sp], writes=[psT[1]])
                    op("act", lambda e: e.activation(out=eke[:], in_=ps[:, 1, :], func=AF.Exp, scale=-1.0 / 16),
                       reads=[psT[1]], writes=[t_eke])
                    op("dve", lambda e: e.tensor_tensor(out=kend[:], in0=ktok[i][:], in1=eke[:], op=ALU.mult),
                       reads=it + [t_eke], writes=[t_kend])
                    for h in range(4):
                        hs = slice(h * 128, (h + 1) * 128)
                        op("pe", lambda e, h=h, hs=hs: e.matmul(ps[:, 2, h * 128:(h + 1) * 128], lhsT=sp_t[:, hs], rhs=lt_f[:], start=True, stop=True),
                           reads=[t_sp, cT], writes=[psT[2]])
                    op("act", lambda e: e.activation(out=eq4[:].rearrange("p h t -> p (h t)"), in_=ps[:, 2, :], func=AF.Exp, scale=-1.0 / 16),
                       reads=[psT[2]], writes=[t_eq])
                    op("act", lambda e: e.activation(out=ek4[:].rearrange("p h t -> p (h t)"), in_=ps[:, 2, :], func=AF.Exp, scale=1.0 / 16),
                       reads=[psT[2]], writes=[t_ek])
                    op("dve", lambda e: e.scalar_tensor_tensor(out=qd4[:].rearrange("p h t -> p (h t)"),
                                                               in0=qTc[i][:].rearrange("p h t -> p (h t)"), scalar=128.0 ** -0.5,
                                                               in1=eq4[:].rearrange("p h t -> p (h t)"), op0=ALU.mult, op1=ALU.mult),
                       reads=it + [t_eq], writes=[t_qd])
                    op("dve", lambda e: e.tensor_tensor(out=ki4[:].rearrange("p h t -> p (h t)"), in0=kTc[i][:].rearrange("p h t -> p (h t)"),
                                                        in1=ek4[:].rearrange("p h t -> p (h t)"), op=ALU.mult),
                       reads=it + [t_ek], writes=[t_ki])
                    for h in range(4):
                        op("pe", lambda e, h=h: e.matmul(ps[:, 3, h * 128:(h + 1) * 128], lhsT=ki4[:, h, :], rhs=qd4[:, h, :], start=True, stop=True),
                           reads=[t_ki, t_qd], writes=[psT[3]])
                    op("dve", lambda e: e.tensor_tensor(out=at4[:], in0=ps[:, 3, :].rearrange("p (h t) -> p h t", h=4),
                                                        in1=lt_f[:].unsqueeze(1).to_broadcast([128, 4, 128]), op=ALU.mult),
                       reads=[psT[3], cT], writes=[t_at])
                    for h in range(4):
                        vs = slice(h * 256, (h + 1) * 256)
                        pso = ps[:, 4 + h // 2, (h % 2) * 256:(h % 2) * 256 + 256]
                        op("pe", lambda e, h=h, vs=vs, pso=pso: e.matmul(pso, lhsT=at4[:, h, :], rhs=vc[i][:, vs], start=True, stop=False),
                           reads=it + [t_at], writes=[psT[4 + h // 2]])
                        op("pe", lambda e, h=h, pso=pso: e.matmul(pso, lhsT=qd4[:, h, :], rhs=S_b[:, h, :], start=False, stop=True),
                           reads=[t_qd, t_Sb[0]], writes=[psT[4 + h // 2]])
                    for h in range(4):
                        hs = slice(h * 128, (h + 1) * 128)
                        vs = slice(h * 256, (h + 1) * 256)
                        pk = ps[:, 6 + h // 2, (h % 2) * 256:(h % 2) * 256 + 256]
                        op("pe", lambda e, hs=hs, vs=vs, pk=pk: e.matmul(pk, lhsT=kend[:, hs], rhs=vc[i][:, vs], start=True, stop=True),
                           reads=it + [t_kend], writes=[psT[6 + h // 2]])
                    for h in range(4):
                        pk = ps[:, 6 + h // 2, (h % 2) * 256:(h % 2) * 256 + 256]
                        op("dve", lambda e, h=h, pk=pk: e.scalar_tensor_tensor(out=S_f[:, h, :], in0=S_f[:, h, :], scalar=eq4[:, h, 127:128],
                                                                               in1=pk, op0=ALU.mult, op1=ALU.add),
                           reads=[t_S[0], t_eq, psT[6 + h // 2]], writes=[t_S[0]])
                    op("act", lambda e: e.activation(out=S_b[:].rearrange("p h v -> p (h v)"), in_=S_f[:].rearrange("p h v -> p (h v)"), func=AF.Copy),
                       reads=[t_S[0]], writes=[t_Sb[0]])
                    for h in range(4):
                        pso = ps[:, 4 + h // 2, (h % 2) * 256:(h % 2) * 256 + 256]
                        op("act", lambda e: e.activation(out=junkC[:], in_=pso, func=AF.Square, accum_out=ss4[:, h:h + 1]),
                           reads=[psT[4 + h // 2]], writes=[t_junk, t_ss4])
                    op("act", lambda e: e.activation(out=ss4[:, 4:8], in_=ss4[:, 0:4], func=AF.Sqrt, bias=eps_t[:], scale=1.0 / 256),
                       reads=[t_ss4, cT], writes=[t_ss4])
                    op("dve", lambda e: e.reciprocal(out=ss4[:, 8:12], in_=ss4[:, 4:8]), reads=[t_ss4], writes=[t_ss4])
                    for h in range(4):
                        vs = slice(h * 256, (h + 1) * 256)
                        pso = ps[:, 4 + h // 2, (h % 2) * 256:(h % 2) * 256 + 256]
                        op("dve", lambda e: e.scalar_tensor_tensor(out=tmpC[:, vs], in0=pso, scalar=ss4[:, 8 + h:9 + h],
                                                                   in1=gngb[:, vs], op0=ALU.mult, op1=ALU.mult),
                           reads=[psT[4 + h // 2], t_ss4, wC], writes=[t_tmp])
                    op("pool", lambda e: e.tensor_tensor(out=ga[:], in0=tmpC[:], in1=src[i][:], op=ALU.mult),
                       reads=it + [t_tmp], writes=[t_ga])
                    pbv = ps[:, 2, :].bitcast(BF16)
                    for kc in range(8):
                        op("pe", lambda e, kc=kc: e.transpose(out=pbv[:, kc * 128:(kc + 1) * 128],
                                                              in_=ga[:, kc * 128:(kc + 1) * 128], identity=ident_b[:]),
                           reads=[t_ga, cT], writes=[psT[2]])
                    op("act", lambda e: e.activation(out=gaT[:], in_=pbv.rearrange("p (k t) -> p k t", k=8), func=AF.Copy),
                       reads=[psT[2]], writes=[t_gaT])
                    for half in range(2):
                        yb_ = (3, 1)[half]
                        for kc in range(8):
                            op("pe", lambda e, kc=kc, yb_=yb_: e.matmul(ps[:, yb_, :], lhsT=gaT[:, kc, :],
                                                                        rhs=wba[:, kc, half * 512:(half + 1) * 512],
                                                                        start=(kc == 0), stop=(kc == 7)),
                               reads=[t_gaT, wC], writes=[psT[yb_]])
                        op("dve", lambda e, yb_=yb_: e.tensor_tensor(out=yag[i][:, half * 512:(half + 1) * 512], in0=ps[:, yb_, :],
                                                                     in1=szac[i][:, half * 512:(half + 1) * 512], op=ALU.mult),
                           reads=it + [psT[yb_]], writes=[yag_bt[i]])
                    op("sp", lambda e: e.dma_start(out=yag_s[c * 128:(c + 1) * 128, :], in_=yag[i][:]),
                       reads=[yag_bt[i]], writes=[yag_t], dma=st_y[i])

        if "E" in stages and "C" in stages:
            while cvs["n"] < NCV or cvs["pending"] is not None:
                conv_step()
        kb.barrier()
        stBC.close()
        x1_t = Trk(multi=True)
        if "D" in stages:
            with ExitStack() as st:
                adab, (G1,) = load_ada(st, "adaD", [G1])
                wuk = sb("wuk", [128, 8, 256], BF16, st=st)
                wuv = sb("wuv", [128, 8, 2, 128], BF16, st=st)
                wbb = sb("wbb", [128, 8, D], BF16, st=st)
                wout = sb("wout", [128, 8, D], BF16, st=st)
                kvgb = sb("kvgb", [128, 256], st=st)
                wD = Trk(multi=True)
                with ExitStack() as st2:
                    stg = [sb(f"stgD{i}", [128, 8, 512], F32, st=st2) for i in range(2)]
                    stg_t = [Trk() for _ in range(2)]
                    ld_stg = [kb.stream(f"ldstgD{i}") for i in range(2)]
                    op("sp", lambda e: e.dma_start(out=stg[0][:, :, 0:256], in_=wuk_d.rearrange("h d c -> d h c")),
                       writes=[stg_t[0]], dma=ld_stg[0])
                    op("pool", lambda e: e.tensor_copy(out=wuk[:], in_=stg[0][:, :, 0:256]), reads=[stg_t[0]], writes=[wD])
                    s1v = stg[1][:, 0:4, :].rearrange("p a (b d) -> p (a b) d", d=128)
                    op("sp", lambda e: e.dma_start(out=s1v, in_=wuv_d.rearrange("h (cc p) d -> p (h cc) d", p=128)),
                       writes=[stg_t[1]], dma=ld_stg[1])
                    op("pool", lambda e: e.tensor_copy(out=wuv[:].rearrange("p h c d -> p (h c) d"), in_=s1v),
                       reads=[stg_t[1]], writes=[wD])
                    op("sp", lambda e: e.dma_start(out=kvgb[:], in_=kvg_d[0:1, :].to_broadcast([128, 256])), writes=[wD], dma=ld_stg[0])
                    k = 0
                    for wdst, wsrc in ((wbb, wbb_d), (wout, wout_d)):
                        wv_ = wsrc.rearrange("(kc p) n -> p kc n", p=128)
                        for half in range(2):
                            i = k % 2
                            k += 1
                            op("sp", lambda e, i=i, half=half, wv_=wv_: e.dma_start(out=stg[i][:], in_=wv_[:, :, half * 512:(half + 1) * 512]),
                               writes=[stg_t[i]], dma=ld_stg[i])
                            op("pool", lambda e, i=i, half=half, wdst=wdst: e.tensor_copy(out=wdst[:, :, half * 512:(half + 1) * 512], in_=stg[i][:]),
                               reads=[stg_t[i]], writes=[wD])
                    kb.barrier()

                ckv = sb("ckv", [128, NT, 257], BF16, st=st)
                ckvT = sb("ckvT", [128, 2, S], BF16, st=st)
                ikT = sb("ikT", [64, S], BF16, st=st)
                iw_all = sb("iw_all", [128, NT, 8], st=st)
                posl = sb("posl", [128, 128], st=st)
                basej = sb("basej", [128, NT], st=st)
                slopes8 = sb("slopes8", [128, 8], st=st)
                esel = sb("esel", [8, 8, 128], BF16, st=st)
                posr = sb("posr", [10, 32, 128], BF16, st=st)
                slopeR = sb("slopeR", [10, 8, 128], BF16, st=st)
                ones8 = sb("ones8", [8, 128], BF16, st=st)
                ones_col = sb("ones_col", [128, 1], BF16, st=st)
                cD = Trk(multi=True)
                ld_cD = kb.stream("ldcD")
                st3 = ExitStack()
                esel_f = sb("esel_f", [8, 1024], st=st3)
                posr_f = sb("posr_f", [10, 32 * 128], st=st3)
                slopeR_f = sb("slopeR_f", [10, 1024], st=st3)
                op("sp", lambda e: e.dma_start(out=posl[:], in_=cst["c_pos1"][:, 0:128]), writes=[cD], dma=ld_cD)
                op("sp", lambda e: e.dma_start(out=basej[:], in_=cst["c_basej"][:, :]), writes=[cD], dma=ld_cD)
                op("sp", lambda e: e.dma_start(out=slopes8[:], in_=cst["c_slopes8"][:, :]), writes=[cD], dma=ld_cD)
                op("sp", lambda e: e.dma_start(out=esel_f[:], in_=cst["c_esel"][:, :]), writes=[cD], dma=ld_cD)
                op("sp", lambda e: e.dma_start(out=posr_f[:], in_=cst["c_posr"][:, :]), writes=[cD], dma=ld_cD)
                op("sp", lambda e: e.dma_start(out=slopeR_f[:], in_=cst["c_slopeR"][:, :]), writes=[cD], dma=ld_cD)
                op("dve", lambda e: e.tensor_copy(out=esel[:].rearrange("k h s -> k (h s)"), in_=esel_f[:]), reads=[cD], writes=[cD])
                op("dve", lambda e: e.tensor_copy(out=posr[:].rearrange("k a s -> k (a s)"), in_=posr_f[:]), reads=[cD], writes=[cD])
                op("dve", lambda e: e.tensor_copy(out=slopeR[:].rearrange("k h q -> k (h q)"), in_=slopeR_f[:]), reads=[cD], writes=[cD])
                op("dve", lambda e: e.memset(ones8[:], 1.0), writes=[cD])
                op("dve", lambda e: e.memset(ones_col[:], 1.0), writes=[cD])
                dkv_all = sb("dkv_all", [128, NT, 256], BF16, st=st3)
                t_dkv, t_ik, t_iw = Trk(), Trk(), Trk()
                ckv_t = [Trk() for _ in range(NT)]
                ckvT_t = [Trk() for _ in range(NT)]
                ld_d0 = kb.stream("ldd0")
                op("sp", lambda e: e.dma_start(out=dkv_all[:], in_=dkv_s.rearrange("(t p) c -> p t c", p=128)),
                   reads=[scr_t], writes=[t_dkv], dma=ld_d0)
                op("sp", lambda e: e.dma_start(out=ikT[:], in_=ikT_s[:, :]), reads=[scr_t], writes=[t_ik], dma=ld_d0)
                op("sp", lambda e: e.dma_start(out=iw_all[:], in_=iw_s.rearrange("(t p) n -> p t n", p=128)),
                   reads=[scr_t], writes=[t_iw], dma=ld_d0)
                ssd = sb("ssd", [128, 3 * NT], st=st3)
                t_ssd = Trk()
                junk0 = sb("junk0", [128, 256], BF16, st=st3)
                t_junk0 = Trk()
                for tt in range(NT):
                    op("act", lambda e: e.activation(out=junk0[:], in_=dkv_all[:, tt, :], func=AF.Square,
                                                     accum_out=ssd[:, tt:tt + 1]), reads=[t_dkv], writes=[t_junk0, t_ssd])
                op("act", lambda e: e.activation(out=ssd[:, NT:2 * NT], in_=ssd[:, 0:NT], func=AF.Sqrt, bias=eps_t[:], scale=1.0 / 256),
                   reads=[t_ssd, cT], writes=[t_ssd])
                op("dve", lambda e: e.reciprocal(out=ssd[:, 2 * NT:3 * NT], in_=ssd[:, NT:2 * NT]), reads=[t_ssd], writes=[t_ssd])
                op("dve", lambda e: e.memset(ckv[:, :, 256:257], 1.0), writes=ckv_t)
                for tt in range(NT):
                    op("dve", lambda e: e.scalar_tensor_tensor(out=ckv[:, tt, 0:256], in0=dkv_all[:, tt, :],
                                                               scalar=ssd[:, 2 * NT + tt:2 * NT + tt + 1], in1=kvgb[:],
                                                               op0=ALU.mult, op1=ALU.mult),
                       reads=[t_dkv, t_ssd, wD], writes=[ckv_t[tt]])
                    pb = 4 + (tt % 2)
                    pbv = ps[:, pb, :].bitcast(BF16)
                    for cc in range(2):
                        op("pe", lambda e, cc=cc: e.transpose(out=pbv[:, cc * 128:(cc + 1) * 128],
                                                              in_=ckv[:, tt, cc * 128:(cc + 1) * 128], identity=ident_b[:]),
                           reads=[ckv_t[tt], cT], writes=[psT[pb]])
                    op("act", lambda e: e.activation(out=ckvT[:, :, tt * 128:(tt + 1) * 128],
                                                     in_=pbv[:, 0:256].rearrange("p (c t) -> p c t", c=2), func=AF.Copy),
                       reads=[psT[pb]], writes=[ckvT_t[tt]])

                kb.barrier()
                st3.close()
                iqc = [sb(f"iqc{i}", [64, 8, 128], BF16, st=st) for i in range(2)]
                dqc = [sb(f"dqc{i}", [128, 8, 128], BF16, st=st) for i in range(2)]
                szbc = [sb(f"szbc{i}", [128, D], BF16, st=st) for i in range(2)]
                yagc = [sb(f"yagc{i}", [128, D], BF16, st=st) for i in range(2)]
                xq1 = sb("xq", [128, D], st=st)
                xq = [xq1, xq1]
                xq_t = Trk()
                ld_xq = kb.stream("ldxq")
                inI_t = [Trk(multi=True) for _ in range(2)]
                inA_t = [Trk(multi=True) for _ in range(2)]
                ld_I = [kb.stream(f"ldI{i}") for i in range(2)]
                ld_A = [kb.stream(f"ldA{i}") for i in range(2)]
                iqT_v = iqT_s.rearrange("(h d) t -> d h t", d=64)
                dqT_v = dqT_s.rearrange("(h d) t -> d h t", d=128)
                acc = sb("accD", [128, S], st=st)
                t_acc = Trk()
                relb = [sb(f"relb{i}", [128, 512], BF16, st=st) for i in range(2)]
                relb_t = [Trk() for _ in range(2)]
                Dg = sb("Dg", [128, 8, 128], BF16, st=st)
                t_Dg = Trk()
                bs = sb("bs", [128, 8], st=st)
                t_bs = Trk()
                sel = sb("sel", [128, S], BF16, st=st)
                t_sel = Trk()
                selT = [sb(f"selT{i}", [128, NT, 128], BF16, st=st) for i in range(2)]
                selT_t = [Trk() for _ in range(2)]
                qlat = sb("qlat", [128, 8, 2, 128], BF16, st=st)
                t_qlat = Trk()
                pT = [sb(f"pT{i}", [128, 4, 128], BF16, st=st) for i in range(3)]
                pT_t = [Trk() for _ in range(3)]
                oT = sb("oT", [128, 8, 128], BF16, st=st)
                t_oT = Trk()
                tmpD = sb("tmpD", [128, D], st=st)
                t_tmpD = Trk()
                ymix = sb("ymix", [128, D], BF16, st=st)
                t_ymix = Trk()
                ymixT = sb("ymixT", [128, 8, 128], BF16, st=st)
                t_ymixT = Trk()
                x1b1 = sb("x1b", [128, D], st=st)
                x1b = [x1b1, x1b1]
                x1b_t1 = Trk()
                x1b_t = [x1b_t1, x1b_t1]
                st_x11 = kb.stream("stx1")
                st_x1 = [st_x11, st_x11]
                nm_bias = sb("nm_bias", [128, 1], st=st)
                op("dve", lambda e: e.memset(nm_bias[:], -30000.0), writes=[cT])
                neg29 = sb("neg29", [128, 1], st=st)
                op("dve", lambda e: e.memset(neg29[:], -1.0e29), writes=[cT])
                corrD = [sb(f"corrD{i}", [10, 8, 128], BF16, st=st) for i in range(2)]
                corrD_t = [Trk() for _ in range(2)]
                for i_ in range(2):
                    op("dve", lambda e, i_=i_: e.tensor_copy(out=corrD[i_][:], in_=slopeR[:]), reads=[cD], writes=[corrD_t[i_]])
                rsrow = sb("rsrow", [1, 512], st=st)
                rsb = sb("rsb", [1, 512], BF16, st=st)
                t_rs = Trk()
                olT = sb("olT", [128, 2, 512], BF16, st=st)
                t_olT = Trk()
                rsB = sb("rsB", [128, 512], st=st)
                t_rsB = Trk()
                corr8 = sb("corr8", [128, 10], st=st)
                cm = sb("cm", [128, 2 * NT], st=st)
                t_corr8 = Trk()
                lg_t = [Trk() for _ in range(8)]
                NBIS = 13

                def load_I(qt):
                    i = qt % 2
                    qs = slice(qt * 128, (qt + 1) * 128)
                    op("sp", lambda e: e.dma_start(out=iqc[i][:], in_=iqT_v[:, :, qs]), reads=[scr_t], writes=[inI_t[i]], dma=ld_I[i])

                def load_A(qt):
                    i = qt % 2
                    qs = slice(qt * 128, (qt + 1) * 128)
                    op("sp", lambda e: e.dma_start(out=dqc[i][:], in_=dqT_v[:, :, qs]), reads=[scr_t], writes=[inA_t[i]], dma=ld_A[i])
                    op("sp", lambda e: e.dma_start(out=szbc[i][:], in_=szb_s[qs, :]), reads=[scr_t], writes=[inA_t[i]], dma=ld_A[i])
                    op("sp", lambda e: e.dma_start(out=yagc[i][:], in_=yag_s[qs, :]), reads=[yag_t], writes=[inA_t[i]], dma=ld_A[i])

                def idx_phase(qt):
                    i = qt % 2
                    Sk = (qt + 1) * 128
                    nkb = (Sk + 511) // 512
                    op("dve", lambda e: e.tensor_tensor(out=Dg[:], in0=ident_b[:].unsqueeze(1).to_broadcast([128, 8, 128]),
                                                        in1=iw_all[:, qt, :].unsqueeze(2).to_broadcast([128, 8, 128]), op=ALU.mult),
                       reads=[cT, t_iw], writes=[t_Dg])
                    nrel = 0
                    for kbi in range(nkb):
                        w = min(512, Sk - kbi * 512)
                        ks = slice(kbi * 512, kbi * 512 + w)
                        prev = None
                        for h in range(8):
                            ri = nrel % 2
                            rb = (0, 2)[nrel % 2]
                            nrel += 1
                            op("pe", lambda e: e.matmul(ps[:, rb, 0:w], lhsT=iqc[i][0:64, h, :], rhs=ikT[0:64, ks], start=True, stop=True),
                               reads=[inI_t[i], t_ik], writes=[psT[rb]])
                            if prev is not None:
                                ph, pri = prev
                                op("pe", lambda e: e.matmul(ps[:, 1, 0:w], lhsT=Dg[:, ph, :], rhs=relb[pri][:, 0:w], start=(ph == 0), stop=False),
                                   reads=[t_Dg, relb_t[pri]], writes=[psT[1]])
                                yield
                            op("act", lambda e: e.activation(out=relb[ri][:, 0:w], in_=ps[:, rb, 0:w], func=AF.Relu),
                               reads=[psT[rb]], writes=[relb_t[ri]])
                            prev = (h, ri)
                        ph, pri = prev
                        op("pe", lambda e: e.matmul(ps[:, 1, 0:w], lhsT=Dg[:, ph, :], rhs=relb[pri][:, 0:w], start=False, stop=True),
                           reads=[t_Dg, relb_t[pri]], writes=[psT[1]])
                        yield
                        if kbi == nkb - 1:
                            if w > 128:
                                op("dve", lambda e: e.tensor_copy(out=acc[:, kbi * 512:kbi * 512 + w - 128], in_=ps[:, 1, 0:w - 128]),
                                   reads=[psT[1]], writes=[t_acc])
                            op("dve", lambda e: e.tensor_tensor(out=acc[:, Sk - 128:Sk], in0=ps[:, 1, w - 128:w], in1=diag_f[:], op=ALU.add),
                               reads=[psT[1], cT], writes=[t_acc])
                        else:
                            op("dve", lambda e: e.tensor_copy(out=acc[:, ks], in_=ps[:, 1, 0:w]), reads=[psT[1]], writes=[t_acc])
                    if qt >= 2:
                        op("dve", lambda e: e.tensor_reduce(out=bs[:, 0:1], in_=acc[:, 0:Sk - 128], axis=mybir.AxisListType.X, op=ALU.min),
                           reads=[t_acc], writes=[t_bs])
                        op("dve", lambda e: e.tensor_reduce(out=bs[:, 5:6], in_=acc[:, 0:Sk], axis=mybir.AxisListType.X, op=ALU.max),
                           reads=[t_acc], writes=[t_bs])
                        op("dve", lambda e: e.tensor_tensor(out=bs[:, 1:2], in0=bs[:, 5:6], in1=bs[:, 0:1], op=ALU.subtract),
                           reads=[t_bs], writes=[t_bs])
                        for it in range(NBIS):
                            f = 2.0 ** -(it + 1)
                            op("dve", lambda e: e.scalar_tensor_tensor(out=bs[:, 2:3], in0=bs[:, 1:2], scalar=f, in1=bs[:, 0:1],
                                                                       op0=ALU.mult, op1=ALU.add), reads=[t_bs], writes=[t_bs])
                            op("dve", lambda e: e.tensor_scalar(out=sel[:, 0:Sk], in0=acc[:, 0:Sk], scalar1=bs[:, 2:3], scalar2=None,
                                                                op0=ALU.is_ge, op1=ALU.add, accum_out=bs[:, 3:4]),
                               reads=[t_acc, t_bs], writes=[t_sel, t_bs])
                            op("dve", lambda e: e.tensor_scalar(out=bs[:, 4:5], in0=bs[:, 3:4], scalar1=255.5, scalar2=f,
                                                                op0=ALU.is_ge, op1=ALU.mult), reads=[t_bs], writes=[t_bs])
                            op("dve", lambda e: e.scalar_tensor_tensor(out=bs[:, 0:1], in0=bs[:, 4:5], scalar=bs[:, 1:2], in1=bs[:, 0:1],
                                                                       op0=ALU.mult, op1=ALU.add), reads=[t_bs], writes=[t_bs])
                            yield "BIS"
                        thr = bs[:, 0:1]
                    else:
                        thr = neg29[:]
                    op("dve", lambda e: e.tensor_scalar(out=sel[:, 0:Sk], in0=acc[:, 0:Sk], scalar1=thr, scalar2=None, op0=ALU.is_ge),
                       reads=[t_acc, t_bs, cT], writes=[t_sel])
                    nch = qt + 1
                    op("dve", lambda e: e.tensor_tensor(out=acc[:, 0:Sk].rearrange("p (j s) -> p j s", s=128),
                                                        in0=sel[:, 0:Sk].rearrange("p (j s) -> p j s", s=128),
                                                        in1=posl[:].unsqueeze(1).to_broadcast([128, nch, 128]), op=ALU.mult),
                       reads=[t_sel, cD], writes=[t_acc])
                    op("dve", lambda e: e.tensor_reduce(out=cm[:, 0:nch], in_=acc[:, 0:Sk].rearrange("p (j s) -> p j s", s=128),
                                                        axis=mybir.AxisListType.X, op=ALU.max), reads=[t_acc], writes=[t_corr8])
                    op("dve", lambda e: e.tensor_scalar(out=cm[:, NT:NT + nch], in0=cm[:, 0:nch], scalar1=0.5, scalar2=None, op0=ALU.is_ge),
                       reads=[t_corr8], writes=[t_corr8])
                    op("dve", lambda e: e.tensor_tensor(out=cm[:, NT:NT + nch], in0=cm[:, NT:NT + nch], in1=basej[:, 0:nch], op=ALU.mult),
                       reads=[t_corr8, cD], writes=[t_corr8])
                    op("dve", lambda e: e.tensor_tensor(out=cm[:, 0:nch], in0=cm[:, 0:nch], in1=cm[:, NT:NT + nch], op=ALU.add),
                       reads=[t_corr8], writes=[t_corr8])
                    op("dve", lambda e: e.tensor_reduce(out=corr8[:, 8:9], in_=cm[:, 0:nch], axis=mybir.AxisListType.X, op=ALU.max),
                       reads=[t_corr8], writes=[t_corr8])
                    op("dve", lambda e: e.tensor_scalar(out=corr8[:, 9:10], in0=corr8[:, 8:9], scalar1=-1.0, scalar2=float(Sk),
                                                        op0=ALU.mult, op1=ALU.add), reads=[t_corr8], writes=[t_corr8])
                    op("dve", lambda e: e.tensor_scalar(out=corr8[:, 0:8], in0=slopes8[:], scalar1=corr8[:, 9:10], scalar2=None, op0=ALU.mult),
                       reads=[t_corr8, cD], writes=[t_corr8])
                    yield "HOLD"
                    op("pe", lambda e: e.transpose(out=ps[0:8, 2, 0:128], in_=corr8[:, 0:8], identity=ident_f[:]),
                       reads=[t_corr8, cT], writes=[psT[2]])
                    op("dve", lambda e: e.tensor_tensor(out=corrD[i][0:8, :, :], in0=esel[:], in1=ps[0:8, 2, 0:128].unsqueeze(1).to_broadcast([8, 8, 128]),
                                                        op=ALU.mult), reads=[psT[2], cD], writes=[corrD_t[i]])
                    yield
                    for j0 in range(0, qt + 1, 8):
                        nj = min(8, qt + 1 - j0)
                        pbv = ps[:, 2, :].bitcast(BF16)
                        for jj in range(nj):
                            j = j0 + jj
                            op("pe", lambda e: e.transpose(out=pbv[:, jj * 128:(jj + 1) * 128], in_=sel[:, j * 128:(j + 1) * 128], identity=ident_b[:]),
                               reads=[t_sel, cT], writes=[psT[2]])
                        op("act", lambda e: e.activation(out=selT[i][:, j0:j0 + nj, :],
                                                         in_=pbv[:, 0:nj * 128].rearrange("p (j t) -> p j t", t=128), func=AF.Identity,
                                                         scale=30000.0, bias=nm_bias[:]),
                           reads=[psT[2], cT], writes=[selT_t[i]])
                        yield

                pend = {"g": None}

                def pump(n=1, release=False):
                    g = pend["g"]
                    if g is None:
                        return
                    if pend.get("held") and not release:
                        return
                    pend["held"] = False
                    for _ in range(n):
                        try:
                            r = next(g)
                            if r == "HOLD" and not release:
                                pend["held"] = True
                                return
                            if r == "BIS" and not release:
                                pend["bis"] = pend.get("bis", 0) + 1
                                if pend["bis"] >= pend.get("bis_per_pump", 1):
                                    pend["bis"] = 0
                                    return
                        except StopIteration:
                            pend["g"] = None
                            return

                def att_phase(qt):
                    i = qt % 2
                    qs = slice(qt * 128, (qt + 1) * 128)
                    ia = [inA_t[i]]
                    for g in range(4):
                        for u in range(4):
                            hc = g * 4 + u
                            h, cc = hc // 2, hc % 2
                            op("pe", lambda e: e.matmul(ps[:, 2, u * 128:(u + 1) * 128], lhsT=wuk[:, h, cc * 128:(cc + 1) * 128],
                                                        rhs=dqc[i][:, h, :], start=True, stop=True),
                               reads=ia + [wD], writes=[psT[2]])
                        op("act", lambda e: e.activation(out=qlat[:, g * 2:g * 2 + 2, :, :].rearrange("p h c q -> p (h c q)"),
                                                         in_=ps[:, 2, :], func=AF.Copy, scale=128.0 ** -0.5),
                           reads=[psT[2]], writes=[t_qlat])
                        pump(6)
                    def emit_lg(g, j, k):
                        hs4 = slice(4 * g, 4 * g + 4)
                        lb = 3 + (k % 2)
                        lgb = ps[:, lb, :]
                        dl = j - qt + 31
                        for cc in range(2):
                            op("pe", lambda e, cc=cc: e.matmul(lgb, lhsT=ckvT[:, cc, j * 128:(j + 1) * 128], rhs=qlat[:, hs4, cc, :],
                                                               start=(cc == 0), stop=False),
                               reads=[ckvT_t[j], t_qlat], writes=[psT[lb]])
                        op("pe", lambda e: e.matmul(lgb, lhsT=posr[0:10, dl, :], rhs=corrD[i][0:10, hs4, :], start=False, stop=False),
                           reads=[cD, corrD_t[i]], writes=[psT[lb]])
                        op("pe", lambda e: e.matmul(lgb, lhsT=ident_b[:], rhs=selT[i][:, j, :].unsqueeze(1).to_broadcast([128, 4, 128]),
                                                    start=False, stop=True),
                           reads=[cT, selT_t[i]], writes=[psT[lb]])

                    def emit_exp_pv(g, j, k):
                        lb = 3 + (k % 2)
                        pi = k % 3
                        pTf = pT[pi][:].rearrange("p h q -> p (h q)")
                        op("act", lambda e: e.activation(out=pTf, in_=ps[:, lb, :], func=AF.Exp), reads=[psT[lb]], writes=[pT_t[pi]])
                        for cc in range(2):
                            op("pe", lambda e, cc=cc: e.matmul(ps[:, 5 + cc, :], lhsT=ckv[:, j, cc * 128:(cc + 1) * 128], rhs=pTf,
                                                               start=(j == 0), stop=(j == qt)),
                               reads=[pT_t[pi], ckv_t[j]], writes=[psT[5 + cc]])
                        op("pe", lambda e: e.matmul(ps[0:1, 7, :], lhsT=ones_col[:, 0:1], rhs=pTf, start=(j == 0), stop=(j == qt)),
                           reads=[pT_t[pi], cD], writes=[psT[7]])

                    kstep = 0
                    for g in range(2):
                        hs4 = slice(4 * g, 4 * g + 4)
                        emit_lg(g, 0, kstep)
                        for j in range(qt + 1):
                            if j + 1 <= qt:
                                emit_lg(g, j + 1, kstep + 1)
                            lbk = 3 + (kstep % 2)
                            emit_exp_pv(g, j, kstep)
                            kstep += 1
                            pump(6)
                        op("act", lambda e: e.activation(out=rsrow[0:1, :], in_=ps[0:1, 7, :], func=AF.Ln), reads=[psT[7]], writes=[t_rs])
                        op("act", lambda e: e.activation(out=rsb[0:1, :], in_=rsrow[0:1, :], func=AF.Exp, scale=-1.0), reads=[t_rs], writes=[t_rs])
                        op("act", lambda e: e.activation(out=olT[:, 0, :], in_=ps[:, 5, :], func=AF.Copy), reads=[psT[5]], writes=[t_olT])
                        op("act", lambda e: e.activation(out=olT[:, 1, :], in_=ps[:, 6, :], func=AF.Copy), reads=[psT[6]], writes=[t_olT])
                        op("pe", lambda e: e.matmul(ps[:, 2, :], lhsT=ones_row[0:1, :], rhs=rsb[0:1, :], start=True, stop=True),
                           reads=[cT, t_rs], writes=[psT[2]])
                        op("act", lambda e: e.activation(out=rsB[:], in_=ps[:, 2, :], func=AF.Copy), reads=[psT[2]], writes=[t_rsB])
                        for u in range(4):
                            h = 4 * g + u
                            for cc in range(2):
                                op("pe", lambda e, cc=cc: e.matmul(ps[:, 2, u * 128:(u + 1) * 128], lhsT=wuv[:, h, cc, :],
                                                                   rhs=olT[:, cc, u * 128:(u + 1) * 128], start=(cc == 0), stop=(cc == 1)),
                                   reads=[wD, t_olT], writes=[psT[2]])
                        op("dve", lambda e: e.tensor_tensor(out=oT[:, hs4, :].rearrange("p h q -> p (h q)"), in0=ps[:, 2, :], in1=rsB[:], op=ALU.mult),
                           reads=[psT[2], t_rsB], writes=[t_oT])
                        pump(6)
                    for half in range(2):
                        hsl = slice(half * 512, (half + 1) * 512)
                        for h in range(8):
                            op("pe", lambda e, h=h: e.matmul(ps[:, 2, :], lhsT=oT[:, h, :], rhs=wbb[:, h, hsl], start=(h == 0), stop=(h == 7)),
                               reads=[t_oT, wD], writes=[psT[2]])
                        op("dve", lambda e: e.tensor_tensor(out=tmpD[:, hsl], in0=ps[:, 2, :], in1=szbc[i][:, hsl], op=ALU.mult),
                           reads=ia + [psT[2]], writes=[t_tmpD])
                        pump(6)
                    op("pool", lambda e: e.tensor_tensor(out=ymix[:], in0=tmpD[:], in1=yagc[i][:], op=ALU.add),
                       reads=ia + [t_tmpD], writes=[t_ymix])
                    pbv = ps[:, 2, :].bitcast(BF16)
                    for kc in range(8):
                        op("pe", lambda e, kc=kc: e.transpose(out=pbv[:, kc * 128:(kc + 1) * 128], in_=ymix[:, kc * 128:(kc + 1) * 128],
                                                              identity=ident_b[:]), reads=[t_ymix, cT], writes=[psT[2]])
                    op("act", lambda e: e.activation(out=ymixT[:], in_=pbv.rearrange("p (k t) -> p k t", k=8), func=AF.Copy),
                       reads=[psT[2]], writes=[t_ymixT])
                    pump(6)
                    for half in range(2):
                        hsl = slice(half * 512, (half + 1) * 512)
                        for kc in range(8):
                            op("pe", lambda e, kc=kc: e.matmul(ps[:, 2, :], lhsT=ymixT[:, kc, :], rhs=wout[:, kc, hsl],
                                                               start=(kc == 0), stop=(kc == 7)),
                               reads=[t_ymixT, wD], writes=[psT[2]])
                        op("dve", lambda e: e.tensor_tensor(out=tmpD[:, hsl], in0=ps[:, 2, :], in1=adab[:, G1][:, hsl], op=ALU.mult),
                           reads=[psT[2], adaT], writes=[t_tmpD])
                        pump(6)
                    op("sp", lambda e: e.dma_start(out=xq1[:], in_=x_d[qs, :]), writes=[xq_t], dma=ld_xq)
                    op("pool", lambda e: e.tensor_tensor(out=x1b[i][:], in0=tmpD[:], in1=xq1[:], op=ALU.add),
                       reads=[xq_t, t_tmpD], writes=[x1b_t[i]])
                    op("sp", lambda e: e.dma_start(out=x1_s[qs, :], in_=x1b[i][:]), reads=[x1b_t[i]], writes=[x1_t], dma=st_x1[i])

                def run_idx(qt):
                    for _ in idx_phase(qt):
                        pass

                dlim = os.environ.get("DLIM", "")
                if dlim == "setup":
                    pass
                elif dlim.startswith("idx"):
                    nq = int(dlim[3:])
                    for qt in range(nq):
                        load_I(qt)
                        run_idx(qt)
                    if "dbg_acc" in debug:
                        dacc = nc.dram_tensor("dbg_acc", [128, S], F32, kind="ExternalOutput").ap()
                        dsel = nc.dram_tensor("dbg_sel", [128, S], BF16, kind="ExternalOutput").ap()
                        dbs = nc.dram_tensor("dbg_bs", [128, 8], F32, kind="ExternalOutput").ap()
                        op("sp", lambda e: e.dma_start(out=dacc[:, :], in_=acc[:]), reads=[t_acc], writes=[x1_t], dma=st_x1[0])
                        op("sp", lambda e: e.dma_start(out=dsel[:, :], in_=sel[:]), reads=[t_sel], writes=[x1_t], dma=st_x1[0])
                        op("sp", lambda e: e.dma_start(out=dbs[:, :], in_=bs[:]), reads=[t_bs], writes=[x1_t], dma=st_x1[0])
                elif dlim.startswith("qts"):
                    for qt in [int(v) for v in dlim[3:].split("_")]:
                        load_I(qt)
                        load_A(qt)
                        run_idx(qt)
                        att_phase(qt)
                elif dlim.startswith("att"):
                    nq = int(dlim[3:])
                    for qt in range(nq):
                        load_I(qt)
                        load_A(qt)
                        run_idx(qt)
                        att_phase(qt)
                else:
                    load_I(0)
                    load_A(0)
                    run_idx(0)
                    for qt in range(NT):
                        if qt + 1 < NT:
                            load_I(qt + 1)
                            load_A(qt + 1)
                            pend["g"] = idx_phase(qt + 1)
                            pend["bis_per_pump"] = 3 if qt < 6 else (2 if qt < 12 else 1)
                            pend["bis"] = 0
                        att_phase(qt)
                        pump(100000, release=True)

        kb.barrier()
        out_t = Trk(multi=True)
        if "E" in stages:
            with ExitStack() as st:
                adae, (SH2, A2, G2) = load_ada(st, "adaE", [SH2, A2, G2])
                wq = sb("wq", [128, 8, 2048], BF16, st=st)
                KT = sb("KT", [128, 2, 128], BF16, st=st)
                fgb = sb("fgb", [128, D], st=st)
                wE = Trk(multi=True)
                with ExitStack() as st2:
                    stg = [sb(f"stgE{i}", [128, 8, 512], F32, st=st2) for i in range(2)]
                    stg_t = [Trk() for _ in range(2)]
                    ld_stg = [kb.stream(f"ldstgE{i}") for i in range(2)]
                    wq_v = wq_d.rearrange("(kc p) n -> p kc n", p=128)
                    for q4 in range(4):
                        i = q4 % 2
                        op("sp", lambda e, i=i, q4=q4: e.dma_start(out=stg[i][:], in_=wq_v[:, :, q4 * 512:(q4 + 1) * 512]),
                           writes=[stg_t[i]], dma=ld_stg[i])
                        op("pool", lambda e, i=i, q4=q4: e.tensor_copy(out=wq[:, :, q4 * 512:(q4 + 1) * 512], in_=stg[i][:]),
                           reads=[stg_t[i]], writes=[wE])
                    for half, skd in enumerate((sk1_d, sk2_d)):
                        op("sp", lambda e, half=half, skd=skd: e.dma_start(out=stg[half][:, 0, 0:128], in_=skd[:, :]),
                           writes=[stg_t[half]], dma=ld_stg[half])
                        op("pe", lambda e, half=half: e.transpose(out=ps[:, half, 0:128], in_=stg[half][:, 0, 0:128], identity=ident_f[:]),
                           reads=[stg_t[half], cT], writes=[psT[half]])
                        op("act", lambda e, half=half: e.activation(out=KT[:, half, :], in_=ps[:, half, 0:128], func=AF.Copy),
                           reads=[psT[half]], writes=[wE])
                    op("sp", lambda e: e.dma_start(out=fgb[:], in_=fg_d[0:1, :].to_broadcast([128, D])), writes=[wE], dma=ld_stg[0])
                    kb.barrier()

                x1t = [sb(f"x1t{i}", [128, D], st=st) for i in range(3)]
                x1t_t = [Trk() for _ in range(3)]
                ld_x1 = [kb.stream(f"ldx1{i}") for i in range(3)]
                ssE = sb("ssE", [128, 8], st=st)
                t_ssE = Trk()
                junkE = sb("junkE", [128, D], BF16, st=st)
                t_junkE = Trk()
                tmp4 = sb("tmp4", [128, D], st=st)
                t_tmp4 = Trk()
                h2b = [sb(f"h2b{i}", [128, D], BF16, st=st) for i in range(2)]
                h2b_t = [Trk() for _ in range(2)]
                h2T = sb("h2T", [128, 8, 128], BF16, st=st)
                t_h2T = Trk()
                qTs = sb("qTs", [128, 16, 128], BF16, st=st)
                t_qTs = Trk()
                Ssb = sb("Ssb", [128, 16, 128], st=st)
                t_Ssb = Trk()
                scr8 = sb("scr8", [128, 2048], st=st)
                t_scr8 = Trk()
                m8 = sb("m8", [128, 16, 16], st=st)
                i8 = sb("i8", [128, 16, 16], U32, st=st)
                i8f = sb("i8f", [128, 16, 16], st=st)
                t_m8, t_i8, t_i8f = Trk(), Trk(), Trk()
                cand = Ssb[:].rearrange("p (h a) t -> p h (a t)", a=2)
                t_cand = t_Ssb
                b8 = sb("b8", [128, 8, 16], st=st)
                c8 = sb("c8", [128, 8, 16], U32, st=st)
                t_b8, t_c8 = Trk(), Trk()
                chi = sb("chi", [128, 128], U32, st=st)
                clo = sb("clo", [128, 128], U32, st=st)
                chif = sb("chif", [128, 8, 16], st=st)
                clof = sb("clof", [128, 8, 16], st=st)
                iab = sb("iab", [128, 2, 128], st=st)
                eidxf = sb("eidxf", [128, 128], st=st)
                t_sm = Trk()
                eidx = [sb(f"eidx{i}", [128, 128], U32, st=st) for i in range(2)]
                eidx_t = [Trk() for _ in range(2)]
                gz = sb("gz", [128, 8, 16], st=st)
                gsum = sb("gsum", [128, 16], st=st)
                t_gz = Trk()
                gates = [sb(f"gates{i}", [128, 128], st=st) for i in range(2)]
                gates_t = [Trk() for _ in range(2)]
                hu = sb("hu", [128, 128], st=st)
                ag = sb("ag", [128, 128], st=st)
                aa = sb("aa", [128, 128], st=st)
                NB = 4
                hu_t = [Trk() for _ in range(128)]
                ag_t = [Trk() for _ in range(128 // NB)]
                aa_t = [Trk() for _ in range(128 // NB)]
                NR = 20
                G = [sb(f"G{i}", [128, 2 * D], BF16, st=st) for i in range(NR)]
                G_t = [Trk() for _ in range(NR)]
                ld_G = [kb.stream(f"ldG{i}") for i in range(NR)]
                dgv = [sb(f"dgv{i}", [128, 128], BF16, st=st) for i in range(4)]
                vs_t = [Trk() for _ in range(4)]
                x2 = sb("x2", [128, D], st=st)
                t_x2 = Trk()
                outt = [sb(f"outt{i}", [128, D], st=st) for i in range(2)]
                outt_t = [Trk() for _ in range(2)]
                st_o = [kb.stream(f"sto{i}") for i in range(2)]

                def load_x1(tt):
                    i = tt % 3
                    op("sp", lambda e: e.dma_start(out=x1t[i][:], in_=x1_s[tt * 128:(tt + 1) * 128, :]),
                       reads=[x1_t], writes=[x1t_t[i]], dma=ld_x1[i])

                def top16(src_fn, n, mout, iout, tm, ti, groups):
                    gm = [Trk() for _ in range(groups)]
                    s2s = (scr8[:, 0:n], scr8[:, 1024:1024 + n])
                    s2t = (t_scr8, Trk())
                    for g0 in range(0, groups, 2):
                        pair = [g for g in (g0, g0 + 1) if g < groups]
                        for g in pair:
                            op("dve", lambda e, g=g: e.max(out=mout[:, g, 0:8], in_=src_fn(g)), reads=[t_Ssb, t_cand], writes=[gm[g]])
                        for k, g in enumerate(pair):
                            op("dve", lambda e, g=g, k=k: e.match_replace(out=s2s[k], in_to_replace=mout[:, g, 0:8], in_values=src_fn(g), imm_value=NEG),
                               reads=[t_Ssb, t_cand, gm[g]], writes=[s2t[k]])
                        for k, g in enumerate(pair):
                            op("dve", lambda e, g=g, k=k: e.max(out=mout[:, g, 8:16], in_=s2s[k]), reads=[s2t[k], gm[g]], writes=[gm[g]])
                        for g in pair:
                            op("dve", lambda e, g=g: e.max_index(out=iout[:, g, 0:8], in_max=mout[:, g, 0:8], in_values=src_fn(g)),
                               reads=[t_Ssb, t_cand, gm[g]], writes=[ti])
                        for g in pair:
                            op("dve", lambda e, g=g: e.max_index(out=iout[:, g, 8:16], in_max=mout[:, g, 8:16], in_values=src_fn(g)),
                               reads=[t_Ssb, t_cand, gm[g]], writes=[ti])
                        yield
                    op("dve", lambda e: e.tensor_copy(out=mout[:, 0, 0:1], in_=mout[:, 0, 0:1]), reads=gm, writes=[tm])

                def front(tt):
                    i = tt % 2
                    x3 = tt % 3
                    op("act", lambda e: e.activation(out=junkE[:], in_=x1t[x3][:], func=AF.Square, accum_out=ssE[:, 0:1]),
                       reads=[x1t_t[x3]], writes=[t_junkE, t_ssE])
                    op("act", lambda e: e.activation(out=ssE[:, 1:2], in_=ssE[:, 0:1], func=AF.Sqrt, bias=eps_t[:], scale=1.0 / D),
                       reads=[t_ssE, cT], writes=[t_ssE])
                    op("dve", lambda e: e.reciprocal(out=ssE[:, 2:3], in_=ssE[:, 1:2]), reads=[t_ssE], writes=[t_ssE])
                    op("dve", lambda e: e.scalar_tensor_tensor(out=tmp4[:], in0=x1t[x3][:], scalar=ssE[:, 2:3], in1=adae[:, A2],
                                                               op0=ALU.mult, op1=ALU.mult), reads=[x1t_t[x3], t_ssE, adaT], writes=[t_tmp4])
                    yield
                    op("dve", lambda e: e.tensor_tensor(out=h2b[i][:], in0=tmp4[:], in1=adae[:, SH2], op=ALU.add),
                       reads=[t_tmp4, adaT], writes=[h2b_t[i]])
                    pbv = ps[:, 7, :].bitcast(BF16)
                    for kc in range(8):
                        op("pe", lambda e, kc=kc: e.transpose(out=pbv[:, kc * 128:(kc + 1) * 128], in_=h2b[i][:, kc * 128:(kc + 1) * 128],
                                                              identity=ident_b[:]), reads=[h2b_t[i], cT], writes=[psT[7]])
                    op("act", lambda e: e.activation(out=h2T[:], in_=pbv.rearrange("p (k t) -> p k t", k=8), func=AF.Copy),
                       reads=[psT[7]], writes=[t_h2T])
                    yield
                    for g in range(4):
                        pb = g % 2
                        for u in range(4):
                            hh = g * 4 + u
                            for kc in range(8):
                                op("pe", lambda e, kc=kc: e.matmul(ps[:, pb, u * 128:(u + 1) * 128], lhsT=wq[:, kc, hh * 128:(hh + 1) * 128],
                                                                   rhs=h2T[:, kc, :], start=(kc == 0), stop=(kc == 7)),
                                   reads=[wE, t_h2T], writes=[psT[pb]])
                        op("act", lambda e: e.activation(out=qTs[:, g * 4:(g + 1) * 4, :].rearrange("p a t -> p (a t)"),
                                                         in_=ps[:, pb, :], func=AF.Copy), reads=[psT[pb]], writes=[t_qTs])
                        yield
                    for g in range(4):
                        pb = 2 + g % 2
                        for u in range(4):
                            hh = g * 4 + u
                            op("pe", lambda e: e.matmul(ps[:, pb, u * 128:(u + 1) * 128], lhsT=qTs[:, hh, :], rhs=KT[:, hh % 2, :],
                                                        start=True, stop=True), reads=[t_qTs, wE], writes=[psT[pb]])
                        op("act", lambda e: e.activation(out=Ssb[:, g * 4:(g + 1) * 4, :].rearrange("p a t -> p (a t)"),
                                                         in_=ps[:, pb, :], func=AF.Copy), reads=[psT[pb]], writes=[t_Ssb])
                        yield
                    yield from top16(lambda g: Ssb[:, g, :], 128, m8, i8, t_m8, t_i8, 16)
                    v4 = m8[:].rearrange("p (h t) k -> p h t k", t=2)
                    op("dve", lambda e: e.tensor_tensor(out=cand.rearrange("p h (a b) -> p h a b", b=16),
                                                        in0=v4[:, :, 0, :].unsqueeze(3).to_broadcast([128, 8, 16, 16]),
                                                        in1=v4[:, :, 1, :].unsqueeze(2).to_broadcast([128, 8, 16, 16]), op=ALU.add),
                       reads=[t_m8], writes=[t_cand])
                    yield
                    yield from top16(lambda g: cand[:, g, :], 256, b8, c8, t_b8, t_c8, 8)
                    c8f = c8[:].rearrange("p h k -> p (h k)")
                    op("dve", lambda e: e.tensor_scalar(out=chi[:], in0=c8f, scalar1=4, scalar2=None, op0=ALU.logical_shift_right),
                       reads=[t_c8], writes=[t_sm])
                    op("dve", lambda e: e.tensor_scalar(out=clo[:], in0=c8f, scalar1=15, scalar2=None, op0=ALU.bitwise_and),
                       reads=[t_c8], writes=[t_sm])
                    op("dve", lambda e: e.tensor_copy(out=chif[:].rearrange("p h k -> p (h k)"), in_=chi[:]), reads=[t_sm], writes=[t_sm])
                    op("dve", lambda e: e.tensor_copy(out=clof[:].rearrange("p h k -> p (h k)"), in_=clo[:]), reads=[t_sm], writes=[t_sm])
                    op("dve", lambda e: e.tensor_copy(out=i8f[:], in_=i8[:]), reads=[t_i8], writes=[t_i8f])
                    yield
                    oh = scr8[:].rearrange("p (h k i) -> p h k i", h=8, k=16)
                    io4 = iota16[:].rearrange("p (k i) -> p k i", i=16).unsqueeze(1).to_broadcast([128, 8, 16, 16])
                    i4 = i8f[:].rearrange("p (h t) k -> p h t k", t=2)
                    for half, cf in enumerate((chif, clof)):
                        op("dve", lambda e: e.tensor_tensor(out=oh, in0=cf[:].unsqueeze(3).to_broadcast([128, 8, 16, 16]), in1=io4, op=ALU.is_equal),
                           reads=[t_sm, cT], writes=[t_scr8])
                        op("dve", lambda e: e.tensor_tensor(out=oh, in0=oh, in1=i4[:, :, half, :].unsqueeze(2).to_broadcast([128, 8, 16, 16]), op=ALU.mult),
                           reads=[t_scr8, t_i8f], writes=[t_scr8])
                        op("dve", lambda e: e.tensor_reduce(out=iab[:, half, :].rearrange("p (h k) -> p h k", k=16), in_=oh,
                                                            axis=mybir.AxisListType.X, op=ALU.add), reads=[t_scr8], writes=[t_sm])
                        yield
                    op("dve", lambda e: e.scalar_tensor_tensor(out=eidxf[:], in0=iab[:, 0, :], scalar=128.0, in1=iab[:, 1, :],
                                                               op0=ALU.mult, op1=ALU.add), reads=[t_sm], writes=[t_sm])
                    op("dve", lambda e: e.tensor_copy(out=eidx[i][:], in_=eidxf[:]), reads=[t_sm], writes=[eidx_t[i]])
                    op("dve", lambda e: e.tensor_tensor(out=gz[:], in0=b8[:], in1=b8[:, :, 0:1].to_broadcast([128, 8, 16]), op=ALU.subtract),
                       reads=[t_b8], writes=[t_gz])
                    op("act", lambda e: e.activation(out=gz[:], in_=gz[:], func=AF.Exp), reads=[t_gz], writes=[t_gz])
                    op("dve", lambda e: e.tensor_reduce(out=gsum[:, 0:8], in_=gz[:], axis=mybir.AxisListType.X, op=ALU.add),
                       reads=[t_gz], writes=[t_gz])
                    op("dve", lambda e: e.reciprocal(out=gsum[:, 8:16], in_=gsum[:, 0:8]), reads=[t_gz], writes=[t_gz])
                    op("dve", lambda e: e.tensor_tensor(out=gates[i][:].rearrange("p (h k) -> p h k", k=16), in0=gz[:],
                                                        in1=gsum[:, 8:16].unsqueeze(2).to_broadcast([128, 8, 16]), op=ALU.mult),
                       reads=[t_gz], writes=[gates_t[i]])
                    yield

                pendE = {"g": None}

                def pumpE(n=1):
                    g = pendE["g"]
                    if g is None:
                        return
                    for _ in range(n):
                        try:
                            next(g)
                        except StopIteration:
                            pendE["g"] = None
                            return

                cnt = {"g": 0, "vs": 0, "jk": 0}
                ring = {}
                jk = [sb(f"jk{i}", [128, D], BF16, st=st) for i in range(2)]
                jk_t = [Trk() for _ in range(2)]

                def gather(tt, hk):
                    i = tt % 2
                    r = cnt["g"] % NR
                    cnt["g"] += 1
                    ring[(tt, hk)] = r
                    op("pool", lambda e: e.indirect_dma_start(out=G[r][:], out_offset=None, in_=uv_s[:, :],
                                                              in_offset=bass.IndirectOffsetOnAxis(ap=eidx[i][:, hk:hk + 1], axis=0)),
                       reads=[eidx_t[i], uv_t], writes=[G_t[r]], dma=ld_G[r])

                def dot(tt, hk):
                    i = tt % 2
                    r = ring[(tt, hk)]
                    bt = hk // NB
                    q = cnt["jk"] % 2
                    cnt["jk"] += 1
                    op("dve", lambda e: e.scalar_tensor_tensor(out=jk[q][:], in0=G[r][:, 0:D], scalar=1.0, in1=h2b[i][:],
                                                               op0=ALU.mult, op1=ALU.mult, accum_out=hu[:, hk:hk + 1]),
                       reads=[G_t[r], h2b_t[i]], writes=[jk_t[q], hu_t[hk]])

                def gate_batch(tt, bt):
                    i = tt % 2
                    cs = slice(bt * NB, (bt + 1) * NB)
                    op("act", lambda e: e.activation(out=ag[:, cs], in_=hu[:, cs], func=AF.Gelu), reads=hu_t[bt * NB:(bt + 1) * NB], writes=[ag_t[bt]])
                    op("dve", lambda e: e.tensor_tensor(out=aa[:, cs], in0=ag[:, cs], in1=gates[i][:, cs], op=ALU.mult),
                       reads=[ag_t[bt], gates_t[i]], writes=[aa_t[bt]])

                def vacc(tt, hk):
                    r = ring.pop((tt, hk))
                    bt = hk // NB
                    r3 = cnt["vs"] % 4
                    cnt["vs"] += 1
                    op("act", lambda e: e.activation(out=dgv[r3][:], in_=ident_b[:], func=AF.Copy, scale=aa[:, hk:hk + 1]),
                       reads=[cT, aa_t[bt]], writes=[vs_t[r3]])
                    for half in range(2):
                        op("pe", lambda e, half=half: e.matmul(ps[:, 4 + half, :], lhsT=dgv[r3][:], rhs=G[r][:, D + half * 512:D + (half + 1) * 512],
                                                               start=(hk == 0), stop=(hk == 127)),
                           reads=[G_t[r], vs_t[r3]], writes=[psT[4 + half]])

                def final(tt):
                    i = tt % 2
                    x3 = tt % 3
                    for half in range(2):
                        hsl = slice(half * 512, (half + 1) * 512)
                        op("dve", lambda e: e.tensor_tensor(out=tmp4[:, hsl], in0=ps[:, 4 + half, :], in1=adae[:, G2][:, hsl], op=ALU.mult),
                           reads=[psT[4 + half], adaT], writes=[t_tmp4])
                    op("pool", lambda e: e.tensor_tensor(out=x2[:], in0=tmp4[:], in1=x1t[x3][:], op=ALU.add),
                       reads=[t_tmp4, x1t_t[x3]], writes=[t_x2])
                    op("act", lambda e: e.activation(out=junkE[:], in_=x2[:], func=AF.Square, accum_out=ssE[:, 4:5]),
                       reads=[t_x2], writes=[t_junkE, t_ssE])
                    op("act", lambda e: e.activation(out=ssE[:, 5:6], in_=ssE[:, 4:5], func=AF.Sqrt, bias=eps_t[:], scale=1.0 / D),
                       reads=[t_ssE, cT], writes=[t_ssE])
                    op("dve", lambda e: e.reciprocal(out=ssE[:, 6:7], in_=ssE[:, 5:6]), reads=[t_ssE], writes=[t_ssE])
                    op("dve", lambda e: e.scalar_tensor_tensor(out=outt[i][:], in0=x2[:], scalar=ssE[:, 6:7], in1=fgb[:],
                                                               op0=ALU.mult, op1=ALU.mult), reads=[t_x2, t_ssE, wE], writes=[outt_t[i]])
                    op("sp", lambda e: e.dma_start(out=out_d[tt * 128:(tt + 1) * 128, :], in_=outt[i][:]),
                       reads=[outt_t[i]], writes=[out_t], dma=st_o[i])

                ntile = int(os.environ.get("ELIM", NT))
                DLY = 1
                LOOK = NR - NB - DLY - 1
                load_x1(0)
                if ntile > 1:
                    load_x1(1)
                for _ in front(0):
                    pass
                for tt in range(ntile):
                    if tt + 2 < ntile:
                        load_x1(tt + 2)
                    if tt + 1 < ntile:
                        pendE["g"] = front(tt + 1)
                    for hk in range(min(LOOK, 128)):
                        gather(tt, hk)
                    for hk in range(128):
                        if hk + LOOK < 128:
                            gather(tt, hk + LOOK)
                        dot(tt, hk)
                        if hk >= DLY and (hk - DLY) % NB == NB - 1:
                            bt = (hk - DLY) // NB
                            gate_batch(tt, bt)
                            for h2 in range(bt * NB, (bt + 1) * NB):
                                vacc(tt, h2)
                        if hk % 3 == 2:
                            pumpE(1)
                    for bt in range((128 - DLY) // NB, 128 // NB):
                        gate_batch(tt, bt)
                        for h2 in range(bt * NB, (bt + 1) * NB):
                            vacc(tt, h2)
                    pumpE(100000)
                    final(tt)
                kb.barrier()

        fin = []
        if "B" in stages:
            fin.append(scr_t)
        fin.append(yag_t)
        fin.append(x1_t)
        fin.append(out_t)
        kb.wait_all("sp", fin)
        print("instructions:", kb.ninst)
    return nc


def _in_maps(inputs, ncores=8):
    consts = make_consts()
    maps = []
    for b in range(ncores):
        m = dict(consts)
        m["x"] = np.ascontiguousarray(inputs["x"][b])
        m["c"] = np.ascontiguousarray(inputs["c"][b:b + 1])
        for k in ("w_ada", "b_ada", "norm1_g", "w_in", "gla_w_gate_up", "gla_b_gate", "gla_norm_g",
                  "dsa_kv_norm_g", "dsa_w_uk", "dsa_w_uv", "w_branch_a", "w_branch_b", "w_out", "norm2_g",
                  "peer_w_q", "peer_sub_keys_1", "peer_sub_keys_2", "peer_u", "peer_v"):
            m[k] = np.ascontiguousarray(inputs[k][0]) if inputs[k].ndim > 1 and inputs[k].shape[0] == 1 else inputs[k]
        for k in ("b_ada", "norm1_g", "gla_b_gate", "gla_norm_g", "dsa_kv_norm_g", "norm2_g"):
            m[k] = np.ascontiguousarray(inputs[k]).reshape(1, -1)
        m["final_norm_g"] = np.ascontiguousarray(inputs["final_norm_g"]).reshape(1, -1)
        maps.append(m)
    return maps


def kernel(**inputs):
    inputs = {k: np.asarray(v) for k, v in inputs.items()}
    nc = build()
    maps = _in_maps(inputs)
    res = run_bass_kernel_spmd(nc, maps, core_ids=list(range(8)))
    return np.stack([r["out"] for r in res.results], axis=0).astype(np.float32)
```

```python
import os
import numpy as np
from contextlib import ExitStack
import concourse.bass as bass
import concourse.mybir as mybir
from concourse.bass_utils import run_bass_kernel_spmd

F32 = mybir.dt.float32
BF16 = mybir.dt.bfloat16
U32 = mybir.dt.uint32
AF = mybir.ActivationFunctionType
ALU = mybir.AluOpType

D = 1024
S = 4096
NT = S // 128
IN_TOTAL = 7000
OFF = dict(gq=0, gk=512, gv=1024, gr=2048, glow=3072, dq=3088, dkv=4112, iq=4368, ik=4880,
           iw=4944, za=4952, zb=5976)
NEG = -1.0e30


class Trk:
    __slots__ = ("w", "r", "multi")

    def __init__(self, multi=False):
        self.w = {}
        self.r = {}
        self.multi = multi


class Stream:
    def __init__(self, sem, sid):
        self.sem = sem
        self.n = 0
        self.sid = sid
        self.maxwait = 0


class Eng:
    def __init__(self, name, e, sem, sid):
        self.name = name
        self.e = e
        self.sem = sem
        self.sid = sid
        self.n = 0
        self.seen = {}


class KB:
    def __init__(self, nc, es):
        self.nc = nc
        self.es = es
        self.nsid = 0
        self.E = {}
        for name, e in (("pe", nc.tensor), ("act", nc.scalar), ("dve", nc.vector),
                        ("pool", nc.gpsimd), ("sp", nc.sync)):
            sem = es.enter_context(nc.semaphore("sem_" + name))
            self.E[name] = Eng(name, e, sem, self.nsid)
            self.nsid += 1
        self.ninst = 0
        self.streams = []
        self.sid2stream = {}

    def stream(self, name):
        sem = self.es.enter_context(self.nc.semaphore("st_" + name))
        s = Stream(sem, self.nsid)
        self.nsid += 1
        self.streams.append(s)
        self.sid2stream[s.sid] = s
        return s

    def op(self, en, fn, reads=(), writes=(), dma=None):
        e = self.E[en]
        need = {}

        def add(evd, skip_own):
            for k, (sem, val) in evd.items():
                if skip_own and k == e.sid:
                    continue
                if need.get(k, (None, 0))[1] < val:
                    need[k] = (sem, val)

        own = (en == "pe" and dma is None)
        for t in reads:
            add(t.w, own)
        for t in writes:
            if not t.multi:
                add(t.w, own)
            add(t.r, own)
        for k, (sem, val) in need.items():
            stt = self.sid2stream.get(k)
            if stt is not None:
                val = stt.n
                stt.maxwait = max(stt.maxwait, val)
            if e.seen.get(k, 0) >= val:
                continue
            e.e.wait_ge(sem, val)
            e.seen[k] = val
            self.ninst += 1
        if dma is not None and dma.maxwait > e.seen.get(dma.sid, 0):
            e.e.wait_ge(dma.sem, dma.maxwait)
            e.seen[dma.sid] = dma.maxwait
            self.ninst += 1
        inst = fn(e.e)
        self.ninst += 1
        if dma is None:
            e.n += 1
            inst.then_inc(e.sem, 1)
            ev = (e.sem, e.n)
            key = e.sid
        else:
            dma.n += 16
            inst.then_inc(dma.sem, 16)
            ev = (dma.sem, dma.n)
            key = dma.sid
        for t in reads:
            if t.r.get(key, (None, 0))[1] < ev[1]:
                t.r[key] = ev
        for t in writes:
            if t.multi:
                if t.w.get(key, (None, 0))[1] < ev[1]:
                    t.w[key] = ev
            else:
                t.w = {key: ev}
                t.r = {}
        return ev

    def barrier(self):
        for e in self.E.values():
            for o in self.E.values():
                if o is e or o.n == 0 or e.seen.get(o.sid, 0) >= o.n:
                    continue
                e.e.wait_ge(o.sem, o.n)
                e.seen[o.sid] = o.n
                self.ninst += 1
            for stt in self.streams:
                if stt.n == 0 or e.seen.get(stt.sid, 0) >= stt.n:
                    continue
                e.e.wait_ge(stt.sem, stt.n)
                e.seen[stt.sid] = stt.n
                stt.maxwait = stt.n
                self.ninst += 1

    def wait_all(self, en, trks):
        e = self.E[en]
        for t in trks:
            for k, (sem, val) in t.w.items():
                stt = self.sid2stream.get(k)
                if stt is not None:
                    val = stt.n
                    stt.maxwait = max(stt.maxwait, val)
                if e.seen.get(k, 0) >= val:
                    continue
                e.e.wait_ge(sem, val)
                e.seen[k] = val


def make_consts():
    p = np.arange(128)
    ident = np.eye(128, dtype=np.float32)
    lt = (p[:, None] <= p[None, :]).astype(np.float32)
    ut = (p[:, None] > p[None, :]).astype(np.float32)
    diag = np.where(p[None, :] <= p[:, None], 0.0, NEG).astype(np.float32)
    slopes = np.exp2(-8.0 * np.arange(1, 9, dtype=np.float32) / 8).astype(np.float32)
    dl = np.arange(32)
    bias = slopes[None, :, None] * (p[:, None, None] - 127.0 + 128.0 * (dl[None, None, :] - 31.0))
    bias = bias.astype(np.float32).reshape(128, 256)
    iota16 = np.tile(np.arange(16, dtype=np.float32)[None, None, :], (128, 16, 1)).reshape(128, 256)
    pos1 = np.tile(np.arange(1, 129, dtype=np.float32)[None, :], (128, 1))
    slopes8 = np.tile(slopes[None, :], (128, 1)).astype(np.float32)
    esel = np.zeros((8, 8, 128), np.float32)
    for h in range(8):
        esel[h, h, :] = 1.0
    posr = np.ones((10, 32, 128), np.float32)
    posr[8, :, :] = (p - 127.0)[None, :]
    posr[9, :, :] = (128.0 * (dl - 31.0))[:, None]
    slopeR = np.zeros((10, 8, 128), np.float32)
    slopeR[8:10] = slopes[None, :, None]
    basej = np.tile((128.0 * np.arange(NT, dtype=np.float32))[None, :], (128, 1))
    return dict(c_posr=posr.reshape(10, 4096), c_slopeR=slopeR.reshape(10, 1024), c_basej=basej,
                c_ident=ident, c_lt=lt, c_ut=ut, c_diag=diag, c_bias=bias, c_iota16=iota16,
                c_pos1=pos1, c_slopes8=slopes8, c_esel=esel.reshape(8, 1024))


def build(stages=("A", "B", "C", "D", "E"), debug=(), scr_in=()):
    nc = bass.Bass("TRN2", target_bir_lowering=False)

    def din(name, shape, dt=F32):
        return nc.dram_tensor(name, list(shape), dt, kind="ExternalInput").ap()

    def dscr(name, shape, dt=BF16):
        kind = "ExternalOutput" if name in debug else ("ExternalInput" if name in scr_in else "Internal")
        return nc.dram_tensor(name, list(shape), dt, kind=kind).ap()

    x_d = din("x", [S, D])
    c_d = din("c", [1, D])
    wada_d = din("w_ada", [D, 6 * D])
    bada_d = din("b_ada", [1, 6 * D])
    g1_d = din("norm1_g", [1, D])
    win_d = din("w_in", [D, IN_TOTAL])
    wgu_d = din("gla_w_gate_up", [16, 512])
    bgate_d = din("gla_b_gate", [1, 512])
    gng_d = din("gla_norm_g", [1, D])
    kvg_d = din("dsa_kv_norm_g", [1, 256])
    wuk_d = din("dsa_w_uk", [8, 128, 256])
    wuv_d = din("dsa_w_uv", [8, 256, 128])
    wba_d = din("w_branch_a", [D, D])
    wbb_d = din("w_branch_b", [D, D])
    wout_d = din("w_out", [D, D])
    g2_d = din("norm2_g", [1, D])
    wq_d = din("peer_w_q", [D, 2048])
    sk1_d = din("peer_sub_keys_1", [128, 128])
    sk2_d = din("peer_sub_keys_2", [128, 128])
    pu_d = din("peer_u", [16384, D])
    pv_d = din("peer_v", [16384, D])
    fg_d = din("final_norm_g", [1, D])
    cst = {k: din(k, v.shape) for k, v in make_consts().items()}

    out_d = nc.dram_tensor("out", [S, D], F32, kind="ExternalOutput").ap()

    ada_s = dscr("ada_s", [1, 6 * D], F32)
    qT_s = dscr("qT_s", [512, S])
    kT_s = dscr("kT_s", [512, S])
    glT_s = dscr("glT_s", [16, S])
    dqT_s = dscr("dqT_s", [1024, S])
    iqT_s = dscr("iqT_s", [512, S])
    ikT_s = dscr("ikT_s", [64, S])
    k_s = dscr("k_s", [S, 512])
    v_s = dscr("v_s", [S, 1024])
    sr_s = dscr("sr_s", [S, 1024])
    dkv_s = dscr("dkv_s", [S, 256])
    iw_s = dscr("iw_s", [S, 8], F32)
    sza_s = dscr("sza_s", [S, 1024])
    szb_s = dscr("szb_s", [S, 1024])
    yag_s = dscr("yag_s", [S, 1024])
    x1_s = dscr("x1_s", [S, D], F32)
    uv_s = dscr("uv_s", [16384, 2 * D])

    with ExitStack() as es:
        kb = KB(nc, es)
        op = kb.op
        es.enter_context(nc.allow_non_contiguous_dma(reason="small strided setup loads"))
        es.enter_context(nc.allow_low_precision(reason="bf16 matmul operands, fp32 accumulation"))

        def sb(name, shape, dt=F32, st=None):
            return (st or es).enter_context(nc.sbuf_tensor(name, list(shape), dt))

        ps = es.enter_context(nc.psum_tensor("ps", [128, 8, 512], F32))
        psT = [Trk() for _ in range(8)]

        ld_c = kb.stream("ldc")
        cT = Trk(multi=True)
        ident_f = sb("ident_f", [128, 128])
        lt_f = sb("lt_f", [128, 128])
        ut_f = sb("ut_f", [128, 128])
        diag_f = sb("diag_f", [128, 128])
        bias_t = sb("bias_t", [128, 256])
        iota16 = sb("iota16", [128, 256])
        for t, nm in ((ident_f, "c_ident"), (lt_f, "c_lt"), (ut_f, "c_ut"), (diag_f, "c_diag"),
                      (bias_t, "c_bias"), (iota16, "c_iota16")):
            op("sp", lambda e, t=t, nm=nm: e.dma_start(out=t[:], in_=cst[nm][:, :]), writes=[cT], dma=ld_c)
        ident_b = sb("ident_b", [128, 128], BF16)
        lt_b = sb("lt_b", [128, 128], BF16)
        eps_t = sb("eps_t", [128, 1])
        one_t = sb("one_t", [128, 1])
        ones_row = sb("ones_row", [1, 128], BF16)
        op("dve", lambda e: e.tensor_copy(out=ident_b[:], in_=ident_f[:]), reads=[cT], writes=[cT])
        op("dve", lambda e: e.tensor_copy(out=lt_b[:], in_=lt_f[:]), reads=[cT], writes=[cT])
        op("dve", lambda e: e.memset(eps_t[:], 1e-6), writes=[cT])
        op("dve", lambda e: e.memset(one_t[:], 1.0), writes=[cT])
        op("dve", lambda e: e.memset(ones_row[:], 1.0), writes=[cT])

        adaT = Trk()
        t_adas = Trk()
        ld_ada = kb.stream("ldada")
        SH1, A1, G1, SH2, A2, G2 = [slice(i * D, (i + 1) * D) for i in range(6)]

        def load_ada(st, name, sls):
            t = sb(name, [128, len(sls) * D], st=st)
            outs = []
            for n, sl in enumerate(sls):
                op("sp", lambda e, n=n, sl=sl: e.dma_start(out=t[:, n * D:(n + 1) * D],
                                                         in_=ada_s[0:1, sl].to_broadcast([128, D])),
                   reads=[t_adas], writes=[adaT], dma=ld_ada)
                outs.append(slice(n * D, (n + 1) * D))
            return t, outs

        if "A" in stages:
            with ExitStack() as st:
                cTt = sb("cTt", [128, 8], st=st)
                scT = sb("scT", [128, 8], st=st)
                wada = [sb(f"wada{i}", [128, 8, 512], st=st) for i in range(2)]
                wadaT = [Trk() for _ in range(2)]
                ld_w = [kb.stream(f"ldwada{i}") for i in range(2)]
                arow = sb("arow", [1, 6 * D], st=st)
                brow = sb("brow", [1, 6 * D], st=st)
                grow = sb("grow", [1, 2 * D], st=st)
                t_c, t_sc, t_arow, t_brow = Trk(), Trk(), Trk(), Trk()
                ld_a = kb.stream("lda")
                op("sp", lambda e: e.dma_start(out=cTt[:], in_=c_d.rearrange("o (kc p) -> p (o kc)", p=128)),
                   writes=[t_c], dma=ld_a)
                op("sp", lambda e: e.dma_start(out=brow[:], in_=bada_d[:, :]), writes=[t_brow], dma=ld_a)
                op("sp", lambda e: e.dma_start(out=grow[0:1, 0:D], in_=g1_d[:, :]), writes=[t_brow], dma=ld_a)
                op("sp", lambda e: e.dma_start(out=grow[0:1, D:2 * D], in_=g2_d[:, :]), writes=[t_brow], dma=ld_a)
                op("act", lambda e: e.activation(out=scT[:], in_=cTt[:], func=AF.Silu), reads=[t_c], writes=[t_sc])
                wv = wada_d.rearrange("(kc p) n -> p kc n", p=128)

                def load_wada(cg):
                    i = cg % 2
                    op("sp", lambda e: e.dma_start(out=wada[i][:], in_=wv[:, :, cg * 512:(cg + 1) * 512]),
                       writes=[wadaT[i]], dma=ld_w[i])
                load_wada(0)
                for cg in range(12):
                    if cg + 1 < 12:
                        load_wada(cg + 1)
                    i = cg % 2
                    b = cg % 2
                    for kc in range(8):
                        op("pe", lambda e, kc=kc: e.matmul(ps[0:1, b, :], lhsT=scT[:, kc:kc + 1], rhs=wada[i][:, kc, :],
                                                           start=(kc == 0), stop=(kc == 7)),
                           reads=[t_sc, wadaT[i]], writes=[psT[b]])
                    op("dve", lambda e: e.tensor_tensor(out=arow[0:1, cg * 512:(cg + 1) * 512], in0=ps[0:1, b, :],
                                                        in1=brow[0:1, cg * 512:(cg + 1) * 512], op=ALU.add),
                       reads=[psT[b], t_brow], writes=[t_arow])
                for (sl, gsl) in ((A1, slice(0, D)), (A2, slice(D, 2 * D))):
                    op("dve", lambda e, sl=sl, gsl=gsl: e.scalar_tensor_tensor(
                        out=arow[0:1, sl], in0=arow[0:1, sl], scalar=1.0, in1=grow[0:1, gsl],
                        op0=ALU.add, op1=ALU.mult), reads=[t_arow, t_brow], writes=[t_arow])
                op("sp", lambda e: e.dma_start(out=ada_s[:, :], in_=arow[:]), reads=[t_arow], writes=[t_adas], dma=ld_a)

        stBC = ExitStack()
        uv_t = Trk(multi=True)
        CVR = 2
        cv32 = [sb(f"cv32_{i}", [128, CVR, D], st=stBC) for i in range(2)]
        cv16 = [sb(f"cv16_{i}", [128, CVR, D], BF16, st=stBC) for i in range(2)]
        cv32_t = [Trk() for _ in range(2)]
        cv16_t = [Trk() for _ in range(2)]
        ld_cv = [kb.stream(f"ldcv{i}") for i in range(2)]
        st_cv = [kb.stream(f"stcv{i}") for i in range(2)]
        uv_v = uv_s.rearrange("(p a) (t d) -> p a t d", a=128, t=2)
        cvs = {"n": 0, "pending": None}
        NCV = 2 * (128 // CVR)

        def conv_store():
            pnd = cvs["pending"]
            if pnd is not None:
                k, t, c = pnd
                op("sp", lambda e: e.dma_start(out=uv_v[:, c * CVR:(c + 1) * CVR, t, :], in_=cv16[k][:]),
                   reads=[cv16_t[k]], writes=[uv_t], dma=st_cv[k])
                cvs["pending"] = None

        def conv_step():
            n = cvs["n"]
            if n >= NCV:
                conv_store()
                return
            cvs["n"] += 1
            k = n % 2
            t, c = n % 2, n // 2
            tab = pu_d if t == 0 else pv_d
            src_v = tab.rearrange("(p a) d -> p a d", a=128)[:, c * CVR:(c + 1) * CVR, :]
            op("sp", lambda e: e.dma_start(out=cv32[k][:], in_=src_v), writes=[cv32_t[k]], dma=ld_cv[k])
            conv_store()
            op("pool", lambda e: e.tensor_copy(out=cv16[k][:], in_=cv32[k][:]), reads=[cv32_t[k]], writes=[cv16_t[k]])
            cvs["pending"] = (k, t, c)

        kb.barrier()
        if "B" in stages:
            with ExitStack() as st:
                adab, (A1, SH1) = load_ada(st, "adaB", [A1, SH1])
                hT = sb("hT", [128, 8, S], BF16, st=st)
                hT_t = [Trk() for _ in range(NT)]
                xt = [sb(f"xt{i}", [128, D], st=st) for i in range(2)]
                xt_t = [Trk() for _ in range(2)]
                ld_x = [kb.stream(f"ldx{i}") for i in range(2)]
                junk = sb("junk", [128, D], BF16, st=st)
                t_junk = Trk()
                tmp = sb("tmpB", [128, D], st=st)
                t_tmp = Trk()
                hb = sb("hb", [128, D], BF16, st=st)
                t_hb = Trk()
                ss = sb("ssB", [128, 4], st=st)
                t_ss = Trk()
                x_v = x_d.rearrange("(t p) d -> t p d", p=128)

                def load_x(tt):
                    i = tt % 2
                    op("sp", lambda e: e.dma_start(out=xt[i][:], in_=x_v[tt]), writes=[xt_t[i]], dma=ld_x[i])
                load_x(0)
                for tt in range(NT):
                    if tt + 1 < NT:
                        load_x(tt + 1)
                    i = tt % 2
                    op("act", lambda e: e.activation(out=junk[:], in_=xt[i][:], func=AF.Square, accum_out=ss[:, 0:1]),
                       reads=[xt_t[i]], writes=[t_junk, t_ss])
                    op("act", lambda e: e.activation(out=ss[:, 1:2], in_=ss[:, 0:1], func=AF.Sqrt, bias=eps_t[:],
                                                     scale=1.0 / D), reads=[t_ss, cT], writes=[t_ss])
                    op("dve", lambda e: e.reciprocal(out=ss[:, 2:3], in_=ss[:, 1:2]), reads=[t_ss], writes=[t_ss])
                    op("dve", lambda e: e.scalar_tensor_tensor(out=tmp[:], in0=xt[i][:], scalar=ss[:, 2:3],
                                                               in1=adab[:, A1], op0=ALU.mult, op1=ALU.mult),
                       reads=[xt_t[i], t_ss, adaT], writes=[t_tmp])
                    op("dve", lambda e: e.tensor_tensor(out=hb[:], in0=tmp[:], in1=adab[:, SH1], op=ALU.add),
                       reads=[t_tmp, adaT], writes=[t_hb])
                    pb = 6 + (tt % 2)
                    pbv = ps[:, pb, :].bitcast(BF16)
                    for kc in range(8):
                        op("pe", lambda e, kc=kc: e.transpose(out=pbv[:, kc * 128:(kc + 1) * 128],
                                                              in_=hb[:, kc * 128:(kc + 1) * 128], identity=ident_b[:]),
                           reads=[t_hb, cT], writes=[psT[pb]])
                    op("act", lambda e: e.activation(out=hT[:, :, tt * 128:(tt + 1) * 128],
                                                     in_=pbv.rearrange("p (k t) -> p k t", k=8), func=AF.Copy),
                       reads=[psT[pb]], writes=[hT_t[tt]])

                wt = [sb(f"wt{i}", [128, 8, 512], BF16, st=st) for i in range(2)]
                wt_t = [Trk() for _ in range(2)]
                ld_wt = [kb.stream(f"ldwt{i}") for i in range(2)]
                stf = [sb(f"stf{i}", [128, S], BF16, st=st) for i in range(2)]
                stf_t = [Trk(multi=True) for _ in range(2)]
                st_f = [kb.stream(f"stf{i}") for i in range(2)]
                stt_ = [sb(f"stt{i}", [128, 4, 512], BF16, st=st) for i in range(2)]
                stt_t = [Trk(multi=True) for _ in range(2)]
                st_t = [kb.stream(f"stt{i}") for i in range(2)]
                sti = sb("sti", [128, NT, 8], st=st)
                sti_t = Trk()
                win_v = win_d.rearrange("(kc p) n -> p kc n", p=128)
                scr_t = Trk(multi=True)
                self_scr = scr_t

                blocks = []
                for nm, dst, n in (("gq", qT_s, 512), ("gk", kT_s, 512), ("glow", glT_s, 16), ("dq", dqT_s, 1024),
                                   ("iq", iqT_s, 512), ("ik", ikT_s, 64)):
                    for c0 in range(0, n, 128):
                        w = min(128, n - c0)
                        blocks.append(("F", OFF[nm] + c0, w, dst, c0, None))
                for nm, dst, n, fn in (("gk", k_s, 512, AF.Copy), ("gv", v_s, 1024, AF.Copy), ("gr", sr_s, 1024, AF.Silu),
                                       ("dkv", dkv_s, 256, AF.Copy), ("iw", iw_s, 8, AF.Copy),
                                       ("za", sza_s, 1024, AF.Sigmoid), ("zb", szb_s, 1024, AF.Sigmoid)):
                    for c0 in range(0, n, 512):
                        w = min(512, n - c0)
                        blocks.append(("T", OFF[nm] + c0, w, dst, c0, fn))

                wt32 = [sb(f"wt32_{i}", [128, 8, 512], F32, st=st) for i in range(2)]
                wt32_t = [Trk() for _ in range(2)]

                def load_w(bi):
                    kind, col, w, dst, c0, fn = blocks[bi]
                    i = bi % 2
                    op("sp", lambda e: e.dma_start(out=wt32[i][:, :, 0:w], in_=win_v[:, :, col:col + w]),
                       writes=[wt32_t[i]], dma=ld_wt[i])
                    op("pool", lambda e: e.tensor_copy(out=wt[i][:, :, 0:w], in_=wt32[i][:, :, 0:w]),
                       reads=[wt32_t[i]], writes=[wt_t[i]])
                load_w(0)
                nf = 0
                ntb = 0
                evac = 0
                for bi, (kind, col, w, dst, c0, fn) in enumerate(blocks):
                    if bi + 1 < len(blocks):
                        load_w(bi + 1)
                    if "E" in stages:
                        conv_step()
                        conv_step()
                    i = bi % 2
                    if kind == "F":
                        si = nf % 2
                        nf += 1
                        for tg in range(8):
                            pb = tg % 4
                            for kc in range(8):
                                op("pe", lambda e, kc=kc: e.matmul(ps[0:w, pb, :], lhsT=wt[i][:, kc, 0:w],
                                                                   rhs=hT[:, kc, tg * 512:(tg + 1) * 512],
                                                                   start=(kc == 0), stop=(kc == 7)),
                                   reads=[wt_t[i]] + hT_t[tg * 4:(tg + 1) * 4], writes=[psT[pb]])
                            en = "act" if evac % 2 == 0 else "dve"
                            evac += 1
                            if en == "act":
                                op("act", lambda e: e.activation(out=stf[si][0:w, tg * 512:(tg + 1) * 512],
                                                                 in_=ps[0:w, pb, :], func=AF.Copy),
                                   reads=[psT[pb]], writes=[stf_t[si]])
                            else:
                                op("dve", lambda e: e.tensor_copy(out=stf[si][0:w, tg * 512:(tg + 1) * 512],
                                                                  in_=ps[0:w, pb, :]),
                                   reads=[psT[pb]], writes=[stf_t[si]])
                        op("sp", lambda e: e.dma_start(out=dst[c0:c0 + w, :], in_=stf[si][0:w, :]),
                           reads=[stf_t[si]], writes=[scr_t], dma=st_f[si])
                    else:
                        is_iw = (dst is iw_s)
                        for tt in range(NT):
                            pb = tt % 4
                            for kc in range(8):
                                op("pe", lambda e, kc=kc: e.matmul(ps[:, pb, 0:w], lhsT=hT[:, kc, tt * 128:(tt + 1) * 128],
                                                                   rhs=wt[i][:, kc, 0:w],
                                                                   start=(kc == 0), stop=(kc == 7)),
                                   reads=[wt_t[i], hT_t[tt]], writes=[psT[pb]])
                            if is_iw:
                                op("dve", lambda e: e.tensor_copy(out=sti[:, tt, :], in_=ps[:, pb, 0:w]),
                                   reads=[psT[pb]], writes=[sti_t])
                                continue
                            si = ntb % 2
                            a = tt % 4
                            if fn == AF.Copy and evac % 2 == 1:
                                op("dve", lambda e: e.tensor_copy(out=stt_[si][:, a, 0:w], in_=ps[:, pb, 0:w]),
                                   reads=[psT[pb]], writes=[stt_t[si]])
                            else:
                                op("act", lambda e: e.activation(out=stt_[si][:, a, 0:w], in_=ps[:, pb, 0:w], func=fn),
                                   reads=[psT[pb]], writes=[stt_t[si]])
                            evac += 1
                            if a == 3:
                                r0 = (tt - 3) * 128
                                op("sp", lambda e: e.dma_start(
                                    out=dst[r0:r0 + 512, c0:c0 + w].rearrange("(a p) n -> p a n", p=128),
                                    in_=stt_[si][:, :, 0:w]), reads=[stt_t[si]], writes=[scr_t], dma=st_t[si])
                                ntb += 1
                        if is_iw:
                            op("sp", lambda e: e.dma_start(out=iw_s.rearrange("(t p) n -> p t n", p=128), in_=sti[:]),
                               reads=[sti_t], writes=[scr_t], dma=st_t[0])
        else:
            scr_t = Trk(multi=True)

        kb.barrier()
        yag_t = Trk(multi=True)
        if "C" in stages:
            with ExitStack() as st:
                stg = [sb(f"stgC{i}", [128, 8, 512], F32, st=st) for i in range(2)]
                stg_t = [Trk() for _ in range(2)]
                ld_stg = [kb.stream(f"ldstgC{i}") for i in range(2)]
                wgu = sb("wgu", [16, 512], BF16, st=st)
                bgr = sb("bgr", [1, 512], BF16, st=st)
                gngb = sb("gngb", [128, D], st=st)
                wba = sb("wba", [128, 8, D], BF16, st=st)
                wC = Trk(multi=True)
                op("sp", lambda e: e.dma_start(out=stg[0][0:16, 0, :], in_=wgu_d[:, :]), writes=[stg_t[0]], dma=ld_stg[0])
                op("pool", lambda e: e.tensor_copy(out=wgu[:], in_=stg[0][0:16, 0, :]), reads=[stg_t[0]], writes=[wC])
                op("sp", lambda e: e.dma_start(out=stg[1][0:1, 0, :], in_=bgate_d[:, :]), writes=[stg_t[1]], dma=ld_stg[1])
                op("pool", lambda e: e.tensor_copy(out=bgr[:], in_=stg[1][0:1, 0, :]), reads=[stg_t[1]], writes=[wC])
                op("sp", lambda e: e.dma_start(out=gngb[:], in_=gng_d[0:1, :].to_broadcast([128, D])), writes=[wC], dma=ld_stg[0])
                wba_v = wba_d.rearrange("(kc p) n -> p kc n", p=128)
                for half in range(2):
                    op("sp", lambda e, half=half: e.dma_start(out=stg[half][:], in_=wba_v[:, :, half * 512:(half + 1) * 512]),
                       writes=[stg_t[half]], dma=ld_stg[half])
                    op("pool", lambda e, half=half: e.tensor_copy(out=wba[:, :, half * 512:(half + 1) * 512], in_=stg[half][:]),
                       reads=[stg_t[half]], writes=[wC])

                qTc = [sb(f"qTc{i}", [128, 4, 128], BF16, st=st) for i in range(2)]
                kTc = [sb(f"kTc{i}", [128, 4, 128], BF16, st=st) for i in range(2)]
                ktok = [sb(f"ktok{i}", [128, 512], BF16, st=st) for i in range(2)]
                vc = [sb(f"vc{i}", [128, D], BF16, st=st) for i in range(2)]
                glc = [sb(f"glc{i}", [16, 128], BF16, st=st) for i in range(2)]
                src = [sb(f"src{i}", [128, D], BF16, st=st) for i in range(2)]
                szac = [sb(f"szac{i}", [128, D], BF16, st=st) for i in range(2)]
                in_t = [Trk(multi=True) for _ in range(2)]
                ld_C = [kb.stream(f"ldC{i}") for i in range(2)]
                qT_v = qT_s.rearrange("(h d) t -> d h t", d=128)
                kT_v = kT_s.rearrange("(h d) t -> d h t", d=128)

                def load_c(c):
                    i = c % 2
                    cs = slice(c * 128, (c + 1) * 128)
                    for dst, srcap in ((qTc[i][:], qT_v[:, :, cs]), (kTc[i][:], kT_v[:, :, cs]), (ktok[i][:], k_s[cs, :]),
                                       (vc[i][:], v_s[cs, :]), (glc[i][:], glT_s[:, cs]), (src[i][:], sr_s[cs, :]),
                                       (szac[i][:], sza_s[cs, :])):
                        op("sp", lambda e, dst=dst, srcap=srcap: e.dma_start(out=dst, in_=srcap),
                           reads=[scr_t], writes=[in_t[i]], dma=ld_C[i])

                sp_t = sb("sp_t", [128, 512], st=st)
                eke = sb("eke", [128, 512], st=st)
                kend = sb("kend", [128, 512], BF16, st=st)
                eq4 = sb("eq4", [128, 4, 128], st=st)
                ek4 = sb("ek4", [128, 4, 128], st=st)
                qd4 = sb("qd4", [128, 4, 128], BF16, st=st)
                ki4 = sb("ki4", [128, 4, 128], BF16, st=st)
                at4 = sb("at4", [128, 4, 128], BF16, st=st)
                S_f = sb("S_f", [128, 4, 256], st=st)
                S_b = sb("S_b", [128, 4, 256], BF16, st=st)
                ss4 = sb("ss4", [128, 12], st=st)
                junkC = sb("junkC", [128, 256], BF16, st=st)
                tmpC = sb("tmpC", [128, D], st=st)
                ga = sb("ga", [128, D], BF16, st=st)
                gaT = sb("gaT", [128, 8, 128], BF16, st=st)
                yag = [sb(f"yag{i}", [128, D], BF16, st=st) for i in range(2)]
                yag_bt = [Trk() for _ in range(2)]
                st_y = [kb.stream(f"sty{i}") for i in range(2)]
                (t_sp, t_eke, t_kend, t_eq, t_ek, t_qd, t_ki, t_at, t_ss4, t_junk, t_tmp, t_ga, t_gaT) = [Trk() for _ in range(13)]
                t_S = [Trk() for _ in range(4)]
                t_Sb = [Trk() for _ in range(4)]
                t_psC = t_psD = psT[2]
                op("dve", lambda e: e.memset(S_f[:], 0.0), writes=t_S)
                op("dve", lambda e: e.memset(S_b[:], 0.0), writes=t_Sb)

                load_c(0)
                for c in range(NT):
                    i = c % 2
                    if c + 1 < NT:
                        load_c(c + 1)
                    conv_step()
                    conv_step()
                    it = [in_t[i]]
                    op("pe", lambda e: e.matmul(ps[:, 0, :], lhsT=glc[i][0:16, :], rhs=wgu[0:16, :], start=True, stop=False),
                       reads=it + [wC], writes=[psT[0]])
                    op("pe", lambda e: e.matmul(ps[:, 0, :], lhsT=ones_row[0:1, :], rhs=bgr[0:1, :], start=False, stop=True),
                       reads=[cT, wC], writes=[psT[0]])
                    op("act", lambda e: e.activation(out=sp_t[:], in_=ps[:, 0, :], func=AF.Exp, scale=-1.0),
                       reads=[psT[0]], writes=[t_sp])
                    op("act", lambda e: e.activation(out=sp_t[:], in_=sp_t[:], func=AF.Ln, bias=one_t[:]),
                       reads=[t_sp, cT], writes=[t_sp])
                    op("pe", lambda e: e.matmul(ps[:, 1, :], lhsT=ut_f[:], rhs=sp_t[:], start=True, stop=True),
                       reads=[cT, t_sp], writes=[psT[1]])
                    op("act", lambda e: e.activation(out=eke[:], in_=ps[:, 1, :], func=AF.Exp, scale=-1.0 / 16),
                       reads=[psT[1]], writes=[t_eke])
                    op("dve", lambda e: e.tensor_tensor(out=kend[:], in0=ktok[i][:], in1=eke[:], op=ALU.mult),
                       reads=it + [t_eke], writes=[t_kend])
                    for h in range(4):
                        hs = slice(h * 128, (h + 1) * 128)
                        op("pe", lambda e, h=h, hs=hs: e.matmul(ps[:, 2, h * 128:(h + 1) * 128], lhsT=sp_t[:, hs], rhs=lt_f[:], start=True, stop=True),
                           reads=[t_sp, cT], writes=[psT[2]])
                    op("act", lambda e: e.activation(out=eq4[:].rearrange("p h t -> p (h t)"), in_=ps[:, 2, :], func=AF.Exp, scale=-1.0 / 16),
                       reads=[psT[2]], writes=[t_eq])
                    op("act", lambda e: e.activation(out=ek4[:].rearrange("p h t -> p (h t)"), in_=ps[:, 2, :], func=AF.Exp, scale=1.0 / 16),
                       reads=[psT[2]], writes=[t_ek])
                    op("dve", lambda e: e.scalar_tensor_tensor(out=qd4[:].rearrange("p h t -> p (h t)"),
                                                               in0=qTc[i][:].rearrange("p h t -> p (h t)"), scalar=128.0 ** -0.5,
                                                               in1=eq4[:].rearrange("p h t -> p (h t)"), op0=ALU.mult, op1=ALU.mult),
                       reads=it + [t_eq], writes=[t_qd])
                    op("dve", lambda e: e.tensor_tensor(out=ki4[:].rearrange("p h t -> p (h t)"), in0=kTc[i][:].rearrange("p h t -> p (h t)"),
                                                        in1=ek4[:].rearrange("p h t -> p (h t)"), op=ALU.mult),
                       reads=it + [t_ek], writes=[t_ki])
                    for h in range(4):
                        op("pe", lambda e, h=h: e.matmul(ps[:, 3, h * 128:(h + 1) * 128], lhsT=ki4[:, h, :], rhs=qd4[:, h, :], start=True, stop=True),
                           reads=[t_ki, t_qd], writes=[psT[3]])
                    op("dve", lambda e: e.tensor_tensor(out=at4[:], in0=ps[:, 3, :].rearrange("p (h t) -> p h t", h=4),
                                                        in1=lt_f[:].unsqueeze(1).to_broadcast([128, 4, 128]), op=ALU.mult),
                       reads=[psT[3], cT], writes=[t_at])
                    for h in range(4):
                        vs = slice(h * 256, (h + 1) * 256)
                        pso = ps[:, 4 + h // 2, (h % 2) * 256:(h % 2) * 256 + 256]
                        op("pe", lambda e, h=h, vs=vs, pso=pso: e.matmul(pso, lhsT=at4[:, h, :], rhs=vc[i][:, vs], start=True, stop=False),
                           reads=it + [t_at], writes=[psT[4 + h // 2]])
                        op("pe", lambda e, h=h, pso=pso: e.matmul(pso, lhsT=qd4[:, h, :], rhs=S_b[:, h, :], start=False, stop=True),
                           reads=[t_qd, t_Sb[0]], writes=[psT[4 + h // 2]])
                    for h in range(4):
                        hs = slice(h * 128, (h + 1) * 128)
                        vs = slice(h * 256, (h + 1) * 256)
                        pk = ps[:, 6 + h // 2, (h % 2) * 256:(h % 2) * 256 + 256]
                        op("pe", lambda e, hs=hs, vs=vs, pk=pk: e.matmul(pk, lhsT=kend[:, hs], rhs=vc[i][:, vs], start=True, stop=True),
                           reads=it + [t_kend], writes=[psT[6 + h // 2]])
                    for h in range(4):
                        pk = ps[:, 6 + h // 2, (h % 2) * 256:(h % 2) * 256 + 256]
                        op("dve", lambda e, h=h, pk=pk: e.scalar_tensor_tensor(out=S_f[:, h, :], in0=S_f[:, h, :], scalar=eq4[:, h, 127:128],
                                                                               in1=pk, op0=ALU.mult, op1=ALU.add),
                           reads=[t_S[0], t_eq, psT[6 + h // 2]], writes=[t_S[0]])
                    op("act", lambda e: e.activation(out=S_b[:].rearrange("p h v -> p (h v)"), in_=S_f[:].rearrange("p h v -> p (h v)"), func=AF.Copy),
                       reads=[t_S[0]], writes=[t_Sb[0]])
                    for h in range(4):
                        pso = ps[:, 4 + h // 2, (h % 2) * 256:(h % 2) * 256 + 256]
                        op("act", lambda e: e.activation(out=junkC[:], in_=pso, func=AF.Square, accum_out=ss4[:, h:h + 1]),
                           reads=[psT[4 + h // 2]], writes=[t_junk, t_ss4])
                    op("act", lambda e: e.activation(out=ss4[:, 4:8], in_=ss4[:, 0:4], func=AF.Sqrt, bias=eps_t[:], scale=1.0 / 256),
                       reads=[t_ss4, cT], writes=[t_ss4])
                    op("dve", lambda e: e.reciprocal(out=ss4[:, 8:12], in_=ss4[:, 4:8]), reads=[t_ss4], writes=[t_ss4])
                    for h in range(4):
                        vs = slice(h * 256, (h + 1) * 256)
                        pso = ps[:, 4 + h // 2, (h % 2) * 256:(h % 2) * 256 + 256]
                        op("dve", lambda e: e.scalar_tensor_tensor(out=tmpC[:, vs], in0=pso, scalar=ss4[:, 8 + h:9 + h],
                                                                   in1=gngb[:, vs], op0=ALU.mult, op1=ALU.mult),
                           reads=[psT[4 + h // 2], t_ss4, wC], writes=[t_tmp])
                    op("pool", lambda e: e.tensor_tensor(out=ga[:], in0=tmpC[:], in1=src[i][:], op=ALU.mult),
                       reads=it + [t_tmp], writes=[t_ga])
                    pbv = ps[:, 2, :].bitcast(BF16)
                    for kc in range(8):
                        op("pe", lambda e, kc=kc: e.transpose(out=pbv[:, kc * 128:(kc + 1) * 128],
                                                              in_=ga[:, kc * 128:(kc + 1) * 128], identity=ident_b[:]),
                           reads=[t_ga, cT], writes=[psT[2]])
                    op("act", lambda e: e.activation(out=gaT[:], in_=pbv.rearrange("p (k t) -> p k t", k=8), func=AF.Copy),
                       reads=[psT[2]], writes=[t_gaT])
                    for half in range(2):
                        yb_ = (3, 1)[half]
                        for kc in range(8):
                            op("pe", lambda e, kc=kc, yb_=yb_: e.matmul(ps[:, yb_, :], lhsT=gaT[:, kc, :],
                                                                        rhs=wba[:, kc, half * 512:(half + 1) * 512],
                                                                        start=(kc == 0), stop=(kc == 7)),
                               reads=[t_gaT, wC], writes=[psT[yb_]])
                        op("dve", lambda e, yb_=yb_: e.tensor_tensor(out=yag[i][:, half * 512:(half + 1) * 512], in0=ps[:, yb_, :],
                                                                     in1=szac[i][:, half * 512:(half + 1) * 512], op=ALU.mult),
                           reads=it + [psT[yb_]], writes=[yag_bt[i]])
                    op("sp", lambda e: e.dma_start(out=yag_s[c * 128:(c + 1) * 128, :], in_=yag[i][:]),
                       reads=[yag_bt[i]], writes=[yag_t], dma=st_y[i])

        if "E" in stages and "C" in stages:
            while cvs["n"] < NCV or cvs["pending"] is not None:
                conv_step()
        kb.barrier()
        stBC.close()
        x1_t = Trk(multi=True)
        if "D" in stages:
            with ExitStack() as st:
                adab, (G1,) = load_ada(st, "adaD", [G1])
                wuk = sb("wuk", [128, 8, 256], BF16, st=st)
                wuv = sb("wuv", [128, 8, 2, 128], BF16, st=st)
                wbb = sb("wbb", [128, 8, D], BF16, st=st)
                wout = sb("wout", [128, 8, D], BF16, st=st)
                kvgb = sb("kvgb", [128, 256], st=st)
                wD = Trk(multi=True)
                with ExitStack() as st2:
                    stg = [sb(f"stgD{i}", [128, 8, 512], F32, st=st2) for i in range(2)]
                    stg_t = [Trk() for _ in range(2)]
                    ld_stg = [kb.stream(f"ldstgD{i}") for i in range(2)]
                    op("sp", lambda e: e.dma_start(out=stg[0][:, :, 0:256], in_=wuk_d.rearrange("h d c -> d h c")),
                       writes=[stg_t[0]], dma=ld_stg[0])
                    op("pool", lambda e: e.tensor_copy(out=wuk[:], in_=stg[0][:, :, 0:256]), reads=[stg_t[0]], writes=[wD])
                    s1v = stg[1][:, 0:4, :].rearrange("p a (b d) -> p (a b) d", d=128)
                    op("sp", lambda e: e.dma_start(out=s1v, in_=wuv_d.rearrange("h (cc p) d -> p (h cc) d", p=128)),
                       writes=[stg_t[1]], dma=ld_stg[1])
                    op("pool", lambda e: e.tensor_copy(out=wuv[:].rearrange("p h c d -> p (h c) d"), in_=s1v),
                       reads=[stg_t[1]], writes=[wD])
                    op("sp", lambda e: e.dma_start(out=kvgb[:], in_=kvg_d[0:1, :].to_broadcast([128, 256])), writes=[wD], dma=ld_stg[0])
                    k = 0
                    for wdst, wsrc in ((wbb, wbb_d), (wout, wout_d)):
                        wv_ = wsrc.rearrange("(kc p) n -> p kc n", p=128)
                        for half in range(2):
                            i = k % 2
                            k += 1
                            op("sp", lambda e, i=i, half=half, wv_=wv_: e.dma_start(out=stg[i][:], in_=wv_[:, :, half * 512:(half + 1) * 512]),
                               writes=[stg_t[i]], dma=ld_stg[i])
                            op("pool", lambda e, i=i, half=half, wdst=wdst: e.tensor_copy(out=wdst[:, :, half * 512:(half + 1) * 512], in_=stg[i][:]),
                               reads=[stg_t[i]], writes=[wD])
                    kb.barrier()

                ckv = sb("ckv", [128, NT, 257], BF16, st=st)
                ckvT = sb("ckvT", [128, 2, S], BF16, st=st)
                ikT = sb("ikT", [64, S], BF16, st=st)
                iw_all = sb("iw_all", [128, NT, 8], st=st)
                posl = sb("posl", [128, 128], st=st)
                basej = sb("basej", [128, NT], st=st)
                slopes8 = sb("slopes8", [128, 8], st=st)
                esel = sb("esel", [8, 8, 128], BF16, st=st)
                posr = sb("posr", [10, 32, 128], BF16, st=st)
                slopeR = sb("slopeR", [10, 8, 128], BF16, st=st)
                ones8 = sb("ones8", [8, 128], BF16, st=st)
                ones_col = sb("ones_col", [128, 1], BF16, st=st)
                cD = Trk(multi=True)
                ld_cD = kb.stream("ldcD")
                st3 = ExitStack()
                esel_f = sb("esel_f", [8, 1024], st=st3)
                posr_f = sb("posr_f", [10, 32 * 128], st=st3)
                slopeR_f = sb("slopeR_f", [10, 1024], st=st3)
                op("sp", lambda e: e.dma_start(out=posl[:], in_=cst["c_pos1"][:, 0:128]), writes=[cD], dma=ld_cD)
                op("sp", lambda e: e.dma_start(out=basej[:], in_=cst["c_basej"][:, :]), writes=[cD], dma=ld_cD)
                op("sp", lambda e: e.dma_start(out=slopes8[:], in_=cst["c_slopes8"][:, :]), writes=[cD], dma=ld_cD)
                op("sp", lambda e: e.dma_start(out=esel_f[:], in_=cst["c_esel"][:, :]), writes=[cD], dma=ld_cD)
                op("sp", lambda e: e.dma_start(out=posr_f[:], in_=cst["c_posr"][:, :]), writes=[cD], dma=ld_cD)
                op("sp", lambda e: e.dma_start(out=slopeR_f[:], in_=cst["c_slopeR"][:, :]), writes=[cD], dma=ld_cD)
                op("dve", lambda e: e.tensor_copy(out=esel[:].rearrange("k h s -> k (h s)"), in_=esel_f[:]), reads=[cD], writes=[cD])
                op("dve", lambda e: e.tensor_copy(out=posr[:].rearrange("k a s -> k (a s)"), in_=posr_f[:]), reads=[cD], writes=[cD])
                op("dve", lambda e: e.tensor_copy(out=slopeR[:].rearrange("k h q -> k (h q)"), in_=slopeR_f[:]), reads=[cD], writes=[cD])
                op("dve", lambda e: e.memset(ones8[:], 1.0), writes=[cD])
                op("dve", lambda e: e.memset(ones_col[:], 1.0), writes=[cD])
                dkv_all = sb("dkv_all", [128, NT, 256], BF16, st=st3)
                t_dkv, t_ik, t_iw = Trk(), Trk(), Trk()
                ckv_t = [Trk() for _ in range(NT)]
                ckvT_t = [Trk() for _ in range(NT)]
                ld_d0 = kb.stream("ldd0")
                op("sp", lambda e: e.dma_start(out=dkv_all[:], in_=dkv_s.rearrange("(t p) c -> p t c", p=128)),
                   reads=[scr_t], writes=[t_dkv], dma=ld_d0)
                op("sp", lambda e: e.dma_start(out=ikT[:], in_=ikT_s[:, :]), reads=[scr_t], writes=[t_ik], dma=ld_d0)
                op("sp", lambda e: e.dma_start(out=iw_all[:], in_=iw_s.rearrange("(t p) n -> p t n", p=128)),
                   reads=[scr_t], writes=[t_iw], dma=ld_d0)
                ssd = sb("ssd", [128, 3 * NT], st=st3)
                t_ssd = Trk()
                junk0 = sb("junk0", [128, 256], BF16, st=st3)
                t_junk0 = Trk()
                for tt in range(NT):
                    op("act", lambda e: e.activation(out=junk0[:], in_=dkv_all[:, tt, :], func=AF.Square,
                                                     accum_out=ssd[:, tt:tt + 1]), reads=[t_dkv], writes=[t_junk0, t_ssd])
                op("act", lambda e: e.activation(out=ssd[:, NT:2 * NT], in_=ssd[:, 0:NT], func=AF.Sqrt, bias=eps_t[:], scale=1.0 / 256),
                   reads=[t_ssd, cT], writes=[t_ssd])
                op("dve", lambda e: e.reciprocal(out=ssd[:, 2 * NT:3 * NT], in_=ssd[:, NT:2 * NT]), reads=[t_ssd], writes=[t_ssd])
                op("dve", lambda e: e.memset(ckv[:, :, 256:257], 1.0), writes=ckv_t)
                for tt in range(NT):
                    op("dve", lambda e: e.scalar_tensor_tensor(out=ckv[:, tt, 0:256], in0=dkv_all[:, tt, :],
                                                               scalar=ssd[:, 2 * NT + tt:2 * NT + tt + 1], in1=kvgb[:],
                                                               op0=ALU.mult, op1=ALU.mult),
                       reads=[t_dkv, t_ssd, wD], writes=[ckv_t[tt]])
                    pb = 4 + (tt % 2)
                    pbv = ps[:, pb, :].bitcast(BF16)
                    for cc in range(2):
                        op("pe", lambda e, cc=cc: e.transpose(out=pbv[:, cc * 128:(cc + 1) * 128],
                                                              in_=ckv[:, tt, cc * 128:(cc + 1) * 128], identity=ident_b[:]),
                           reads=[ckv_t[tt], cT], writes=[psT[pb]])
                    op("act", lambda e: e.activation(out=ckvT[:, :, tt * 128:(tt + 1) * 128],
                                                     in_=pbv[:, 0:256].rearrange("p (c t) -> p c t", c=2), func=AF.Copy),
                       reads=[psT[pb]], writes=[ckvT_t[tt]])

                kb.barrier()
                st3.close()
                iqc = [sb(f"iqc{i}", [64, 8, 128], BF16, st=st) for i in range(2)]
                dqc = [sb(f"dqc{i}", [128, 8, 128], BF16, st=st) for i in range(2)]
                szbc = [sb(f"szbc{i}", [128, D], BF16, st=st) for i in range(2)]
                yagc = [sb(f"yagc{i}", [128, D], BF16, st=st) for i in range(2)]
                xq1 = sb("xq", [128, D], st=st)
                xq = [xq1, xq1]
                xq_t = Trk()
                ld_xq = kb.stream("ldxq")
                inI_t = [Trk(multi=True) for _ in range(2)]
                inA_t = [Trk(multi=True) for _ in range(2)]
                ld_I = [kb.stream(f"ldI{i}") for i in range(2)]
                ld_A = [kb.stream(f"ldA{i}") for i in range(2)]
                iqT_v = iqT_s.rearrange("(h d) t -> d h t", d=64)
                dqT_v = dqT_s.rearrange("(h d) t -> d h t", d=128)
                acc = sb("accD", [128, S], st=st)
                t_acc = Trk()
                relb = [sb(f"relb{i}", [128, 512], BF16, st=st) for i in range(2)]
                relb_t = [Trk() for _ in range(2)]
                Dg = sb("Dg", [128, 8, 128], BF16, st=st)
                t_Dg = Trk()
                bs = sb("bs", [128, 8], st=st)
                t_bs = Trk()
                sel = sb("sel", [128, S], BF16, st=st)
                t_sel = Trk()
                selT = [sb(f"selT{i}", [128, NT, 128], BF16, st=st) for i in range(2)]
                selT_t = [Trk() for _ in range(2)]
                qlat = sb("qlat", [128, 8, 2, 128], BF16, st=st)
                t_qlat = Trk()
                pT = [sb(f"pT{i}", [128, 4, 128], BF16, st=st) for i in range(3)]
                pT_t = [Trk() for _ in range(3)]
                oT = sb("oT", [128, 8, 128], BF16, st=st)
                t_oT = Trk()
                tmpD = sb("tmpD", [128, D], st=st)
                t_tmpD = Trk()
                ymix = sb("ymix", [128, D], BF16, st=st)
                t_ymix = Trk()
                ymixT = sb("ymixT", [128, 8, 128], BF16, st=st)
                t_ymixT = Trk()
                x1b1 = sb("x1b", [128, D], st=st)
                x1b = [x1b1, x1b1]
                x1b_t1 = Trk()
                x1b_t = [x1b_t1, x1b_t1]
                st_x11 = kb.stream("stx1")
                st_x1 = [st_x11, st_x11]
                nm_bias = sb("nm_bias", [128, 1], st=st)
                op("dve", lambda e: e.memset(nm_bias[:], -30000.0), writes=[cT])
                neg29 = sb("neg29", [128, 1], st=st)
                op("dve", lambda e: e.memset(neg29[:], -1.0e29), writes=[cT])
                corrD = [sb(f"corrD{i}", [10, 8, 128], BF16, st=st) for i in range(2)]
                corrD_t = [Trk() for _ in range(2)]
                for i_ in range(2):
                    op("dve", lambda e, i_=i_: e.tensor_copy(out=corrD[i_][:], in_=slopeR[:]), reads=[cD], writes=[corrD_t[i_]])
                rsrow = sb("rsrow", [1, 512], st=st)
                rsb = sb("rsb", [1, 512], BF16, st=st)
                t_rs = Trk()
                olT = sb("olT", [128, 2, 512], BF16, st=st)
                t_olT = Trk()
                rsB = sb("rsB", [128, 512], st=st)
                t_rsB = Trk()
                corr8 = sb("corr8", [128, 10], st=st)
                cm = sb("cm", [128, 2 * NT], st=st)
                t_corr8 = Trk()
                lg_t = [Trk() for _ in range(8)]
                NBIS = 13
                fvec = sb("fvec", [128, NBIS], st=st)
                wf = sb("wf", [128, NBIS], st=st)
                for k_ in range(NBIS):
                    op("dve", lambda e, k_=k_: e.memset(fvec[:, k_:k_ + 1], 2.0 ** -(k_ + 1)), writes=[cT])

                def load_I(qt):
                    i = qt % 2
                    qs = slice(qt * 128, (qt + 1) * 128)
                    op("sp", lambda e: e.dma_start(out=iqc[i][:], in_=iqT_v[:, :, qs]), reads=[scr_t], writes=[inI_t[i]], dma=ld_I[i])

                def load_A(qt):
                    i = qt % 2
                    qs = slice(qt * 128, (qt + 1) * 128)
                    op("sp", lambda e: e.dma_start(out=dqc[i][:], in_=dqT_v[:, :, qs]), reads=[scr_t], writes=[inA_t[i]], dma=ld_A[i])
                    op("sp", lambda e: e.dma_start(out=szbc[i][:], in_=szb_s[qs, :]), reads=[scr_t], writes=[inA_t[i]], dma=ld_A[i])
                    op("sp", lambda e: e.dma_start(out=yagc[i][:], in_=yag_s[qs, :]), reads=[yag_t], writes=[inA_t[i]], dma=ld_A[i])

                def idx_phase(qt):
                    i = qt % 2
                    Sk = (qt + 1) * 128
                    nkb = (Sk + 511) // 512
                    op("dve", lambda e: e.tensor_tensor(out=Dg[:], in0=ident_b[:].unsqueeze(1).to_broadcast([128, 8, 128]),
                                                        in1=iw_all[:, qt, :].unsqueeze(2).to_broadcast([128, 8, 128]), op=ALU.mult),
                       reads=[cT, t_iw], writes=[t_Dg])
                    nrel = 0
                    for kbi in range(nkb):
                        w = min(512, Sk - kbi * 512)
                        ks = slice(kbi * 512, kbi * 512 + w)
                        prev = None
                        for h in range(8):
                            ri = nrel % 2
                            rb = (0, 2)[nrel % 2]
                            nrel += 1
                            op("pe", lambda e: e.matmul(ps[:, rb, 0:w], lhsT=iqc[i][0:64, h, :], rhs=ikT[0:64, ks], start=True, stop=True),
                               reads=[inI_t[i], t_ik], writes=[psT[rb]])
                            if prev is not None:
                                ph, pri = prev
                                op("pe", lambda e: e.matmul(ps[:, 1, 0:w], lhsT=Dg[:, ph, :], rhs=relb[pri][:, 0:w], start=(ph == 0), stop=False),
                                   reads=[t_Dg, relb_t[pri]], writes=[psT[1]])
                                yield
                            op("act", lambda e: e.activation(out=relb[ri][:, 0:w], in_=ps[:, rb, 0:w], func=AF.Relu),
                               reads=[psT[rb]], writes=[relb_t[ri]])
                            prev = (h, ri)
                        ph, pri = prev
                        op("pe", lambda e: e.matmul(ps[:, 1, 0:w], lhsT=Dg[:, ph, :], rhs=relb[pri][:, 0:w], start=False, stop=True),
                           reads=[t_Dg, relb_t[pri]], writes=[psT[1]])
                        yield
                        if kbi == nkb - 1:
                            if w > 128:
                                op("dve", lambda e: e.tensor_copy(out=acc[:, kbi * 512:kbi * 512 + w - 128], in_=ps[:, 1, 0:w - 128]),
                                   reads=[psT[1]], writes=[t_acc])
                            op("dve", lambda e: e.tensor_tensor(out=acc[:, Sk - 128:Sk], in0=ps[:, 1, w - 128:w], in1=diag_f[:], op=ALU.add),
                               reads=[psT[1], cT], writes=[t_acc])
                        else:
                            op("dve", lambda e: e.tensor_copy(out=acc[:, ks], in_=ps[:, 1, 0:w]), reads=[psT[1]], writes=[t_acc])
                    if qt >= 2:
                        op("dve", lambda e: e.tensor_reduce(out=bs[:, 0:1], in_=acc[:, 0:Sk - 128], axis=mybir.AxisListType.X, op=ALU.min),
                           reads=[t_acc], writes=[t_bs])
                        op("dve", lambda e: e.tensor_reduce(out=bs[:, 5:6], in_=acc[:, 0:Sk], axis=mybir.AxisListType.X, op=ALU.max),
                           reads=[t_acc], writes=[t_bs])
                        op("dve", lambda e: e.tensor_tensor(out=bs[:, 1:2], in0=bs[:, 5:6], in1=bs[:, 0:1], op=ALU.subtract),
                           reads=[t_bs], writes=[t_bs])
                        op("dve", lambda e: e.tensor_scalar(out=wf[:], in0=fvec[:], scalar1=bs[:, 1:2], scalar2=None, op0=ALU.mult),
                           reads=[t_bs, cT], writes=[t_bs])
                        op("dve", lambda e: e.tensor_tensor(out=bs[:, 2:3], in0=bs[:, 0:1], in1=wf[:, 0:1], op=ALU.add),
                           reads=[t_bs], writes=[t_bs])
                        for it in range(NBIS):
                            op("dve", lambda e: e.tensor_scalar(out=sel[:, 0:Sk], in0=acc[:, 0:Sk], scalar1=bs[:, 2:3], scalar2=None,
                                                                op0=ALU.is_ge, op1=ALU.add, accum_out=bs[:, 3:4]),
                               reads=[t_acc, t_bs], writes=[t_sel, t_bs])
                            op("dve", lambda e, it=it: e.scalar_tensor_tensor(out=bs[:, 4:5], in0=bs[:, 3:4], scalar=255.5, in1=wf[:, it:it + 1],
                                                                              op0=ALU.is_ge, op1=ALU.mult), reads=[t_bs], writes=[t_bs])
                            if it < NBIS - 1:
                                op("dve", lambda e, it=it: e.scalar_tensor_tensor(out=bs[:, 2:3], in0=bs[:, 4:5], scalar=wf[:, it + 1:it + 2], in1=bs[:, 2:3],
                                                                                  op0=ALU.subtract, op1=ALU.add), reads=[t_bs], writes=[t_bs])
                            else:
                                op("dve", lambda e, it=it: e.scalar_tensor_tensor(out=bs[:, 0:1], in0=bs[:, 4:5], scalar=wf[:, it:it + 1], in1=bs[:, 2:3],
                                                                                  op0=ALU.subtract, op1=ALU.add), reads=[t_bs], writes=[t_bs])
                            yield "BIS"
                        thr = bs[:, 0:1]
                    else:
                        thr = neg29[:]
                    op("dve", lambda e: e.tensor_scalar(out=sel[:, 0:Sk], in0=acc[:, 0:Sk], scalar1=thr, scalar2=None, op0=ALU.is_ge),
                       reads=[t_acc, t_bs, cT], writes=[t_sel])
                    nch = qt + 1
                    op("dve", lambda e: e.tensor_tensor(out=acc[:, 0:Sk].rearrange("p (j s) -> p j s", s=128),
                                                        in0=sel[:, 0:Sk].rearrange("p (j s) -> p j s", s=128),
                                                        in1=posl[:].unsqueeze(1).to_broadcast([128, nch, 128]), op=ALU.mult),
                       reads=[t_sel, cD], writes=[t_acc])
                    op("dve", lambda e: e.tensor_reduce(out=cm[:, 0:nch], in_=acc[:, 0:Sk].rearrange("p (j s) -> p j s", s=128),
                                                        axis=mybir.AxisListType.X, op=ALU.max), reads=[t_acc], writes=[t_corr8])
                    op("dve", lambda e: e.tensor_scalar(out=cm[:, NT:NT + nch], in0=cm[:, 0:nch], scalar1=0.5, scalar2=None, op0=ALU.is_ge),
                       reads=[t_corr8], writes=[t_corr8])
                    op("dve", lambda e: e.tensor_tensor(out=cm[:, NT:NT + nch], in0=cm[:, NT:NT + nch], in1=basej[:, 0:nch], op=ALU.mult),
                       reads=[t_corr8, cD], writes=[t_corr8])
                    op("dve", lambda e: e.tensor_tensor(out=cm[:, 0:nch], in0=cm[:, 0:nch], in1=cm[:, NT:NT + nch], op=ALU.add),
                       reads=[t_corr8], writes=[t_corr8])
                    op("dve", lambda e: e.tensor_reduce(out=corr8[:, 8:9], in_=cm[:, 0:nch], axis=mybir.AxisListType.X, op=ALU.max),
                       reads=[t_corr8], writes=[t_corr8])
                    op("dve", lambda e: e.tensor_scalar(out=corr8[:, 9:10], in0=corr8[:, 8:9], scalar1=-1.0, scalar2=float(Sk),
                                                        op0=ALU.mult, op1=ALU.add), reads=[t_corr8], writes=[t_corr8])
                    op("dve", lambda e: e.tensor_scalar(out=corr8[:, 0:8], in0=slopes8[:], scalar1=corr8[:, 9:10], scalar2=None, op0=ALU.mult),
                       reads=[t_corr8, cD], writes=[t_corr8])
                    yield "HOLD"
                    op("pe", lambda e: e.transpose(out=ps[0:8, 2, 0:128], in_=corr8[:, 0:8], identity=ident_f[:]),
                       reads=[t_corr8, cT], writes=[psT[2]])
                    op("dve", lambda e: e.tensor_tensor(out=corrD[i][0:8, :, :], in0=esel[:], in1=ps[0:8, 2, 0:128].unsqueeze(1).to_broadcast([8, 8, 128]),
                                                        op=ALU.mult), reads=[psT[2], cD], writes=[corrD_t[i]])
                    yield
                    for j0 in range(0, qt + 1, 8):
                        nj = min(8, qt + 1 - j0)
                        pbv = ps[:, 2, :].bitcast(BF16)
                        for jj in range(nj):
                            j = j0 + jj
                            op("pe", lambda e: e.transpose(out=pbv[:, jj * 128:(jj + 1) * 128], in_=sel[:, j * 128:(j + 1) * 128], identity=ident_b[:]),
                               reads=[t_sel, cT], writes=[psT[2]])
                        op("act", lambda e: e.activation(out=selT[i][:, j0:j0 + nj, :],
                                                         in_=pbv[:, 0:nj * 128].rearrange("p (j t) -> p j t", t=128), func=AF.Identity,
                                                         scale=30000.0, bias=nm_bias[:]),
                           reads=[psT[2], cT], writes=[selT_t[i]])
                        yield

                pend = {"g": None}

                def pump(n=1, release=False):
                    g = pend["g"]
                    if g is None:
                        return
                    if pend.get("held") and not release:
                        return
                    pend["held"] = False
                    for _ in range(n):
                        try:
                            r = next(g)
                            if r == "HOLD" and not release:
                                pend["held"] = True
                                return
                            if r == "BIS" and not release:
                                pend["bis"] = pend.get("bis", 0) + 1
                                if pend["bis"] >= pend.get("bis_per_pump", 1):
                                    pend["bis"] = 0
                                    return
                        except StopIteration:
                            pend["g"] = None
                            return

                def att_phase(qt):
                    i = qt % 2
                    qs = slice(qt * 128, (qt + 1) * 128)
                    ia = [inA_t[i]]
                    for g in range(4):
                        for u in range(4):
                            hc = g * 4 + u
                            h, cc = hc // 2, hc % 2
                            op("pe", lambda e: e.matmul(ps[:, 2, u * 128:(u + 1) * 128], lhsT=wuk[:, h, cc * 128:(cc + 1) * 128],
                                                        rhs=dqc[i][:, h, :], start=True, stop=True),
                               reads=ia + [wD], writes=[psT[2]])
                        op("act", lambda e: e.activation(out=qlat[:, g * 2:g * 2 + 2, :, :].rearrange("p h c q -> p (h c q)"),
                                                         in_=ps[:, 2, :], func=AF.Copy, scale=128.0 ** -0.5),
                           reads=[psT[2]], writes=[t_qlat])
                        pump(6)
                    def emit_lg(g, j, k):
                        hs4 = slice(4 * g, 4 * g + 4)
                        lb = 3 + (k % 2)
                        lgb = ps[:, lb, :]
                        dl = j - qt + 31
                        for cc in range(2):
                            op("pe", lambda e, cc=cc: e.matmul(lgb, lhsT=ckvT[:, cc, j * 128:(j + 1) * 128], rhs=qlat[:, hs4, cc, :],
                                                               start=(cc == 0), stop=False),
                               reads=[ckvT_t[j], t_qlat], writes=[psT[lb]])
                        op("pe", lambda e: e.matmul(lgb, lhsT=posr[0:10, dl, :], rhs=corrD[i][0:10, hs4, :], start=False, stop=False),
                           reads=[cD, corrD_t[i]], writes=[psT[lb]])
                        op("pe", lambda e: e.matmul(lgb, lhsT=ident_b[:], rhs=selT[i][:, j, :].unsqueeze(1).to_broadcast([128, 4, 128]),
                                                    start=False, stop=True),
                           reads=[cT, selT_t[i]], writes=[psT[lb]])

                    def emit_exp_pv(g, j, k):
                        lb = 3 + (k % 2)
                        pi = k % 3
                        pTf = pT[pi][:].rearrange("p h q -> p (h q)")
                        op("act", lambda e: e.activation(out=pTf, in_=ps[:, lb, :], func=AF.Exp), reads=[psT[lb]], writes=[pT_t[pi]])
                        for cc in range(2):
                            op("pe", lambda e, cc=cc: e.matmul(ps[:, 5 + cc, :], lhsT=ckv[:, j, cc * 128:(cc + 1) * 128], rhs=pTf,
                                                               start=(j == 0), stop=(j == qt)),
                               reads=[pT_t[pi], ckv_t[j]], writes=[psT[5 + cc]])
                        op("pe", lambda e: e.matmul(ps[0:1, 7, :], lhsT=ones_col[:, 0:1], rhs=pTf, start=(j == 0), stop=(j == qt)),
                           reads=[pT_t[pi], cD], writes=[psT[7]])

                    kstep = 0
                    for g in range(2):
                        hs4 = slice(4 * g, 4 * g + 4)
                        emit_lg(g, 0, kstep)
                        for j in range(qt + 1):
                            if j + 1 <= qt:
                                emit_lg(g, j + 1, kstep + 1)
                            lbk = 3 + (kstep % 2)
                            emit_exp_pv(g, j, kstep)
                            kstep += 1
                            pump(6)
                        op("act", lambda e: e.activation(out=rsrow[0:1, :], in_=ps[0:1, 7, :], func=AF.Ln), reads=[psT[7]], writes=[t_rs])
                        op("act", lambda e: e.activation(out=rsb[0:1, :], in_=rsrow[0:1, :], func=AF.Exp, scale=-1.0), reads=[t_rs], writes=[t_rs])
                        op("act", lambda e: e.activation(out=olT[:, 0, :], in_=ps[:, 5, :], func=AF.Copy), reads=[psT[5]], writes=[t_olT])
                        op("act", lambda e: e.activation(out=olT[:, 1, :], in_=ps[:, 6, :], func=AF.Copy), reads=[psT[6]], writes=[t_olT])
                        op("pe", lambda e: e.matmul(ps[:, 2, :], lhsT=ones_row[0:1, :], rhs=rsb[0:1, :], start=True, stop=True),
                           reads=[cT, t_rs], writes=[psT[2]])
                        op("act", lambda e: e.activation(out=rsB[:], in_=ps[:, 2, :], func=AF.Copy), reads=[psT[2]], writes=[t_rsB])
                        for u in range(4):
                            h = 4 * g + u
                            for cc in range(2):
                                op("pe", lambda e, cc=cc: e.matmul(ps[:, 2, u * 128:(u + 1) * 128], lhsT=wuv[:, h, cc, :],
                                                                   rhs=olT[:, cc, u * 128:(u + 1) * 128], start=(cc == 0), stop=(cc == 1)),
                                   reads=[wD, t_olT], writes=[psT[2]])
                        op("dve", lambda e: e.tensor_tensor(out=oT[:, hs4, :].rearrange("p h q -> p (h q)"), in0=ps[:, 2, :], in1=rsB[:], op=ALU.mult),
                           reads=[psT[2], t_rsB], writes=[t_oT])
                        pump(6)
                    for half in range(2):
                        hsl = slice(half * 512, (half + 1) * 512)
                        for h in range(8):
                            op("pe", lambda e, h=h: e.matmul(ps[:, 2, :], lhsT=oT[:, h, :], rhs=wbb[:, h, hsl], start=(h == 0), stop=(h == 7)),
                               reads=[t_oT, wD], writes=[psT[2]])
                        op("dve", lambda e: e.tensor_tensor(out=tmpD[:, hsl], in0=ps[:, 2, :], in1=szbc[i][:, hsl], op=ALU.mult),
                           reads=ia + [psT[2]], writes=[t_tmpD])
                        pump(6)
                    op("pool", lambda e: e.tensor_tensor(out=ymix[:], in0=tmpD[:], in1=yagc[i][:], op=ALU.add),
                       reads=ia + [t_tmpD], writes=[t_ymix])
                    pbv = ps[:, 2, :].bitcast(BF16)
                    for kc in range(8):
                        op("pe", lambda e, kc=kc: e.transpose(out=pbv[:, kc * 128:(kc + 1) * 128], in_=ymix[:, kc * 128:(kc + 1) * 128],
                                                              identity=ident_b[:]), reads=[t_ymix, cT], writes=[psT[2]])
                    op("act", lambda e: e.activation(out=ymixT[:], in_=pbv.rearrange("p (k t) -> p k t", k=8), func=AF.Copy),
                       reads=[psT[2]], writes=[t_ymixT])
                    pump(6)
                    for half in range(2):
                        hsl = slice(half * 512, (half + 1) * 512)
                        for kc in range(8):
                            op("pe", lambda e, kc=kc: e.matmul(ps[:, 2, :], lhsT=ymixT[:, kc, :], rhs=wout[:, kc, hsl],
                                                               start=(kc == 0), stop=(kc == 7)),
                               reads=[t_ymixT, wD], writes=[psT[2]])
                        op("dve", lambda e: e.tensor_tensor(out=tmpD[:, hsl], in0=ps[:, 2, :], in1=adab[:, G1][:, hsl], op=ALU.mult),
                           reads=[psT[2], adaT], writes=[t_tmpD])
                        pump(6)
                    op("sp", lambda e: e.dma_start(out=xq1[:], in_=x_d[qs, :]), writes=[xq_t], dma=ld_xq)
                    op("pool", lambda e: e.tensor_tensor(out=x1b[i][:], in0=tmpD[:], in1=xq1[:], op=ALU.add),
                       reads=[xq_t, t_tmpD], writes=[x1b_t[i]])
                    op("sp", lambda e: e.dma_start(out=x1_s[qs, :], in_=x1b[i][:]), reads=[x1b_t[i]], writes=[x1_t], dma=st_x1[i])

                def run_idx(qt):
                    for _ in idx_phase(qt):
                        pass

                dlim = os.environ.get("DLIM", "")
                if dlim == "setup":
                    pass
                elif dlim.startswith("idx"):
                    nq = int(dlim[3:])
                    for qt in range(nq):
                        load_I(qt)
                        run_idx(qt)
                    if "dbg_acc" in debug:
                        dacc = nc.dram_tensor("dbg_acc", [128, S], F32, kind="ExternalOutput").ap()
                        dsel = nc.dram_tensor("dbg_sel", [128, S], BF16, kind="ExternalOutput").ap()
                        dbs = nc.dram_tensor("dbg_bs", [128, 8], F32, kind="ExternalOutput").ap()
                        op("sp", lambda e: e.dma_start(out=dacc[:, :], in_=acc[:]), reads=[t_acc], writes=[x1_t], dma=st_x1[0])
                        op("sp", lambda e: e.dma_start(out=dsel[:, :], in_=sel[:]), reads=[t_sel], writes=[x1_t], dma=st_x1[0])
                        op("sp", lambda e: e.dma_start(out=dbs[:, :], in_=bs[:]), reads=[t_bs], writes=[x1_t], dma=st_x1[0])
                elif dlim.startswith("qts"):
                    for qt in [int(v) for v in dlim[3:].split("_")]:
                        load_I(qt)
                        load_A(qt)
                        run_idx(qt)
                        att_phase(qt)
                elif dlim.startswith("att"):
                    nq = int(dlim[3:])
                    for qt in range(nq):
                        load_I(qt)
                        load_A(qt)
                        run_idx(qt)
                        att_phase(qt)
                else:
                    load_I(0)
                    load_A(0)
                    run_idx(0)
                    for qt in range(NT):
                        if qt + 1 < NT:
                            load_I(qt + 1)
                            load_A(qt + 1)
                            pend["g"] = idx_phase(qt + 1)
                            pend["bis_per_pump"] = 3 if qt < 6 else (2 if qt < 12 else 1)
                            pend["bis"] = 0
                        att_phase(qt)
                        pump(100000, release=True)

        kb.barrier()
        out_t = Trk(multi=True)
        if "E" in stages:
            with ExitStack() as st:
                adae, (SH2, A2, G2) = load_ada(st, "adaE", [SH2, A2, G2])
                wq = sb("wq", [128, 8, 2048], BF16, st=st)
                KT = sb("KT", [128, 2, 128], BF16, st=st)
                fgb = sb("fgb", [128, D], st=st)
                wE = Trk(multi=True)
                with ExitStack() as st2:
                    stg = [sb(f"stgE{i}", [128, 8, 512], F32, st=st2) for i in range(2)]
                    stg_t = [Trk() for _ in range(2)]
                    ld_stg = [kb.stream(f"ldstgE{i}") for i in range(2)]
                    wq_v = wq_d.rearrange("(kc p) n -> p kc n", p=128)
                    for q4 in range(4):
                        i = q4 % 2
                        op("sp", lambda e, i=i, q4=q4: e.dma_start(out=stg[i][:], in_=wq_v[:, :, q4 * 512:(q4 + 1) * 512]),
                           writes=[stg_t[i]], dma=ld_stg[i])
                        op("pool", lambda e, i=i, q4=q4: e.tensor_copy(out=wq[:, :, q4 * 512:(q4 + 1) * 512], in_=stg[i][:]),
                           reads=[stg_t[i]], writes=[wE])
                    for half, skd in enumerate((sk1_d, sk2_d)):
                        op("sp", lambda e, half=half, skd=skd: e.dma_start(out=stg[half][:, 0, 0:128], in_=skd[:, :]),
                           writes=[stg_t[half]], dma=ld_stg[half])
                        op("pe", lambda e, half=half: e.transpose(out=ps[:, half, 0:128], in_=stg[half][:, 0, 0:128], identity=ident_f[:]),
                           reads=[stg_t[half], cT], writes=[psT[half]])
                        op("act", lambda e, half=half: e.activation(out=KT[:, half, :], in_=ps[:, half, 0:128], func=AF.Copy),
                           reads=[psT[half]], writes=[wE])
                    op("sp", lambda e: e.dma_start(out=fgb[:], in_=fg_d[0:1, :].to_broadcast([128, D])), writes=[wE], dma=ld_stg[0])
                    kb.barrier()

                x1t = [sb(f"x1t{i}", [128, D], st=st) for i in range(3)]
                x1t_t = [Trk() for _ in range(3)]
                ld_x1 = [kb.stream(f"ldx1{i}") for i in range(3)]
                ssE = sb("ssE", [128, 8], st=st)
                t_ssE = Trk()
                junkE = sb("junkE", [128, D], BF16, st=st)
                t_junkE = Trk()
                tmp4 = sb("tmp4", [128, D], st=st)
                t_tmp4 = Trk()
                h2b = [sb(f"h2b{i}", [128, D], BF16, st=st) for i in range(2)]
                h2b_t = [Trk() for _ in range(2)]
                h2T = sb("h2T", [128, 8, 128], BF16, st=st)
                t_h2T = Trk()
                qTs = sb("qTs", [128, 16, 128], BF16, st=st)
                t_qTs = Trk()
                Ssb = sb("Ssb", [128, 16, 128], st=st)
                t_Ssb = Trk()
                scr8 = sb("scr8", [128, 2048], st=st)
                t_scr8 = Trk()
                m8 = sb("m8", [128, 16, 16], st=st)
                i8 = sb("i8", [128, 16, 16], U32, st=st)
                i8f = sb("i8f", [128, 16, 16], st=st)
                t_m8, t_i8, t_i8f = Trk(), Trk(), Trk()
                cand = Ssb[:].rearrange("p (h a) t -> p h (a t)", a=2)
                t_cand = t_Ssb
                b8 = sb("b8", [128, 8, 16], st=st)
                c8 = sb("c8", [128, 8, 16], U32, st=st)
                t_b8, t_c8 = Trk(), Trk()
                chi = sb("chi", [128, 128], U32, st=st)
                clo = sb("clo", [128, 128], U32, st=st)
                chif = sb("chif", [128, 8, 16], st=st)
                clof = sb("clof", [128, 8, 16], st=st)
                iab = sb("iab", [128, 2, 128], st=st)
                eidxf = sb("eidxf", [128, 128], st=st)
                t_sm = Trk()
                eidx = [sb(f"eidx{i}", [128, 128], U32, st=st) for i in range(2)]
                eidx_t = [Trk() for _ in range(2)]
                gz = sb("gz", [128, 8, 16], st=st)
                gsum = sb("gsum", [128, 16], st=st)
                t_gz = Trk()
                gates = [sb(f"gates{i}", [128, 128], st=st) for i in range(2)]
                gates_t = [Trk() for _ in range(2)]
                hu = sb("hu", [128, 128], st=st)
                ag = sb("ag", [128, 128], st=st)
                aa = sb("aa", [128, 128], st=st)
                NB = 4
                hu_t = [Trk() for _ in range(128)]
                ag_t = [Trk() for _ in range(128 // NB)]
                aa_t = [Trk() for _ in range(128 // NB)]
                NR = 20
                G = [sb(f"G{i}", [128, 2 * D], BF16, st=st) for i in range(NR)]
                G_t = [Trk() for _ in range(NR)]
                ld_G = [kb.stream(f"ldG{i}") for i in range(NR)]
                dgv = [sb(f"dgv{i}", [128, 128], BF16, st=st) for i in range(4)]
                vs_t = [Trk() for _ in range(4)]
                x2 = sb("x2", [128, D], st=st)
                t_x2 = Trk()
                outt = [sb(f"outt{i}", [128, D], st=st) for i in range(2)]
                outt_t = [Trk() for _ in range(2)]
                st_o = [kb.stream(f"sto{i}") for i in range(2)]

                def load_x1(tt):
                    i = tt % 3
                    op("sp", lambda e: e.dma_start(out=x1t[i][:], in_=x1_s[tt * 128:(tt + 1) * 128, :]),
                       reads=[x1_t], writes=[x1t_t[i]], dma=ld_x1[i])

                def top16(src_fn, n, mout, iout, tm, ti, groups):
                    gm = [Trk() for _ in range(groups)]
                    s2s = (scr8[:, 0:n], scr8[:, 1024:1024 + n])
                    s2t = (t_scr8, Trk())
                    for g0 in range(0, groups, 2):
                        pair = [g for g in (g0, g0 + 1) if g < groups]
                        for g in pair:
                            op("dve", lambda e, g=g: e.max(out=mout[:, g, 0:8], in_=src_fn(g)), reads=[t_Ssb, t_cand], writes=[gm[g]])
                        for k, g in enumerate(pair):
                            op("dve", lambda e, g=g, k=k: e.match_replace(out=s2s[k], in_to_replace=mout[:, g, 0:8], in_values=src_fn(g), imm_value=NEG),
                               reads=[t_Ssb, t_cand, gm[g]], writes=[s2t[k]])
                        for k, g in enumerate(pair):
                            op("dve", lambda e, g=g, k=k: e.max(out=mout[:, g, 8:16], in_=s2s[k]), reads=[s2t[k], gm[g]], writes=[gm[g]])
                        for g in pair:
                            op("dve", lambda e, g=g: e.max_index(out=iout[:, g, 0:8], in_max=mout[:, g, 0:8], in_values=src_fn(g)),
                               reads=[t_Ssb, t_cand, gm[g]], writes=[ti])
                        for g in pair:
                            op("dve", lambda e, g=g: e.max_index(out=iout[:, g, 8:16], in_max=mout[:, g, 8:16], in_values=src_fn(g)),
                               reads=[t_Ssb, t_cand, gm[g]], writes=[ti])
                        yield
                    op("dve", lambda e: e.tensor_copy(out=mout[:, 0, 0:1], in_=mout[:, 0, 0:1]), reads=gm, writes=[tm])

                def front(tt):
                    i = tt % 2
                    x3 = tt % 3
                    op("act", lambda e: e.activation(out=junkE[:], in_=x1t[x3][:], func=AF.Square, accum_out=ssE[:, 0:1]),
                       reads=[x1t_t[x3]], writes=[t_junkE, t_ssE])
                    op("act", lambda e: e.activation(out=ssE[:, 1:2], in_=ssE[:, 0:1], func=AF.Sqrt, bias=eps_t[:], scale=1.0 / D),
                       reads=[t_ssE, cT], writes=[t_ssE])
                    op("dve", lambda e: e.reciprocal(out=ssE[:, 2:3], in_=ssE[:, 1:2]), reads=[t_ssE], writes=[t_ssE])
                    op("dve", lambda e: e.scalar_tensor_tensor(out=tmp4[:], in0=x1t[x3][:], scalar=ssE[:, 2:3], in1=adae[:, A2],
                                                               op0=ALU.mult, op1=ALU.mult), reads=[x1t_t[x3], t_ssE, adaT], writes=[t_tmp4])
                    yield
                    op("dve", lambda e: e.tensor_tensor(out=h2b[i][:], in0=tmp4[:], in1=adae[:, SH2], op=ALU.add),
                       reads=[t_tmp4, adaT], writes=[h2b_t[i]])
                    pbv = ps[:, 7, :].bitcast(BF16)
                    for kc in range(8):
                        op("pe", lambda e, kc=kc: e.transpose(out=pbv[:, kc * 128:(kc + 1) * 128], in_=h2b[i][:, kc * 128:(kc + 1) * 128],
                                                              identity=ident_b[:]), reads=[h2b_t[i], cT], writes=[psT[7]])
                    op("act", lambda e: e.activation(out=h2T[:], in_=pbv.rearrange("p (k t) -> p k t", k=8), func=AF.Copy),
                       reads=[psT[7]], writes=[t_h2T])
                    yield
                    for g in range(4):
                        pb = g % 2
                        for u in range(4):
                            hh = g * 4 + u
                            for kc in range(8):
                                op("pe", lambda e, kc=kc: e.matmul(ps[:, pb, u * 128:(u + 1) * 128], lhsT=wq[:, kc, hh * 128:(hh + 1) * 128],
                                                                   rhs=h2T[:, kc, :], start=(kc == 0), stop=(kc == 7)),
                                   reads=[wE, t_h2T], writes=[psT[pb]])
                        op("act", lambda e: e.activation(out=qTs[:, g * 4:(g + 1) * 4, :].rearrange("p a t -> p (a t)"),
                                                         in_=ps[:, pb, :], func=AF.Copy), reads=[psT[pb]], writes=[t_qTs])
                        yield
                    for g in range(4):
                        pb = 2 + g % 2
                        for u in range(4):
                            hh = g * 4 + u
                            op("pe", lambda e: e.matmul(ps[:, pb, u * 128:(u + 1) * 128], lhsT=qTs[:, hh, :], rhs=KT[:, hh % 2, :],
                                                        start=True, stop=True), reads=[t_qTs, wE], writes=[psT[pb]])
                        op("act", lambda e: e.activation(out=Ssb[:, g * 4:(g + 1) * 4, :].rearrange("p a t -> p (a t)"),
                                                         in_=ps[:, pb, :], func=AF.Copy), reads=[psT[pb]], writes=[t_Ssb])
                        yield
                    yield from top16(lambda g: Ssb[:, g, :], 128, m8, i8, t_m8, t_i8, 16)
                    v4 = m8[:].rearrange("p (h t) k -> p h t k", t=2)
                    op("dve", lambda e: e.tensor_tensor(out=cand.rearrange("p h (a b) -> p h a b", b=16),
                                                        in0=v4[:, :, 0, :].unsqueeze(3).to_broadcast([128, 8, 16, 16]),
                                                        in1=v4[:, :, 1, :].unsqueeze(2).to_broadcast([128, 8, 16, 16]), op=ALU.add),
                       reads=[t_m8], writes=[t_cand])
                    yield
                    yield from top16(lambda g: cand[:, g, :], 256, b8, c8, t_b8, t_c8, 8)
                    c8f = c8[:].rearrange("p h k -> p (h k)")
                    op("dve", lambda e: e.tensor_scalar(out=chi[:], in0=c8f, scalar1=4, scalar2=None, op0=ALU.logical_shift_right),
                       reads=[t_c8], writes=[t_sm])
                    op("dve", lambda e: e.tensor_scalar(out=clo[:], in0=c8f, scalar1=15, scalar2=None, op0=ALU.bitwise_and),
                       reads=[t_c8], writes=[t_sm])
                    op("dve", lambda e: e.tensor_copy(out=chif[:].rearrange("p h k -> p (h k)"), in_=chi[:]), reads=[t_sm], writes=[t_sm])
                    op("dve", lambda e: e.tensor_copy(out=clof[:].rearrange("p h k -> p (h k)"), in_=clo[:]), reads=[t_sm], writes=[t_sm])
                    op("dve", lambda e: e.tensor_copy(out=i8f[:], in_=i8[:]), reads=[t_i8], writes=[t_i8f])
                    yield
                    oh = scr8[:].rearrange("p (h k i) -> p h k i", h=8, k=16)
                    io4 = iota16[:].rearrange("p (k i) -> p k i", i=16).unsqueeze(1).to_broadcast([128, 8, 16, 16])
                    i4 = i8f[:].rearrange("p (h t) k -> p h t k", t=2)
                    for half, cf in enumerate((chif, clof)):
                        op("dve", lambda e: e.tensor_tensor(out=oh, in0=cf[:].unsqueeze(3).to_broadcast([128, 8, 16, 16]), in1=io4, op=ALU.is_equal),
                           reads=[t_sm, cT], writes=[t_scr8])
                        op("dve", lambda e: e.tensor_tensor(out=oh, in0=oh, in1=i4[:, :, half, :].unsqueeze(2).to_broadcast([128, 8, 16, 16]), op=ALU.mult),
                           reads=[t_scr8, t_i8f], writes=[t_scr8])
                        op("dve", lambda e: e.tensor_reduce(out=iab[:, half, :].rearrange("p (h k) -> p h k", k=16), in_=oh,
                                                            axis=mybir.AxisListType.X, op=ALU.add), reads=[t_scr8], writes=[t_sm])
                        yield
                    op("dve", lambda e: e.scalar_tensor_tensor(out=eidxf[:], in0=iab[:, 0, :], scalar=128.0, in1=iab[:, 1, :],
                                                               op0=ALU.mult, op1=ALU.add), reads=[t_sm], writes=[t_sm])
                    op("dve", lambda e: e.tensor_copy(out=eidx[i][:], in_=eidxf[:]), reads=[t_sm], writes=[eidx_t[i]])
                    op("dve", lambda e: e.tensor_tensor(out=gz[:], in0=b8[:], in1=b8[:, :, 0:1].to_broadcast([128, 8, 16]), op=ALU.subtract),
                       reads=[t_b8], writes=[t_gz])
                    op("act", lambda e: e.activation(out=gz[:], in_=gz[:], func=AF.Exp), reads=[t_gz], writes=[t_gz])
                    op("dve", lambda e: e.tensor_reduce(out=gsum[:, 0:8], in_=gz[:], axis=mybir.AxisListType.X, op=ALU.add),
                       reads=[t_gz], writes=[t_gz])
                    op("dve", lambda e: e.reciprocal(out=gsum[:, 8:16], in_=gsum[:, 0:8]), reads=[t_gz], writes=[t_gz])
                    op("dve", lambda e: e.tensor_tensor(out=gates[i][:].rearrange("p (h k) -> p h k", k=16), in0=gz[:],
                                                        in1=gsum[:, 8:16].unsqueeze(2).to_broadcast([128, 8, 16]), op=ALU.mult),
                       reads=[t_gz], writes=[gates_t[i]])
                    yield

                pendE = {"g": None}

                def pumpE(n=1):
                    g = pendE["g"]
                    if g is None:
                        return
                    for _ in range(n):
                        try:
                            next(g)
                        except StopIteration:
                            pendE["g"] = None
                            return

                cnt = {"g": 0, "vs": 0, "jk": 0}
                ring = {}
                jk = [sb(f"jk{i}", [128, D], BF16, st=st) for i in range(2)]
                jk_t = [Trk() for _ in range(2)]

                def gather(tt, hk):
                    i = tt % 2
                    r = cnt["g"] % NR
                    cnt["g"] += 1
                    ring[(tt, hk)] = r
                    op("pool", lambda e: e.indirect_dma_start(out=G[r][:], out_offset=None, in_=uv_s[:, :],
                                                              in_offset=bass.IndirectOffsetOnAxis(ap=eidx[i][:, hk:hk + 1], axis=0)),
                       reads=[eidx_t[i], uv_t], writes=[G_t[r]], dma=ld_G[r])

                def dot(tt, hk):
                    i = tt % 2
                    r = ring[(tt, hk)]
                    bt = hk // NB
                    q = cnt["jk"] % 2
                    cnt["jk"] += 1
                    op("dve", lambda e: e.scalar_tensor_tensor(out=jk[q][:], in0=G[r][:, 0:D], scalar=1.0, in1=h2b[i][:],
                                                               op0=ALU.mult, op1=ALU.mult, accum_out=hu[:, hk:hk + 1]),
                       reads=[G_t[r], h2b_t[i]], writes=[jk_t[q], hu_t[hk]])

                def gate_batch(tt, bt):
                    i = tt % 2
                    cs = slice(bt * NB, (bt + 1) * NB)
                    op("act", lambda e: e.activation(out=ag[:, cs], in_=hu[:, cs], func=AF.Gelu), reads=hu_t[bt * NB:(bt + 1) * NB], writes=[ag_t[bt]])
                    op("dve", lambda e: e.tensor_tensor(out=aa[:, cs], in0=ag[:, cs], in1=gates[i][:, cs], op=ALU.mult),
                       reads=[ag_t[bt], gates_t[i]], writes=[aa_t[bt]])

                def vacc(tt, hk):
                    r = ring.pop((tt, hk))
                    bt = hk // NB
                    r3 = cnt["vs"] % 4
                    cnt["vs"] += 1
                    op("act", lambda e: e.activation(out=dgv[r3][:], in_=ident_b[:], func=AF.Copy, scale=aa[:, hk:hk + 1]),
                       reads=[cT, aa_t[bt]], writes=[vs_t[r3]])
                    for half in range(2):
                        op("pe", lambda e, half=half: e.matmul(ps[:, 4 + half, :], lhsT=dgv[r3][:], rhs=G[r][:, D + half * 512:D + (half + 1) * 512],
                                                               start=(hk == 0), stop=(hk == 127)),
                           reads=[G_t[r], vs_t[r3]], writes=[psT[4 + half]])

                def final(tt):
                    i = tt % 2
                    x3 = tt % 3
                    for half in range(2):
                        hsl = slice(half * 512, (half + 1) * 512)
                        op("dve", lambda e: e.tensor_tensor(out=tmp4[:, hsl], in0=ps[:, 4 + half, :], in1=adae[:, G2][:, hsl], op=ALU.mult),
                           reads=[psT[4 + half], adaT], writes=[t_tmp4])
                    op("pool", lambda e: e.tensor_tensor(out=x2[:], in0=tmp4[:], in1=x1t[x3][:], op=ALU.add),
                       reads=[t_tmp4, x1t_t[x3]], writes=[t_x2])
                    op("act", lambda e: e.activation(out=junkE[:], in_=x2[:], func=AF.Square, accum_out=ssE[:, 4:5]),
                       reads=[t_x2], writes=[t_junkE, t_ssE])
                    op("act", lambda e: e.activation(out=ssE[:, 5:6], in_=ssE[:, 4:5], func=AF.Sqrt, bias=eps_t[:], scale=1.0 / D),
                       reads=[t_ssE, cT], writes=[t_ssE])
                    op("dve", lambda e: e.reciprocal(out=ssE[:, 6:7], in_=ssE[:, 5:6]), reads=[t_ssE], writes=[t_ssE])
                    op("dve", lambda e: e.scalar_tensor_tensor(out=outt[i][:], in0=x2[:], scalar=ssE[:, 6:7], in1=fgb[:],
                                                               op0=ALU.mult, op1=ALU.mult), reads=[t_x2, t_ssE, wE], writes=[outt_t[i]])
                    op("sp", lambda e: e.dma_start(out=out_d[tt * 128:(tt + 1) * 128, :], in_=outt[i][:]),
                       reads=[outt_t[i]], writes=[out_t], dma=st_o[i])

                ntile = int(os.environ.get("ELIM", NT))
                DLY = 1
                LOOK = NR - NB - DLY - 1
                load_x1(0)
                if ntile > 1:
                    load_x1(1)
                for _ in front(0):
                    pass
                for tt in range(ntile):
                    if tt + 2 < ntile:
                        load_x1(tt + 2)
                    if tt + 1 < ntile:
                        pendE["g"] = front(tt + 1)
                    for hk in range(min(LOOK, 128)):
                        gather(tt, hk)
                    for hk in range(128):
                        if hk + LOOK < 128:
                            gather(tt, hk + LOOK)
                        dot(tt, hk)
                        if hk >= DLY and (hk - DLY) % NB == NB - 1:
                            bt = (hk - DLY) // NB
                            gate_batch(tt, bt)
                            for h2 in range(bt * NB, (bt + 1) * NB):
                                vacc(tt, h2)
                        if hk % 3 == 2:
                            pumpE(1)
                    for bt in range((128 - DLY) // NB, 128 // NB):
                        gate_batch(tt, bt)
                        for h2 in range(bt * NB, (bt + 1) * NB):
                            vacc(tt, h2)
                    pumpE(100000)
                    final(tt)
                kb.barrier()

        fin = []
        if "B" in stages:
            fin.append(scr_t)
        fin.append(yag_t)
        fin.append(x1_t)
        fin.append(out_t)
        kb.wait_all("sp", fin)
        print("instructions:", kb.ninst)
    return nc


def _in_maps(inputs, ncores=8):
    consts = make_consts()
    maps = []
    for b in range(ncores):
        m = dict(consts)
        m["x"] = np.ascontiguousarray(inputs["x"][b])
        m["c"] = np.ascontiguousarray(inputs["c"][b:b + 1])
        for k in ("w_ada", "b_ada", "norm1_g", "w_in", "gla_w_gate_up", "gla_b_gate", "gla_norm_g",
                  "dsa_kv_norm_g", "dsa_w_uk", "dsa_w_uv", "w_branch_a", "w_branch_b", "w_out", "norm2_g",
                  "peer_w_q", "peer_sub_keys_1", "peer_sub_keys_2", "peer_u", "peer_v"):
            m[k] = np.ascontiguousarray(inputs[k][0]) if inputs[k].ndim > 1 and inputs[k].shape[0] == 1 else inputs[k]
        for k in ("b_ada", "norm1_g", "gla_b_gate", "gla_norm_g", "dsa_kv_norm_g", "norm2_g"):
            m[k] = np.ascontiguousarray(inputs[k]).reshape(1, -1)
        m["final_norm_g"] = np.ascontiguousarray(inputs["final_norm_g"]).reshape(1, -1)
        maps.append(m)
    return maps


def kernel(**inputs):
    inputs = {k: np.asarray(v) for k, v in inputs.items()}
    nc = build()
    maps = _in_maps(inputs)
    res = run_bass_kernel_spmd(nc, maps, core_ids=list(range(8)))
    return np.stack([r["out"] for r in res.results], axis=0).astype(np.float32)
```

```python
import os
import numpy as np
from contextlib import ExitStack
import concourse.bass as bass
import concourse.mybir as mybir
from concourse.bass_utils import run_bass_kernel_spmd

F32 = mybir.dt.float32
BF16 = mybir.dt.bfloat16
U32 = mybir.dt.uint32
AF = mybir.ActivationFunctionType
ALU = mybir.AluOpType

D = 1024
S = 4096
NT = S // 128
IN_TOTAL = 7000
OFF = dict(gq=0, gk=512, gv=1024, gr=2048, glow=3072, dq=3088, dkv=4112, iq=4368, ik=4880,
           iw=4944, za=4952, zb=5976)
NEG = -1.0e30


class Trk:
    __slots__ = ("w", "r", "multi")

    def __init__(self, multi=False):
        self.w = {}
        self.r = {}
        self.multi = multi


class Stream:
    def __init__(self, sem, sid):
        self.sem = sem
        self.n = 0
        self.sid = sid
        self.maxwait = 0


class Eng:
    def __init__(self, name, e, sem, sid):
        self.name = name
        self.e = e
        self.sem = sem
        self.sid = sid
        self.n = 0
        self.seen = {}


class KB:
    def __init__(self, nc, es):
        self.nc = nc
        self.es = es
        self.nsid = 0
        self.E = {}
        for name, e in (("pe", nc.tensor), ("act", nc.scalar), ("dve", nc.vector),
                        ("pool", nc.gpsimd), ("sp", nc.sync)):
            sem = es.enter_context(nc.semaphore("sem_" + name))
            self.E[name] = Eng(name, e, sem, self.nsid)
            self.nsid += 1
        self.ninst = 0
        self.streams = []
        self.sid2stream = {}

    def stream(self, name):
        sem = self.es.enter_context(self.nc.semaphore("st_" + name))
        s = Stream(sem, self.nsid)
        self.nsid += 1
        self.streams.append(s)
        self.sid2stream[s.sid] = s
        return s

    def op(self, en, fn, reads=(), writes=(), dma=None):
        e = self.E[en]
        need = {}

        def add(evd, skip_own):
            for k, (sem, val) in evd.items():
                if skip_own and k == e.sid:
                    continue
                if need.get(k, (None, 0))[1] < val:
                    need[k] = (sem, val)

        own = (en == "pe" and dma is None)
        for t in reads:
            add(t.w, own)
        for t in writes:
            if not t.multi:
                add(t.w, own)
            add(t.r, own)
        for k, (sem, val) in need.items():
            stt = self.sid2stream.get(k)
            if stt is not None:
                val = stt.n
                stt.maxwait = max(stt.maxwait, val)
            if e.seen.get(k, 0) >= val:
                continue
            e.e.wait_ge(sem, val)
            e.seen[k] = val
            self.ninst += 1
        if dma is not None and dma.maxwait > e.seen.get(dma.sid, 0):
            e.e.wait_ge(dma.sem, dma.maxwait)
            e.seen[dma.sid] = dma.maxwait
            self.ninst += 1
        inst = fn(e.e)
        self.ninst += 1
        if dma is None:
            e.n += 1
            inst.then_inc(e.sem, 1)
            ev = (e.sem, e.n)
            key = e.sid
        else:
            dma.n += 16
            inst.then_inc(dma.sem, 16)
            ev = (dma.sem, dma.n)
            key = dma.sid
        for t in reads:
            if t.r.get(key, (None, 0))[1] < ev[1]:
                t.r[key] = ev
        for t in writes:
            if t.multi:
                if t.w.get(key, (None, 0))[1] < ev[1]:
                    t.w[key] = ev
            else:
                t.w = {key: ev}
                t.r = {}
        return ev

    def barrier(self):
        for e in self.E.values():
            for o in self.E.values():
                if o is e or o.n == 0 or e.seen.get(o.sid, 0) >= o.n:
                    continue
                e.e.wait_ge(o.sem, o.n)
                e.seen[o.sid] = o.n
                self.ninst += 1
            for stt in self.streams:
                if stt.n == 0 or e.seen.get(stt.sid, 0) >= stt.n:
                    continue
                e.e.wait_ge(stt.sem, stt.n)
                e.seen[stt.sid] = stt.n
                stt.maxwait = stt.n
                self.ninst += 1

    def wait_all(self, en, trks):
        e = self.E[en]
        for t in trks:
            for k, (sem, val) in t.w.items():
                stt = self.sid2stream.get(k)
                if stt is not None:
                    val = stt.n
                    stt.maxwait = max(stt.maxwait, val)
                if e.seen.get(k, 0) >= val:
                    continue
                e.e.wait_ge(sem, val)
                e.seen[k] = val


def make_consts():
    p = np.arange(128)
    ident = np.eye(128, dtype=np.float32)
    lt = (p[:, None] <= p[None, :]).astype(np.float32)
    ut = (p[:, None] > p[None, :]).astype(np.float32)
    diag = np.where(p[None, :] <= p[:, None], 0.0, NEG).astype(np.float32)
    slopes = np.exp2(-8.0 * np.arange(1, 9, dtype=np.float32) / 8).astype(np.float32)
    dl = np.arange(32)
    bias = slopes[None, :, None] * (p[:, None, None] - 127.0 + 128.0 * (dl[None, None, :] - 31.0))
    bias = bias.astype(np.float32).reshape(128, 256)
    iota16 = np.tile(np.arange(16, dtype=np.float32)[None, None, :], (128, 16, 1)).reshape(128, 256)
    pos1 = np.tile(np.arange(1, 129, dtype=np.float32)[None, :], (128, 1))
    slopes8 = np.tile(slopes[None, :], (128, 1)).astype(np.float32)
    esel = np.zeros((8, 8, 128), np.float32)
    for h in range(8):
        esel[h, h, :] = 1.0
    posr = np.ones((10, 32, 128), np.float32)
    posr[8, :, :] = (p - 127.0)[None, :]
    posr[9, :, :] = (128.0 * (dl - 31.0))[:, None]
    slopeR = np.zeros((10, 8, 128), np.float32)
    slopeR[8:10] = slopes[None, :, None]
    basej = np.tile((128.0 * np.arange(NT, dtype=np.float32))[None, :], (128, 1))
    return dict(c_posr=posr.reshape(10, 4096), c_slopeR=slopeR.reshape(10, 1024), c_basej=basej,
                c_ident=ident, c_lt=lt, c_ut=ut, c_diag=diag, c_bias=bias, c_iota16=iota16,
                c_pos1=pos1, c_slopes8=slopes8, c_esel=esel.reshape(8, 1024))


def build(stages=("A", "B", "C", "D", "E"), debug=(), scr_in=()):
    nc = bass.Bass("TRN2", target_bir_lowering=False)

    def din(name, shape, dt=F32):
        return nc.dram_tensor(name, list(shape), dt, kind="ExternalInput").ap()

    def dscr(name, shape, dt=BF16):
        kind = "ExternalOutput" if name in debug else ("ExternalInput" if name in scr_in else "Internal")
        return nc.dram_tensor(name, list(shape), dt, kind=kind).ap()

    x_d = din("x", [S, D])
    c_d = din("c", [1, D])
    wada_d = din("w_ada", [D, 6 * D])
    bada_d = din("b_ada", [1, 6 * D])
    g1_d = din("norm1_g", [1, D])
    win_d = din("w_in", [D, IN_TOTAL])
    wgu_d = din("gla_w_gate_up", [16, 512])
    bgate_d = din("gla_b_gate", [1, 512])
    gng_d = din("gla_norm_g", [1, D])
    kvg_d = din("dsa_kv_norm_g", [1, 256])
    wuk_d = din("dsa_w_uk", [8, 128, 256])
    wuv_d = din("dsa_w_uv", [8, 256, 128])
    wba_d = din("w_branch_a", [D, D])
    wbb_d = din("w_branch_b", [D, D])
    wout_d = din("w_out", [D, D])
    g2_d = din("norm2_g", [1, D])
    wq_d = din("peer_w_q", [D, 2048])
    sk1_d = din("peer_sub_keys_1", [128, 128])
    sk2_d = din("peer_sub_keys_2", [128, 128])
    pu_d = din("peer_u", [16384, D])
    pv_d = din("peer_v", [16384, D])
    fg_d = din("final_norm_g", [1, D])
    cst = {k: din(k, v.shape) for k, v in make_consts().items()}

    out_d = nc.dram_tensor("out", [S, D], F32, kind="ExternalOutput").ap()

    ada_s = dscr("ada_s", [1, 6 * D], F32)
    qT_s = dscr("qT_s", [512, S])
    kT_s = dscr("kT_s", [512, S])
    glT_s = dscr("glT_s", [16, S])
    dqT_s = dscr("dqT_s", [1024, S])
    iqT_s = dscr("iqT_s", [512, S])
    ikT_s = dscr("ikT_s", [64, S])
    k_s = dscr("k_s", [S, 512])
    v_s = dscr("v_s", [S, 1024])
    sr_s = dscr("sr_s", [S, 1024])
    dkv_s = dscr("dkv_s", [S, 256])
    iw_s = dscr("iw_s", [S, 8], F32)
    sza_s = dscr("sza_s", [S, 1024])
    szb_s = dscr("szb_s", [S, 1024])
    yag_s = dscr("yag_s", [S, 1024])
    x1_s = dscr("x1_s", [S, D], F32)
    uv_s = dscr("uv_s", [16384, 2 * D])

    with ExitStack() as es:
        kb = KB(nc, es)
        op = kb.op
        es.enter_context(nc.allow_non_contiguous_dma(reason="small strided setup loads"))
        es.enter_context(nc.allow_low_precision(reason="bf16 matmul operands, fp32 accumulation"))

        def sb(name, shape, dt=F32, st=None):
            return (st or es).enter_context(nc.sbuf_tensor(name, list(shape), dt))

        ps = es.enter_context(nc.psum_tensor("ps", [128, 8, 512], F32))
        psT = [Trk() for _ in range(8)]

        ld_c = kb.stream("ldc")
        cT = Trk(multi=True)
        ident_f = sb("ident_f", [128, 128])
        lt_f = sb("lt_f", [128, 128])
        ut_f = sb("ut_f", [128, 128])
        diag_f = sb("diag_f", [128, 128])
        bias_t = sb("bias_t", [128, 256])
        iota16 = sb("iota16", [128, 256])
        for t, nm in ((ident_f, "c_ident"), (lt_f, "c_lt"), (ut_f, "c_ut"), (diag_f, "c_diag"),
                      (bias_t, "c_bias"), (iota16, "c_iota16")):
            op("sp", lambda e, t=t, nm=nm: e.dma_start(out=t[:], in_=cst[nm][:, :]), writes=[cT], dma=ld_c)
        ident_b = sb("ident_b", [128, 128], BF16)
        lt_b = sb("lt_b", [128, 128], BF16)
        eps_t = sb("eps_t", [128, 1])
        one_t = sb("one_t", [128, 1])
        ones_row = sb("ones_row", [1, 128], BF16)
        op("dve", lambda e: e.tensor_copy(out=ident_b[:], in_=ident_f[:]), reads=[cT], writes=[cT])
        op("dve", lambda e: e.tensor_copy(out=lt_b[:], in_=lt_f[:]), reads=[cT], writes=[cT])
        op("dve", lambda e: e.memset(eps_t[:], 1e-6), writes=[cT])
        op("dve", lambda e: e.memset(one_t[:], 1.0), writes=[cT])
        op("dve", lambda e: e.memset(ones_row[:], 1.0), writes=[cT])

        adaT = Trk()
        t_adas = Trk()
        ld_ada = kb.stream("ldada")
        SH1, A1, G1, SH2, A2, G2 = [slice(i * D, (i + 1) * D) for i in range(6)]

        def load_ada(st, name, sls):
            t = sb(name, [128, len(sls) * D], st=st)
            outs = []
            for n, sl in enumerate(sls):
                op("sp", lambda e, n=n, sl=sl: e.dma_start(out=t[:, n * D:(n + 1) * D],
                                                         in_=ada_s[0:1, sl].to_broadcast([128, D])),
                   reads=[t_adas], writes=[adaT], dma=ld_ada)
                outs.append(slice(n * D, (n + 1) * D))
            return t, outs

        if "A" in stages:
            with ExitStack() as st:
                cTt = sb("cTt", [128, 8], st=st)
                scT = sb("scT", [128, 8], st=st)
                wada = [sb(f"wada{i}", [128, 8, 512], st=st) for i in range(2)]
                wadaT = [Trk() for _ in range(2)]
                ld_w = [kb.stream(f"ldwada{i}") for i in range(2)]
                arow = sb("arow", [1, 6 * D], st=st)
                brow = sb("brow", [1, 6 * D], st=st)
                grow = sb("grow", [1, 2 * D], st=st)
                t_c, t_sc, t_arow, t_brow = Trk(), Trk(), Trk(), Trk()
                ld_a = kb.stream("lda")
                op("sp", lambda e: e.dma_start(out=cTt[:], in_=c_d.rearrange("o (kc p) -> p (o kc)", p=128)),
                   writes=[t_c], dma=ld_a)
                op("sp", lambda e: e.dma_start(out=brow[:], in_=bada_d[:, :]), writes=[t_brow], dma=ld_a)
                op("sp", lambda e: e.dma_start(out=grow[0:1, 0:D], in_=g1_d[:, :]), writes=[t_brow], dma=ld_a)
                op("sp", lambda e: e.dma_start(out=grow[0:1, D:2 * D], in_=g2_d[:, :]), writes=[t_brow], dma=ld_a)
                op("act", lambda e: e.activation(out=scT[:], in_=cTt[:], func=AF.Silu), reads=[t_c], writes=[t_sc])
                wv = wada_d.rearrange("(kc p) n -> p kc n", p=128)

                def load_wada(cg):
                    i = cg % 2
                    op("sp", lambda e: e.dma_start(out=wada[i][:], in_=wv[:, :, cg * 512:(cg + 1) * 512]),
                       writes=[wadaT[i]], dma=ld_w[i])
                load_wada(0)
                for cg in range(12):
                    if cg + 1 < 12:
                        load_wada(cg + 1)
                    i = cg % 2
                    b = cg % 2
                    for kc in range(8):
                        op("pe", lambda e, kc=kc: e.matmul(ps[0:1, b, :], lhsT=scT[:, kc:kc + 1], rhs=wada[i][:, kc, :],
                                                           start=(kc == 0), stop=(kc == 7)),
                           reads=[t_sc, wadaT[i]], writes=[psT[b]])
                    op("dve", lambda e: e.tensor_tensor(out=arow[0:1, cg * 512:(cg + 1) * 512], in0=ps[0:1, b, :],
                                                        in1=brow[0:1, cg * 512:(cg + 1) * 512], op=ALU.add),
                       reads=[psT[b], t_brow], writes=[t_arow])
                for (sl, gsl) in ((A1, slice(0, D)), (A2, slice(D, 2 * D))):
                    op("dve", lambda e, sl=sl, gsl=gsl: e.scalar_tensor_tensor(
                        out=arow[0:1, sl], in0=arow[0:1, sl], scalar=1.0, in1=grow[0:1, gsl],
                        op0=ALU.add, op1=ALU.mult), reads=[t_arow, t_brow], writes=[t_arow])
                op("sp", lambda e: e.dma_start(out=ada_s[:, :], in_=arow[:]), reads=[t_arow], writes=[t_adas], dma=ld_a)

        stBC = ExitStack()
        uv_t = Trk(multi=True)
        CVR = 2
        cv32 = [sb(f"cv32_{i}", [128, CVR, D], st=stBC) for i in range(2)]
        cv16 = [sb(f"cv16_{i}", [128, CVR, D], BF16, st=stBC) for i in range(2)]
        cv32_t = [Trk() for _ in range(2)]
        cv16_t = [Trk() for _ in range(2)]
        ld_cv = [kb.stream(f"ldcv{i}") for i in range(2)]
        st_cv = [kb.stream(f"stcv{i}") for i in range(2)]
        uv_v = uv_s.rearrange("(p a) (t d) -> p a t d", a=128, t=2)
        cvs = {"n": 0, "pending": None}
        NCV = 2 * (128 // CVR)

        def conv_store():
            pnd = cvs["pending"]
            if pnd is not None:
                k, t, c = pnd
                op("sp", lambda e: e.dma_start(out=uv_v[:, c * CVR:(c + 1) * CVR, t, :], in_=cv16[k][:]),
                   reads=[cv16_t[k]], writes=[uv_t], dma=st_cv[k])
                cvs["pending"] = None

        def conv_step():
            n = cvs["n"]
            if n >= NCV:
                conv_store()
                return
            cvs["n"] += 1
            k = n % 2
            t, c = n % 2, n // 2
            tab = pu_d if t == 0 else pv_d
            src_v = tab.rearrange("(p a) d -> p a d", a=128)[:, c * CVR:(c + 1) * CVR, :]
            op("sp", lambda e: e.dma_start(out=cv32[k][:], in_=src_v), writes=[cv32_t[k]], dma=ld_cv[k])
            conv_store()
            op("pool", lambda e: e.tensor_copy(out=cv16[k][:], in_=cv32[k][:]), reads=[cv32_t[k]], writes=[cv16_t[k]])
            cvs["pending"] = (k, t, c)

        kb.barrier()
        if "B" in stages:
            with ExitStack() as st:
                adab, (A1, SH1) = load_ada(st, "adaB", [A1, SH1])
                hT = sb("hT", [128, 8, S], BF16, st=st)
                hT_t = [Trk() for _ in range(NT)]
                xt = [sb(f"xt{i}", [128, D], st=st) for i in range(2)]
                xt_t = [Trk() for _ in range(2)]
                ld_x = [kb.stream(f"ldx{i}") for i in range(2)]
                junk = sb("junk", [128, D], BF16, st=st)
                t_junk = Trk()
                tmp = sb("tmpB", [128, D], st=st)
                t_tmp = Trk()
                hb = sb("hb", [128, D], BF16, st=st)
                t_hb = Trk()
                ss = sb("ssB", [128, 4], st=st)
                t_ss = Trk()
                x_v = x_d.rearrange("(t p) d -> t p d", p=128)

                def load_x(tt):
                    i = tt % 2
                    op("sp", lambda e: e.dma_start(out=xt[i][:], in_=x_v[tt]), writes=[xt_t[i]], dma=ld_x[i])
                load_x(0)
                for tt in range(NT):
                    if tt + 1 < NT:
                        load_x(tt + 1)
                    i = tt % 2
                    op("act", lambda e: e.activation(out=junk[:], in_=xt[i][:], func=AF.Square, accum_out=ss[:, 0:1]),
                       reads=[xt_t[i]], writes=[t_junk, t_ss])
                    op("act", lambda e: e.activation(out=ss[:, 1:2], in_=ss[:, 0:1], func=AF.Sqrt, bias=eps_t[:],
                                                     scale=1.0 / D), reads=[t_ss, cT], writes=[t_ss])
                    op("dve", lambda e: e.reciprocal(out=ss[:, 2:3], in_=ss[:, 1:2]), reads=[t_ss], writes=[t_ss])
                    op("dve", lambda e: e.scalar_tensor_tensor(out=tmp[:], in0=xt[i][:], scalar=ss[:, 2:3],
                                                               in1=adab[:, A1], op0=ALU.mult, op1=ALU.mult),
                       reads=[xt_t[i], t_ss, adaT], writes=[t_tmp])
                    op("dve", lambda e: e.tensor_tensor(out=hb[:], in0=tmp[:], in1=adab[:, SH1], op=ALU.add),
                       reads=[t_tmp, adaT], writes=[t_hb])
                    pb = 6 + (tt % 2)
                    pbv = ps[:, pb, :].bitcast(BF16)
                    for kc in range(8):
                        op("pe", lambda e, kc=kc: e.transpose(out=pbv[:, kc * 128:(kc + 1) * 128],
                                                              in_=hb[:, kc * 128:(kc + 1) * 128], identity=ident_b[:]),
                           reads=[t_hb, cT], writes=[psT[pb]])
                    op("act", lambda e: e.activation(out=hT[:, :, tt * 128:(tt + 1) * 128],
                                                     in_=pbv.rearrange("p (k t) -> p k t", k=8), func=AF.Copy),
                       reads=[psT[pb]], writes=[hT_t[tt]])

                wt = [sb(f"wt{i}", [128, 8, 512], BF16, st=st) for i in range(2)]
                wt_t = [Trk() for _ in range(2)]
                ld_wt = [kb.stream(f"ldwt{i}") for i in range(2)]
                stf = [sb(f"stf{i}", [128, S], BF16, st=st) for i in range(2)]
                stf_t = [Trk(multi=True) for _ in range(2)]
                st_f = [kb.stream(f"stf{i}") for i in range(2)]
                stt_ = [sb(f"stt{i}", [128, 4, 512], BF16, st=st) for i in range(2)]
                stt_t = [Trk(multi=True) for _ in range(2)]
                st_t = [kb.stream(f"stt{i}") for i in range(2)]
                sti = sb("sti", [128, NT, 8], st=st)
                sti_t = Trk()
                win_v = win_d.rearrange("(kc p) n -> p kc n", p=128)
                scr_t = Trk(multi=True)
                self_scr = scr_t

                blocks = []
                for nm, dst, n in (("gq", qT_s, 512), ("gk", kT_s, 512), ("glow", glT_s, 16), ("dq", dqT_s, 1024),
                                   ("iq", iqT_s, 512), ("ik", ikT_s, 64)):
                    for c0 in range(0, n, 128):
                        w = min(128, n - c0)
                        blocks.append(("F", OFF[nm] + c0, w, dst, c0, None))
                for nm, dst, n, fn in (("gk", k_s, 512, AF.Copy), ("gv", v_s, 1024, AF.Copy), ("gr", sr_s, 1024, AF.Silu),
                                       ("dkv", dkv_s, 256, AF.Copy), ("iw", iw_s, 8, AF.Copy),
                                       ("za", sza_s, 1024, AF.Sigmoid), ("zb", szb_s, 1024, AF.Sigmoid)):
                    for c0 in range(0, n, 512):
                        w = min(512, n - c0)
                        blocks.append(("T", OFF[nm] + c0, w, dst, c0, fn))

                wt32 = [sb(f"wt32_{i}", [128, 8, 512], F32, st=st) for i in range(2)]
                wt32_t = [Trk() for _ in range(2)]

                def load_w(bi):
                    kind, col, w, dst, c0, fn = blocks[bi]
                    i = bi % 2
                    op("sp", lambda e: e.dma_start(out=wt32[i][:, :, 0:w], in_=win_v[:, :, col:col + w]),
                       writes=[wt32_t[i]], dma=ld_wt[i])
                    op("pool", lambda e: e.tensor_copy(out=wt[i][:, :, 0:w], in_=wt32[i][:, :, 0:w]),
                       reads=[wt32_t[i]], writes=[wt_t[i]])
                load_w(0)
                nf = 0
                ntb = 0
                evac = 0
                for bi, (kind, col, w, dst, c0, fn) in enumerate(blocks):
                    if bi + 1 < len(blocks):
                        load_w(bi + 1)
                    if "E" in stages:
                        conv_step()
                    i = bi % 2
                    if kind == "F":
                        si = nf % 2
                        nf += 1
                        for tg in range(8):
                            pb = tg % 4
                            for kc in range(8):
                                op("pe", lambda e, kc=kc: e.matmul(ps[0:w, pb, :], lhsT=wt[i][:, kc, 0:w],
                                                                   rhs=hT[:, kc, tg * 512:(tg + 1) * 512],
                                                                   start=(kc == 0), stop=(kc == 7)),
                                   reads=[wt_t[i]] + hT_t[tg * 4:(tg + 1) * 4], writes=[psT[pb]])
                            en = "act" if evac % 2 == 0 else "dve"
                            evac += 1
                            if en == "act":
                                op("act", lambda e: e.activation(out=stf[si][0:w, tg * 512:(tg + 1) * 512],
                                                                 in_=ps[0:w, pb, :], func=AF.Copy),
                                   reads=[psT[pb]], writes=[stf_t[si]])
                            else:
                                op("dve", lambda e: e.tensor_copy(out=stf[si][0:w, tg * 512:(tg + 1) * 512],
                                                                  in_=ps[0:w, pb, :]),
                                   reads=[psT[pb]], writes=[stf_t[si]])
                        op("sp", lambda e: e.dma_start(out=dst[c0:c0 + w, :], in_=stf[si][0:w, :]),
                           reads=[stf_t[si]], writes=[scr_t], dma=st_f[si])
                    else:
                        is_iw = (dst is iw_s)
                        for tt in range(NT):
                            pb = tt % 4
                            for kc in range(8):
                                op("pe", lambda e, kc=kc: e.matmul(ps[:, pb, 0:w], lhsT=hT[:, kc, tt * 128:(tt + 1) * 128],
                                                                   rhs=wt[i][:, kc, 0:w],
                                                                   start=(kc == 0), stop=(kc == 7)),
                                   reads=[wt_t[i], hT_t[tt]], writes=[psT[pb]])
                            if is_iw:
                                op("dve", lambda e: e.tensor_copy(out=sti[:, tt, :], in_=ps[:, pb, 0:w]),
                                   reads=[psT[pb]], writes=[sti_t])
                                continue
                            si = ntb % 2
                            a = tt % 4
                            if fn == AF.Copy and evac % 2 == 1:
                                op("dve", lambda e: e.tensor_copy(out=stt_[si][:, a, 0:w], in_=ps[:, pb, 0:w]),
                                   reads=[psT[pb]], writes=[stt_t[si]])
                            else:
                                op("act", lambda e: e.activation(out=stt_[si][:, a, 0:w], in_=ps[:, pb, 0:w], func=fn),
                                   reads=[psT[pb]], writes=[stt_t[si]])
                            evac += 1
                            if a == 3:
                                r0 = (tt - 3) * 128
                                op("sp", lambda e: e.dma_start(
                                    out=dst[r0:r0 + 512, c0:c0 + w].rearrange("(a p) n -> p a n", p=128),
                                    in_=stt_[si][:, :, 0:w]), reads=[stt_t[si]], writes=[scr_t], dma=st_t[si])
                                ntb += 1
                        if is_iw:
                            op("sp", lambda e: e.dma_start(out=iw_s.rearrange("(t p) n -> p t n", p=128), in_=sti[:]),
                               reads=[sti_t], writes=[scr_t], dma=st_t[0])
        else:
            scr_t = Trk(multi=True)

        kb.barrier()
        yag_t = Trk(multi=True)
        if "C" in stages:
            with ExitStack() as st:
                stg = [sb(f"stgC{i}", [128, 8, 512], F32, st=st) for i in range(2)]
                stg_t = [Trk() for _ in range(2)]
                ld_stg = [kb.stream(f"ldstgC{i}") for i in range(2)]
                wgu = sb("wgu", [16, 512], BF16, st=st)
                bgr = sb("bgr", [1, 512], BF16, st=st)
                gngb = sb("gngb", [128, D], st=st)
                wba = sb("wba", [128, 8, D], BF16, st=st)
                wC = Trk(multi=True)
                op("sp", lambda e: e.dma_start(out=stg[0][0:16, 0, :], in_=wgu_d[:, :]), writes=[stg_t[0]], dma=ld_stg[0])
                op("pool", lambda e: e.tensor_copy(out=wgu[:], in_=stg[0][0:16, 0, :]), reads=[stg_t[0]], writes=[wC])
                op("sp", lambda e: e.dma_start(out=stg[1][0:1, 0, :], in_=bgate_d[:, :]), writes=[stg_t[1]], dma=ld_stg[1])
                op("pool", lambda e: e.tensor_copy(out=bgr[:], in_=stg[1][0:1, 0, :]), reads=[stg_t[1]], writes=[wC])
                op("sp", lambda e: e.dma_start(out=gngb[:], in_=gng_d[0:1, :].to_broadcast([128, D])), writes=[wC], dma=ld_stg[0])
                wba_v = wba_d.rearrange("(kc p) n -> p kc n", p=128)
                for half in range(2):
                    op("sp", lambda e, half=half: e.dma_start(out=stg[half][:], in_=wba_v[:, :, half * 512:(half + 1) * 512]),
                       writes=[stg_t[half]], dma=ld_stg[half])
                    op("pool", lambda e, half=half: e.tensor_copy(out=wba[:, :, half * 512:(half + 1) * 512], in_=stg[half][:]),
                       reads=[stg_t[half]], writes=[wC])

                qTc = [sb(f"qTc{i}", [128, 4, 128], BF16, st=st) for i in range(2)]
                kTc = [sb(f"kTc{i}", [128, 4, 128], BF16, st=st) for i in range(2)]
                ktok = [sb(f"ktok{i}", [128, 512], BF16, st=st) for i in range(2)]
                vc = [sb(f"vc{i}", [128, D], BF16, st=st) for i in range(2)]
                glc = [sb(f"glc{i}", [16, 128], BF16, st=st) for i in range(2)]
                src = [sb(f"src{i}", [128, D], BF16, st=st) for i in range(2)]
                szac = [sb(f"szac{i}", [128, D], BF16, st=st) for i in range(2)]
                in_t = [Trk(multi=True) for _ in range(2)]
                ld_C = [kb.stream(f"ldC{i}") for i in range(2)]
                qT_v = qT_s.rearrange("(h d) t -> d h t", d=128)
                kT_v = kT_s.rearrange("(h d) t -> d h t", d=128)

                def load_c(c):
                    i = c % 2
                    cs = slice(c * 128, (c + 1) * 128)
                    for dst, srcap in ((qTc[i][:], qT_v[:, :, cs]), (kTc[i][:], kT_v[:, :, cs]), (ktok[i][:], k_s[cs, :]),
                                       (vc[i][:], v_s[cs, :]), (glc[i][:], glT_s[:, cs]), (src[i][:], sr_s[cs, :]),
                                       (szac[i][:], sza_s[cs, :])):
                        op("sp", lambda e, dst=dst, srcap=srcap: e.dma_start(out=dst, in_=srcap),
                           reads=[scr_t], writes=[in_t[i]], dma=ld_C[i])

                sp_t = sb("sp_t", [128, 512], st=st)
                eke = sb("eke", [128, 512], st=st)
                kend = sb("kend", [128, 512], BF16, st=st)
                eq4 = sb("eq4", [128, 4, 128], st=st)
                ek4 = sb("ek4", [128, 4, 128], st=st)
                qd4 = sb("qd4", [128, 4, 128], BF16, st=st)
                ki4 = sb("ki4", [128, 4, 128], BF16, st=st)
                at4 = sb("at4", [128, 4, 128], BF16, st=st)
                S_f = sb("S_f", [128, 4, 256], st=st)
                S_b = sb("S_b", [128, 4, 256], BF16, st=st)
                ss4 = sb("ss4", [128, 12], st=st)
                junkC = sb("junkC", [128, 256], BF16, st=st)
                tmpC = sb("tmpC", [128, D], st=st)
                ga = sb("ga", [128, D], BF16, st=st)
                gaT = sb("gaT", [128, 8, 128], BF16, st=st)
                yag = [sb(f"yag{i}", [128, D], BF16, st=st) for i in range(2)]
                yag_bt = [Trk() for _ in range(2)]
                st_y = [kb.stream(f"sty{i}") for i in range(2)]
                (t_sp, t_eke, t_kend, t_eq, t_ek, t_qd, t_ki, t_at, t_ss4, t_junk, t_tmp, t_ga, t_gaT) = [Trk() for _ in range(13)]
                t_S = [Trk() for _ in range(4)]
                t_Sb = [Trk() for _ in range(4)]
                t_psC = t_psD = psT[2]
                op("dve", lambda e: e.memset(S_f[:], 0.0), writes=t_S)
                op("dve", lambda e: e.memset(S_b[:], 0.0), writes=t_Sb)

                load_c(0)
                for c in range(NT):
                    i = c % 2
                    if c + 1 < NT:
                        load_c(c + 1)
                    conv_step()
                    conv_step()
                    conv_step()
                    it = [in_t[i]]
                    op("pe", lambda e: e.matmul(ps[:, 0, :], lhsT=glc[i][0:16, :], rhs=wgu[0:16, :], start=True, stop=False),
                       reads=it + [wC], writes=[psT[0]])
                    op("pe", lambda e: e.matmul(ps[:, 0, :], lhsT=ones_row[0:1, :], rhs=bgr[0:1, :], start=False, stop=True),
                       reads=[cT, wC], writes=[psT[0]])
                    op("act", lambda e: e.activation(out=sp_t[:], in_=ps[:, 0, :], func=AF.Exp, scale=-1.0),
                       reads=[psT[0]], writes=[t_sp])
                    op("act", lambda e: e.activation(out=sp_t[:], in_=sp_t[:], func=AF.Ln, bias=one_t[:]),
                       reads=[t_sp, cT], writes=[t_sp])
                    op("pe", lambda e: e.matmul(ps[:, 1, :], lhsT=ut_f[:], rhs=sp_t[:], start=True, stop=True),
                       reads=[cT, t_sp], writes=[psT[1]])
                    op("act", lambda e: e.activation(out=eke[:], in_=ps[:, 1, :], func=AF.Exp, scale=-1.0 / 16),
                       reads=[psT[1]], writes=[t_eke])
                    op("dve", lambda e: e.tensor_tensor(out=kend[:], in0=ktok[i][:], in1=eke[:], op=ALU.mult),
                       reads=it + [t_eke], writes=[t_kend])
                    for h in range(4):
                        hs = slice(h * 128, (h + 1) * 128)
                        op("pe", lambda e, h=h, hs=hs: e.matmul(ps[:, 2, h * 128:(h + 1) * 128], lhsT=sp_t[:, hs], rhs=lt_f[:], start=True, stop=True),
                           reads=[t_sp, cT], writes=[psT[2]])
                    op("act", lambda e: e.activation(out=eq4[:].rearrange("p h t -> p (h t)"), in_=ps[:, 2, :], func=AF.Exp, scale=-1.0 / 16),
                       reads=[psT[2]], writes=[t_eq])
                    op("act", lambda e: e.activation(out=ek4[:].rearrange("p h t -> p (h t)"), in_=ps[:, 2, :], func=AF.Exp, scale=1.0 / 16),
                       reads=[psT[2]], writes=[t_ek])
                    op("dve", lambda e: e.scalar_tensor_tensor(out=qd4[:].rearrange("p h t -> p (h t)"),
                                                               in0=qTc[i][:].rearrange("p h t -> p (h t)"), scalar=128.0 ** -0.5,
                                                               in1=eq4[:].rearrange("p h t -> p (h t)"), op0=ALU.mult, op1=ALU.mult),
                       reads=it + [t_eq], writes=[t_qd])
                    op("dve", lambda e: e.tensor_tensor(out=ki4[:].rearrange("p h t -> p (h t)"), in0=kTc[i][:].rearrange("p h t -> p (h t)"),
                                                        in1=ek4[:].rearrange("p h t -> p (h t)"), op=ALU.mult),
                       reads=it + [t_ek], writes=[t_ki])
                    for h in range(4):
                        op("pe", lambda e, h=h: e.matmul(ps[:, 3, h * 128:(h + 1) * 128], lhsT=ki4[:, h, :], rhs=qd4[:, h, :], start=True, stop=True),
                           reads=[t_ki, t_qd], writes=[psT[3]])
                    op("dve", lambda e: e.tensor_tensor(out=at4[:], in0=ps[:, 3, :].rearrange("p (h t) -> p h t", h=4),
                                                        in1=lt_f[:].unsqueeze(1).to_broadcast([128, 4, 128]), op=ALU.mult),
                       reads=[psT[3], cT], writes=[t_at])
                    for h in range(4):
                        vs = slice(h * 256, (h + 1) * 256)
                        pso = ps[:, 4 + h // 2, (h % 2) * 256:(h % 2) * 256 + 256]
                        op("pe", lambda e, h=h, vs=vs, pso=pso: e.matmul(pso, lhsT=at4[:, h, :], rhs=vc[i][:, vs], start=True, stop=False),
                           reads=it + [t_at], writes=[psT[4 + h // 2]])
                        op("pe", lambda e, h=h, pso=pso: e.matmul(pso, lhsT=qd4[:, h, :], rhs=S_b[:, h, :], start=False, stop=True),
                           reads=[t_qd, t_Sb[0]], writes=[psT[4 + h // 2]])
                    for h in range(4):
                        hs = slice(h * 128, (h + 1) * 128)
                        vs = slice(h * 256, (h + 1) * 256)
                        pk = ps[:, 6 + h // 2, (h % 2) * 256:(h % 2) * 256 + 256]
                        op("pe", lambda e, hs=hs, vs=vs, pk=pk: e.matmul(pk, lhsT=kend[:, hs], rhs=vc[i][:, vs], start=True, stop=True),
                           reads=it + [t_kend], writes=[psT[6 + h // 2]])
                    for h in range(4):
                        pk = ps[:, 6 + h // 2, (h % 2) * 256:(h % 2) * 256 + 256]
                        op("dve", lambda e, h=h, pk=pk: e.scalar_tensor_tensor(out=S_f[:, h, :], in0=S_f[:, h, :], scalar=eq4[:, h, 127:128],
                                                                               in1=pk, op0=ALU.mult, op1=ALU.add),
                           reads=[t_S[0], t_eq, psT[6 + h // 2]], writes=[t_S[0]])
                    op("act", lambda e: e.activation(out=S_b[:].rearrange("p h v -> p (h v)"), in_=S_f[:].rearrange("p h v -> p (h v)"), func=AF.Copy),
                       reads=[t_S[0]], writes=[t_Sb[0]])
                    for h in range(4):
                        pso = ps[:, 4 + h // 2, (h % 2) * 256:(h % 2) * 256 + 256]
                        op("act", lambda e: e.activation(out=junkC[:], in_=pso, func=AF.Square, accum_out=ss4[:, h:h + 1]),
                           reads=[psT[4 + h // 2]], writes=[t_junk, t_ss4])
                    op("act", lambda e: e.activation(out=ss4[:, 4:8], in_=ss4[:, 0:4], func=AF.Sqrt, bias=eps_t[:], scale=1.0 / 256),
                       reads=[t_ss4, cT], writes=[t_ss4])
                    op("dve", lambda e: e.reciprocal(out=ss4[:, 8:12], in_=ss4[:, 4:8]), reads=[t_ss4], writes=[t_ss4])
                    for h in range(4):
                        vs = slice(h * 256, (h + 1) * 256)
                        pso = ps[:, 4 + h // 2, (h % 2) * 256:(h % 2) * 256 + 256]
                        op("dve", lambda e: e.scalar_tensor_tensor(out=tmpC[:, vs], in0=pso, scalar=ss4[:, 8 + h:9 + h],
                                                                   in1=gngb[:, vs], op0=ALU.mult, op1=ALU.mult),
                           reads=[psT[4 + h // 2], t_ss4, wC], writes=[t_tmp])
                    op("pool", lambda e: e.tensor_tensor(out=ga[:], in0=tmpC[:], in1=src[i][:], op=ALU.mult),
                       reads=it + [t_tmp], writes=[t_ga])
                    pbv = ps[:, 2, :].bitcast(BF16)
                    for kc in range(8):
                        op("pe", lambda e, kc=kc: e.transpose(out=pbv[:, kc * 128:(kc + 1) * 128],
                                                              in_=ga[:, kc * 128:(kc + 1) * 128], identity=ident_b[:]),
                           reads=[t_ga, cT], writes=[psT[2]])
                    op("act", lambda e: e.activation(out=gaT[:], in_=pbv.rearrange("p (k t) -> p k t", k=8), func=AF.Copy),
                       reads=[psT[2]], writes=[t_gaT])
                    for half in range(2):
                        yb_ = (3, 1)[half]
                        for kc in range(8):
                            op("pe", lambda e, kc=kc, yb_=yb_: e.matmul(ps[:, yb_, :], lhsT=gaT[:, kc, :],
                                                                        rhs=wba[:, kc, half * 512:(half + 1) * 512],
                                                                        start=(kc == 0), stop=(kc == 7)),
                               reads=[t_gaT, wC], writes=[psT[yb_]])
                        op("dve", lambda e, yb_=yb_: e.tensor_tensor(out=yag[i][:, half * 512:(half + 1) * 512], in0=ps[:, yb_, :],
                                                                     in1=szac[i][:, half * 512:(half + 1) * 512], op=ALU.mult),
                           reads=it + [psT[yb_]], writes=[yag_bt[i]])
                    op("sp", lambda e: e.dma_start(out=yag_s[c * 128:(c + 1) * 128, :], in_=yag[i][:]),
                       reads=[yag_bt[i]], writes=[yag_t], dma=st_y[i])

        if "E" in stages and "C" in stages:
            while cvs["n"] < NCV or cvs["pending"] is not None:
                conv_step()
        kb.barrier()
        stBC.close()
        x1_t = Trk(multi=True)
        if "D" in stages:
            with ExitStack() as st:
                adab, (G1,) = load_ada(st, "adaD", [G1])
                wuk = sb("wuk", [128, 8, 256], BF16, st=st)
                wuv = sb("wuv", [128, 8, 2, 128], BF16, st=st)
                wbb = sb("wbb", [128, 8, D], BF16, st=st)
                wout = sb("wout", [128, 8, D], BF16, st=st)
                kvgb = sb("kvgb", [128, 256], st=st)
                wD = Trk(multi=True)
                with ExitStack() as st2:
                    stg = [sb(f"stgD{i}", [128, 8, 512], F32, st=st2) for i in range(2)]
                    stg_t = [Trk() for _ in range(2)]
                    ld_stg = [kb.stream(f"ldstgD{i}") for i in range(2)]
                    op("sp", lambda e: e.dma_start(out=stg[0][:, :, 0:256], in_=wuk_d.rearrange("h d c -> d h c")),
                       writes=[stg_t[0]], dma=ld_stg[0])
                    op("pool", lambda e: e.tensor_copy(out=wuk[:], in_=stg[0][:, :, 0:256]), reads=[stg_t[0]], writes=[wD])
                    s1v = stg[1][:, 0:4, :].rearrange("p a (b d) -> p (a b) d", d=128)
                    op("sp", lambda e: e.dma_start(out=s1v, in_=wuv_d.rearrange("h (cc p) d -> p (h cc) d", p=128)),
                       writes=[stg_t[1]], dma=ld_stg[1])
                    op("pool", lambda e: e.tensor_copy(out=wuv[:].rearrange("p h c d -> p (h c) d"), in_=s1v),
                       reads=[stg_t[1]], writes=[wD])
                    op("sp", lambda e: e.dma_start(out=kvgb[:], in_=kvg_d[0:1, :].to_broadcast([128, 256])), writes=[wD], dma=ld_stg[0])
                    k = 0
                    for wdst, wsrc in ((wbb, wbb_d), (wout, wout_d)):
                        wv_ = wsrc.rearrange("(kc p) n -> p kc n", p=128)
                        for half in range(2):
                            i = k % 2
                            k += 1
                            op("sp", lambda e, i=i, half=half, wv_=wv_: e.dma_start(out=stg[i][:], in_=wv_[:, :, half * 512:(half + 1) * 512]),
                               writes=[stg_t[i]], dma=ld_stg[i])
                            op("pool", lambda e, i=i, half=half, wdst=wdst: e.tensor_copy(out=wdst[:, :, half * 512:(half + 1) * 512], in_=stg[i][:]),
                               reads=[stg_t[i]], writes=[wD])
                    kb.barrier()

                ckv = sb("ckv", [128, NT, 257], BF16, st=st)
                ckvT = sb("ckvT", [128, 2, S], BF16, st=st)
                ikT = sb("ikT", [64, S], BF16, st=st)
                iw_all = sb("iw_all", [128, NT, 8], st=st)
                posl = sb("posl", [128, 128], st=st)
                basej = sb("basej", [128, NT], st=st)
                slopes8 = sb("slopes8", [128, 8], st=st)
                esel = sb("esel", [8, 8, 128], BF16, st=st)
                posr = sb("posr", [10, 32, 128], BF16, st=st)
                slopeR = sb("slopeR", [10, 8, 128], BF16, st=st)
                ones8 = sb("ones8", [8, 128], BF16, st=st)
                ones_col = sb("ones_col", [128, 1], BF16, st=st)
                cD = Trk(multi=True)
                ld_cD = kb.stream("ldcD")
                st3 = ExitStack()
                esel_f = sb("esel_f", [8, 1024], st=st3)
                posr_f = sb("posr_f", [10, 32 * 128], st=st3)
                slopeR_f = sb("slopeR_f", [10, 1024], st=st3)
                op("sp", lambda e: e.dma_start(out=posl[:], in_=cst["c_pos1"][:, 0:128]), writes=[cD], dma=ld_cD)
                op("sp", lambda e: e.dma_start(out=basej[:], in_=cst["c_basej"][:, :]), writes=[cD], dma=ld_cD)
                op("sp", lambda e: e.dma_start(out=slopes8[:], in_=cst["c_slopes8"][:, :]), writes=[cD], dma=ld_cD)
                op("sp", lambda e: e.dma_start(out=esel_f[:], in_=cst["c_esel"][:, :]), writes=[cD], dma=ld_cD)
                op("sp", lambda e: e.dma_start(out=posr_f[:], in_=cst["c_posr"][:, :]), writes=[cD], dma=ld_cD)
                op("sp", lambda e: e.dma_start(out=slopeR_f[:], in_=cst["c_slopeR"][:, :]), writes=[cD], dma=ld_cD)
                op("dve", lambda e: e.tensor_copy(out=esel[:].rearrange("k h s -> k (h s)"), in_=esel_f[:]), reads=[cD], writes=[cD])
                op("dve", lambda e: e.tensor_copy(out=posr[:].rearrange("k a s -> k (a s)"), in_=posr_f[:]), reads=[cD], writes=[cD])
                op("dve", lambda e: e.tensor_copy(out=slopeR[:].rearrange("k h q -> k (h q)"), in_=slopeR_f[:]), reads=[cD], writes=[cD])
                op("dve", lambda e: e.memset(ones8[:], 1.0), writes=[cD])
                op("dve", lambda e: e.memset(ones_col[:], 1.0), writes=[cD])
                dkv_all = sb("dkv_all", [128, NT, 256], BF16, st=st3)
                t_dkv, t_ik, t_iw = Trk(), Trk(), Trk()
                ckv_t = [Trk() for _ in range(NT)]
                ckvT_t = [Trk() for _ in range(NT)]
                ld_d0 = kb.stream("ldd0")
                op("sp", lambda e: e.dma_start(out=dkv_all[:], in_=dkv_s.rearrange("(t p) c -> p t c", p=128)),
                   reads=[scr_t], writes=[t_dkv], dma=ld_d0)
                op("sp", lambda e: e.dma_start(out=ikT[:], in_=ikT_s[:, :]), reads=[scr_t], writes=[t_ik], dma=ld_d0)
                op("sp", lambda e: e.dma_start(out=iw_all[:], in_=iw_s.rearrange("(t p) n -> p t n", p=128)),
                   reads=[scr_t], writes=[t_iw], dma=ld_d0)
                ssd = sb("ssd", [128, 3 * NT], st=st3)
                t_ssd = Trk()
                junk0 = sb("junk0", [128, 256], BF16, st=st3)
                t_junk0 = Trk()
                for tt in range(NT):
                    op("act", lambda e: e.activation(out=junk0[:], in_=dkv_all[:, tt, :], func=AF.Square,
                                                     accum_out=ssd[:, tt:tt + 1]), reads=[t_dkv], writes=[t_junk0, t_ssd])
                op("act", lambda e: e.activation(out=ssd[:, NT:2 * NT], in_=ssd[:, 0:NT], func=AF.Sqrt, bias=eps_t[:], scale=1.0 / 256),
                   reads=[t_ssd, cT], writes=[t_ssd])
                op("dve", lambda e: e.reciprocal(out=ssd[:, 2 * NT:3 * NT], in_=ssd[:, NT:2 * NT]), reads=[t_ssd], writes=[t_ssd])
                op("dve", lambda e: e.memset(ckv[:, :, 256:257], 1.0), writes=ckv_t)
                for tt in range(NT):
                    op("dve", lambda e: e.scalar_tensor_tensor(out=ckv[:, tt, 0:256], in0=dkv_all[:, tt, :],
                                                               scalar=ssd[:, 2 * NT + tt:2 * NT + tt + 1], in1=kvgb[:],
                                                               op0=ALU.mult, op1=ALU.mult),
                       reads=[t_dkv, t_ssd, wD], writes=[ckv_t[tt]])
                    pb = 4 + (tt % 2)
                    pbv = ps[:, pb, :].bitcast(BF16)
                    for cc in range(2):
                        op("pe", lambda e, cc=cc: e.transpose(out=pbv[:, cc * 128:(cc + 1) * 128],
                                                              in_=ckv[:, tt, cc * 128:(cc + 1) * 128], identity=ident_b[:]),
                           reads=[ckv_t[tt], cT], writes=[psT[pb]])
                    op("act", lambda e: e.activation(out=ckvT[:, :, tt * 128:(tt + 1) * 128],
                                                     in_=pbv[:, 0:256].rearrange("p (c t) -> p c t", c=2), func=AF.Copy),
                       reads=[psT[pb]], writes=[ckvT_t[tt]])

                kb.barrier()
                st3.close()
                iqc = [sb(f"iqc{i}", [64, 8, 128], BF16, st=st) for i in range(2)]
                dqc = [sb(f"dqc{i}", [128, 8, 128], BF16, st=st) for i in range(2)]
                szbc = [sb(f"szbc{i}", [128, D], BF16, st=st) for i in range(2)]
                yagc = [sb(f"yagc{i}", [128, D], BF16, st=st) for i in range(2)]
                xq1 = sb("xq", [128, D], st=st)
                xq = [xq1, xq1]
                xq_t = Trk()
                ld_xq = kb.stream("ldxq")
                inI_t = [Trk(multi=True) for _ in range(2)]
                inA_t = [Trk(multi=True) for _ in range(2)]
                ld_I = [kb.stream(f"ldI{i}") for i in range(2)]
                ld_A = [kb.stream(f"ldA{i}") for i in range(2)]
                iqT_v = iqT_s.rearrange("(h d) t -> d h t", d=64)
                dqT_v = dqT_s.rearrange("(h d) t -> d h t", d=128)
                acc = sb("accD", [128, S], st=st)
                t_acc = Trk()
                relb = [sb(f"relb{i}", [128, 512], BF16, st=st) for i in range(2)]
                relb_t = [Trk() for _ in range(2)]
                Dg = sb("Dg", [128, 8, 128], BF16, st=st)
                t_Dg = Trk()
                bs = sb("bs", [128, 8], st=st)
                t_bs = Trk()
                sel = sb("sel", [128, S], BF16, st=st)
                t_sel = Trk()
                selT = [sb(f"selT{i}", [128, NT, 128], BF16, st=st) for i in range(2)]
                selT_t = [Trk() for _ in range(2)]
                qlat = sb("qlat", [128, 8, 2, 128], BF16, st=st)
                t_qlat = Trk()
                pT = [sb(f"pT{i}", [128, 4, 128], BF16, st=st) for i in range(3)]
                pT_t = [Trk() for _ in range(3)]
                oT = sb("oT", [128, 8, 128], BF16, st=st)
                t_oT = Trk()
                tmpD = sb("tmpD", [128, D], st=st)
                t_tmpD = Trk()
                ymix = sb("ymix", [128, D], BF16, st=st)
                t_ymix = Trk()
                ymixT = sb("ymixT", [128, 8, 128], BF16, st=st)
                t_ymixT = Trk()
                x1b1 = sb("x1b", [128, D], st=st)
                x1b = [x1b1, x1b1]
                x1b_t1 = Trk()
                x1b_t = [x1b_t1, x1b_t1]
                st_x11 = kb.stream("stx1")
                st_x1 = [st_x11, st_x11]
                nm_bias = sb("nm_bias", [128, 1], st=st)
                op("dve", lambda e: e.memset(nm_bias[:], -30000.0), writes=[cT])
                neg29 = sb("neg29", [128, 1], st=st)
                op("dve", lambda e: e.memset(neg29[:], -1.0e29), writes=[cT])
                corrD = [sb(f"corrD{i}", [10, 8, 128], BF16, st=st) for i in range(2)]
                corrD_t = [Trk() for _ in range(2)]
                for i_ in range(2):
                    op("dve", lambda e, i_=i_: e.tensor_copy(out=corrD[i_][:], in_=slopeR[:]), reads=[cD], writes=[corrD_t[i_]])
                rsrow = sb("rsrow", [1, 512], st=st)
                rsb = sb("rsb", [1, 512], BF16, st=st)
                t_rs = Trk()
                olT = sb("olT", [128, 2, 512], BF16, st=st)
                t_olT = Trk()
                rsB = sb("rsB", [128, 512], st=st)
                t_rsB = Trk()
                corr8 = sb("corr8", [128, 10], st=st)
                cm = sb("cm", [128, 2 * NT], st=st)
                t_corr8 = Trk()
                lg_t = [Trk() for _ in range(8)]
                NBIS = 13
                fvec = sb("fvec", [128, NBIS], st=st)
                wf = sb("wf", [128, NBIS], st=st)
                for k_ in range(NBIS):
                    op("dve", lambda e, k_=k_: e.memset(fvec[:, k_:k_ + 1], 2.0 ** -(k_ + 1)), writes=[cT])

                def load_I(qt):
                    i = qt % 2
                    qs = slice(qt * 128, (qt + 1) * 128)
                    op("sp", lambda e: e.dma_start(out=iqc[i][:], in_=iqT_v[:, :, qs]), reads=[scr_t], writes=[inI_t[i]], dma=ld_I[i])

                def load_A(qt):
                    i = qt % 2
                    qs = slice(qt * 128, (qt + 1) * 128)
                    op("sp", lambda e: e.dma_start(out=dqc[i][:], in_=dqT_v[:, :, qs]), reads=[scr_t], writes=[inA_t[i]], dma=ld_A[i])
                    op("sp", lambda e: e.dma_start(out=szbc[i][:], in_=szb_s[qs, :]), reads=[scr_t], writes=[inA_t[i]], dma=ld_A[i])
                    op("sp", lambda e: e.dma_start(out=yagc[i][:], in_=yag_s[qs, :]), reads=[yag_t], writes=[inA_t[i]], dma=ld_A[i])

                def idx_phase(qt):
                    i = qt % 2
                    Sk = (qt + 1) * 128
                    nkb = (Sk + 511) // 512
                    op("dve", lambda e: e.tensor_tensor(out=Dg[:], in0=ident_b[:].unsqueeze(1).to_broadcast([128, 8, 128]),
                                                        in1=iw_all[:, qt, :].unsqueeze(2).to_broadcast([128, 8, 128]), op=ALU.mult),
                       reads=[cT, t_iw], writes=[t_Dg])
                    nrel = 0
                    for kbi in range(nkb):
                        w = min(512, Sk - kbi * 512)
                        ks = slice(kbi * 512, kbi * 512 + w)
                        prev = None
                        for h in range(8):
                            ri = nrel % 2
                            rb = (0, 2)[nrel % 2]
                            nrel += 1
                            op("pe", lambda e: e.matmul(ps[:, rb, 0:w], lhsT=iqc[i][0:64, h, :], rhs=ikT[0:64, ks], start=True, stop=True),
                               reads=[inI_t[i], t_ik], writes=[psT[rb]])
                            if prev is not None:
                                ph, pri = prev
                                op("pe", lambda e: e.matmul(ps[:, 1, 0:w], lhsT=Dg[:, ph, :], rhs=relb[pri][:, 0:w], start=(ph == 0), stop=False),
                                   reads=[t_Dg, relb_t[pri]], writes=[psT[1]])
                                yield
                            op("act", lambda e: e.activation(out=relb[ri][:, 0:w], in_=ps[:, rb, 0:w], func=AF.Relu),
                               reads=[psT[rb]], writes=[relb_t[ri]])
                            prev = (h, ri)
                        ph, pri = prev
                        op("pe", lambda e: e.matmul(ps[:, 1, 0:w], lhsT=Dg[:, ph, :], rhs=relb[pri][:, 0:w], start=False, stop=True),
                           reads=[t_Dg, relb_t[pri]], writes=[psT[1]])
                        yield
                        if kbi == nkb - 1:
                            if w > 128:
                                op("dve", lambda e: e.tensor_copy(out=acc[:, kbi * 512:kbi * 512 + w - 128], in_=ps[:, 1, 0:w - 128]),
                                   reads=[psT[1]], writes=[t_acc])
                            op("dve", lambda e: e.tensor_tensor(out=acc[:, Sk - 128:Sk], in0=ps[:, 1, w - 128:w], in1=diag_f[:], op=ALU.add),
                               reads=[psT[1], cT], writes=[t_acc])
                        else:
                            op("dve", lambda e: e.tensor_copy(out=acc[:, ks], in_=ps[:, 1, 0:w]), reads=[psT[1]], writes=[t_acc])
                    if qt >= 2:
                        op("dve", lambda e: e.tensor_reduce(out=bs[:, 0:1], in_=acc[:, 0:Sk - 128], axis=mybir.AxisListType.X, op=ALU.min),
                           reads=[t_acc], writes=[t_bs])
                        op("dve", lambda e: e.tensor_reduce(out=bs[:, 5:6], in_=acc[:, 0:Sk], axis=mybir.AxisListType.X, op=ALU.max),
                           reads=[t_acc], writes=[t_bs])
                        op("dve", lambda e: e.tensor_tensor(out=bs[:, 1:2], in0=bs[:, 5:6], in1=bs[:, 0:1], op=ALU.subtract),
                           reads=[t_bs], writes=[t_bs])
                        op("dve", lambda e: e.tensor_scalar(out=wf[:], in0=fvec[:], scalar1=bs[:, 1:2], scalar2=None, op0=ALU.mult),
                           reads=[t_bs, cT], writes=[t_bs])
                        op("dve", lambda e: e.tensor_tensor(out=bs[:, 2:3], in0=bs[:, 0:1], in1=wf[:, 0:1], op=ALU.add),
                           reads=[t_bs], writes=[t_bs])
                        for it in range(NBIS):
                            op("dve", lambda e: e.tensor_scalar(out=sel[:, 0:Sk], in0=acc[:, 0:Sk], scalar1=bs[:, 2:3], scalar2=None,
                                                                op0=ALU.is_ge, op1=ALU.add, accum_out=bs[:, 3:4]),
                               reads=[t_acc, t_bs], writes=[t_sel, t_bs])
                            op("dve", lambda e, it=it: e.scalar_tensor_tensor(out=bs[:, 4:5], in0=bs[:, 3:4], scalar=255.5, in1=wf[:, it:it + 1],
                                                                              op0=ALU.is_ge, op1=ALU.mult), reads=[t_bs], writes=[t_bs])
                            if it < NBIS - 1:
                                op("dve", lambda e, it=it: e.scalar_tensor_tensor(out=bs[:, 2:3], in0=bs[:, 4:5], scalar=wf[:, it + 1:it + 2], in1=bs[:, 2:3],
                                                                                  op0=ALU.subtract, op1=ALU.add), reads=[t_bs], writes=[t_bs])
                            else:
                                op("dve", lambda e, it=it: e.scalar_tensor_tensor(out=bs[:, 0:1], in0=bs[:, 4:5], scalar=wf[:, it:it + 1], in1=bs[:, 2:3],
                                                                                  op0=ALU.subtract, op1=ALU.add), reads=[t_bs], writes=[t_bs])
                            yield "BIS"
                        thr = bs[:, 0:1]
                    else:
                        thr = neg29[:]
                    op("dve", lambda e: e.tensor_scalar(out=sel[:, 0:Sk], in0=acc[:, 0:Sk], scalar1=thr, scalar2=None, op0=ALU.is_ge),
                       reads=[t_acc, t_bs, cT], writes=[t_sel])
                    nch = qt + 1
                    op("dve", lambda e: e.tensor_tensor(out=acc[:, 0:Sk].rearrange("p (j s) -> p j s", s=128),
                                                        in0=sel[:, 0:Sk].rearrange("p (j s) -> p j s", s=128),
                                                        in1=posl[:].unsqueeze(1).to_broadcast([128, nch, 128]), op=ALU.mult),
                       reads=[t_sel, cD], writes=[t_acc])
                    op("dve", lambda e: e.tensor_reduce(out=cm[:, 0:nch], in_=acc[:, 0:Sk].rearrange("p (j s) -> p j s", s=128),
                                                        axis=mybir.AxisListType.X, op=ALU.max), reads=[t_acc], writes=[t_corr8])
                    op("dve", lambda e: e.tensor_scalar(out=cm[:, NT:NT + nch], in0=cm[:, 0:nch], scalar1=0.5, scalar2=None, op0=ALU.is_ge),
                       reads=[t_corr8], writes=[t_corr8])
                    op("dve", lambda e: e.tensor_tensor(out=cm[:, NT:NT + nch], in0=cm[:, NT:NT + nch], in1=basej[:, 0:nch], op=ALU.mult),
                       reads=[t_corr8, cD], writes=[t_corr8])
                    op("dve", lambda e: e.tensor_tensor(out=cm[:, 0:nch], in0=cm[:, 0:nch], in1=cm[:, NT:NT + nch], op=ALU.add),
                       reads=[t_corr8], writes=[t_corr8])
                    op("dve", lambda e: e.tensor_reduce(out=corr8[:, 8:9], in_=cm[:, 0:nch], axis=mybir.AxisListType.X, op=ALU.max),
                       reads=[t_corr8], writes=[t_corr8])
                    op("dve", lambda e: e.tensor_scalar(out=corr8[:, 9:10], in0=corr8[:, 8:9], scalar1=-1.0, scalar2=float(Sk),
                                                        op0=ALU.mult, op1=ALU.add), reads=[t_corr8], writes=[t_corr8])
                    op("dve", lambda e: e.tensor_scalar(out=corr8[:, 0:8], in0=slopes8[:], scalar1=corr8[:, 9:10], scalar2=None, op0=ALU.mult),
                       reads=[t_corr8, cD], writes=[t_corr8])
                    yield "HOLD"
                    op("pe", lambda e: e.transpose(out=ps[0:8, 2, 0:128], in_=corr8[:, 0:8], identity=ident_f[:]),
                       reads=[t_corr8, cT], writes=[psT[2]])
                    op("dve", lambda e: e.tensor_tensor(out=corrD[i][0:8, :, :], in0=esel[:], in1=ps[0:8, 2, 0:128].unsqueeze(1).to_broadcast([8, 8, 128]),
                                                        op=ALU.mult), reads=[psT[2], cD], writes=[corrD_t[i]])
                    yield
                    for j0 in range(0, qt + 1, 8):
                        nj = min(8, qt + 1 - j0)
                        pbv = ps[:, 2, :].bitcast(BF16)
                        for jj in range(nj):
                            j = j0 + jj
                            op("pe", lambda e: e.transpose(out=pbv[:, jj * 128:(jj + 1) * 128], in_=sel[:, j * 128:(j + 1) * 128], identity=ident_b[:]),
                               reads=[t_sel, cT], writes=[psT[2]])
                        op("act", lambda e: e.activation(out=selT[i][:, j0:j0 + nj, :],
                                                         in_=pbv[:, 0:nj * 128].rearrange("p (j t) -> p j t", t=128), func=AF.Identity,
                                                         scale=30000.0, bias=nm_bias[:]),
                           reads=[psT[2], cT], writes=[selT_t[i]])
                        yield

                pend = {"g": None}

                def pump(n=1, release=False):
                    g = pend["g"]
                    if g is None:
                        return
                    if pend.get("held") and not release:
                        return
                    pend["held"] = False
                    for _ in range(n):
                        try:
                            r = next(g)
                            if r == "HOLD" and not release:
                                pend["held"] = True
                                return
                            if r == "BIS" and not release:
                                pend["bis"] = pend.get("bis", 0) + 1
                                if pend["bis"] >= pend.get("bis_per_pump", 1):
                                    pend["bis"] = 0
                                    return
                        except StopIteration:
                            pend["g"] = None
                            return

                def att_phase(qt):
                    i = qt % 2
                    qs = slice(qt * 128, (qt + 1) * 128)
                    ia = [inA_t[i]]
                    for g in range(4):
                        for u in range(4):
                            hc = g * 4 + u
                            h, cc = hc // 2, hc % 2
                            op("pe", lambda e: e.matmul(ps[:, 2, u * 128:(u + 1) * 128], lhsT=wuk[:, h, cc * 128:(cc + 1) * 128],
                                                        rhs=dqc[i][:, h, :], start=True, stop=True),
                               reads=ia + [wD], writes=[psT[2]])
                        op("act", lambda e: e.activation(out=qlat[:, g * 2:g * 2 + 2, :, :].rearrange("p h c q -> p (h c q)"),
                                                         in_=ps[:, 2, :], func=AF.Copy, scale=128.0 ** -0.5),
                           reads=[psT[2]], writes=[t_qlat])
                        pump(6)
                    def emit_lg(g, j, k):
                        hs4 = slice(4 * g, 4 * g + 4)
                        lb = 3 + (k % 2)
                        lgb = ps[:, lb, :]
                        dl = j - qt + 31
                        for cc in range(2):
                            op("pe", lambda e, cc=cc: e.matmul(lgb, lhsT=ckvT[:, cc, j * 128:(j + 1) * 128], rhs=qlat[:, hs4, cc, :],
                                                               start=(cc == 0), stop=False),
                               reads=[ckvT_t[j], t_qlat], writes=[psT[lb]])
                        op("pe", lambda e: e.matmul(lgb, lhsT=posr[0:10, dl, :], rhs=corrD[i][0:10, hs4, :], start=False, stop=False),
                           reads=[cD, corrD_t[i]], writes=[psT[lb]])
                        op("pe", lambda e: e.matmul(lgb, lhsT=ident_b[:], rhs=selT[i][:, j, :].unsqueeze(1).to_broadcast([128, 4, 128]),
                                                    start=False, stop=True),
                           reads=[cT, selT_t[i]], writes=[psT[lb]])

                    def emit_exp_pv(g, j, k):
                        lb = 3 + (k % 2)
                        pi = k % 3
                        pTf = pT[pi][:].rearrange("p h q -> p (h q)")
                        op("act", lambda e: e.activation(out=pTf, in_=ps[:, lb, :], func=AF.Exp), reads=[psT[lb]], writes=[pT_t[pi]])
                        for cc in range(2):
                            op("pe", lambda e, cc=cc: e.matmul(ps[:, 5 + cc, :], lhsT=ckv[:, j, cc * 128:(cc + 1) * 128], rhs=pTf,
                                                               start=(j == 0), stop=(j == qt)),
                               reads=[pT_t[pi], ckv_t[j]], writes=[psT[5 + cc]])
                        op("pe", lambda e: e.matmul(ps[0:1, 7, :], lhsT=ones_col[:, 0:1], rhs=pTf, start=(j == 0), stop=(j == qt)),
                           reads=[pT_t[pi], cD], writes=[psT[7]])

                    kstep = 0
                    for g in range(2):
                        hs4 = slice(4 * g, 4 * g + 4)
                        emit_lg(g, 0, kstep)
                        for j in range(qt + 1):
                            if j + 1 <= qt:
                                emit_lg(g, j + 1, kstep + 1)
                            lbk = 3 + (kstep % 2)
                            emit_exp_pv(g, j, kstep)
                            kstep += 1
                            pump(6)
                        op("act", lambda e: e.activation(out=rsrow[0:1, :], in_=ps[0:1, 7, :], func=AF.Ln), reads=[psT[7]], writes=[t_rs])
                        op("act", lambda e: e.activation(out=rsb[0:1, :], in_=rsrow[0:1, :], func=AF.Exp, scale=-1.0), reads=[t_rs], writes=[t_rs])
                        op("act", lambda e: e.activation(out=olT[:, 0, :], in_=ps[:, 5, :], func=AF.Copy), reads=[psT[5]], writes=[t_olT])
                        op("act", lambda e: e.activation(out=olT[:, 1, :], in_=ps[:, 6, :], func=AF.Copy), reads=[psT[6]], writes=[t_olT])
                        op("pe", lambda e: e.matmul(ps[:, 2, :], lhsT=ones_row[0:1, :], rhs=rsb[0:1, :], start=True, stop=True),
                           reads=[cT, t_rs], writes=[psT[2]])
                        op("act", lambda e: e.activation(out=rsB[:], in_=ps[:, 2, :], func=AF.Copy), reads=[psT[2]], writes=[t_rsB])
                        for u in range(4):
                            h = 4 * g + u
                            for cc in range(2):
                                op("pe", lambda e, cc=cc: e.matmul(ps[:, 2, u * 128:(u + 1) * 128], lhsT=wuv[:, h, cc, :],
                                                                   rhs=olT[:, cc, u * 128:(u + 1) * 128], start=(cc == 0), stop=(cc == 1)),
                                   reads=[wD, t_olT], writes=[psT[2]])
                        op("dve", lambda e: e.tensor_tensor(out=oT[:, hs4, :].rearrange("p h q -> p (h q)"), in0=ps[:, 2, :], in1=rsB[:], op=ALU.mult),
                           reads=[psT[2], t_rsB], writes=[t_oT])
                        pump(6)
                    for half in range(2):
                        hsl = slice(half * 512, (half + 1) * 512)
                        for h in range(8):
                            op("pe", lambda e, h=h: e.matmul(ps[:, 2, :], lhsT=oT[:, h, :], rhs=wbb[:, h, hsl], start=(h == 0), stop=(h == 7)),
                               reads=[t_oT, wD], writes=[psT[2]])
                        op("dve", lambda e: e.tensor_tensor(out=tmpD[:, hsl], in0=ps[:, 2, :], in1=szbc[i][:, hsl], op=ALU.mult),
                           reads=ia + [psT[2]], writes=[t_tmpD])
                        pump(6)
                    op("pool", lambda e: e.tensor_tensor(out=ymix[:], in0=tmpD[:], in1=yagc[i][:], op=ALU.add),
                       reads=ia + [t_tmpD], writes=[t_ymix])
                    pbv = ps[:, 2, :].bitcast(BF16)
                    for kc in range(8):
                        op("pe", lambda e, kc=kc: e.transpose(out=pbv[:, kc * 128:(kc + 1) * 128], in_=ymix[:, kc * 128:(kc + 1) * 128],
                                                              identity=ident_b[:]), reads=[t_ymix, cT], writes=[psT[2]])
                    op("act", lambda e: e.activation(out=ymixT[:], in_=pbv.rearrange("p (k t) -> p k t", k=8), func=AF.Copy),
                       reads=[psT[2]], writes=[t_ymixT])
                    pump(6)
                    for half in range(2):
                        hsl = slice(half * 512, (half + 1) * 512)
                        for kc in range(8):
                            op("pe", lambda e, kc=kc: e.matmul(ps[:, 2, :], lhsT=ymixT[:, kc, :], rhs=wout[:, kc, hsl],
                                                               start=(kc == 0), stop=(kc == 7)),
                               reads=[t_ymixT, wD], writes=[psT[2]])
                        op("dve", lambda e: e.tensor_tensor(out=tmpD[:, hsl], in0=ps[:, 2, :], in1=adab[:, G1][:, hsl], op=ALU.mult),
                           reads=[psT[2], adaT], writes=[t_tmpD])
                        pump(6)
                    op("sp", lambda e: e.dma_start(out=xq1[:], in_=x_d[qs, :]), writes=[xq_t], dma=ld_xq)
                    op("pool", lambda e: e.tensor_tensor(out=x1b[i][:], in0=tmpD[:], in1=xq1[:], op=ALU.add),
                       reads=[xq_t, t_tmpD], writes=[x1b_t[i]])
                    op("sp", lambda e: e.dma_start(out=x1_s[qs, :], in_=x1b[i][:]), reads=[x1b_t[i]], writes=[x1_t], dma=st_x1[i])

                def run_idx(qt):
                    for _ in idx_phase(qt):
                        pass

                dlim = os.environ.get("DLIM", "")
                if dlim == "setup":
                    pass
                elif dlim.startswith("idx"):
                    nq = int(dlim[3:])
                    for qt in range(nq):
                        load_I(qt)
                        run_idx(qt)
                    if "dbg_acc" in debug:
                        dacc = nc.dram_tensor("dbg_acc", [128, S], F32, kind="ExternalOutput").ap()
                        dsel = nc.dram_tensor("dbg_sel", [128, S], BF16, kind="ExternalOutput").ap()
                        dbs = nc.dram_tensor("dbg_bs", [128, 8], F32, kind="ExternalOutput").ap()
                        op("sp", lambda e: e.dma_start(out=dacc[:, :], in_=acc[:]), reads=[t_acc], writes=[x1_t], dma=st_x1[0])
                        op("sp", lambda e: e.dma_start(out=dsel[:, :], in_=sel[:]), reads=[t_sel], writes=[x1_t], dma=st_x1[0])
                        op("sp", lambda e: e.dma_start(out=dbs[:, :], in_=bs[:]), reads=[t_bs], writes=[x1_t], dma=st_x1[0])
                elif dlim.startswith("qts"):
                    for qt in [int(v) for v in dlim[3:].split("_")]:
                        load_I(qt)
                        load_A(qt)
                        run_idx(qt)
                        att_phase(qt)
                elif dlim.startswith("att"):
                    nq = int(dlim[3:])
                    for qt in range(nq):
                        load_I(qt)
                        load_A(qt)
                        run_idx(qt)
                        att_phase(qt)
                else:
                    load_I(0)
                    load_A(0)
                    run_idx(0)
                    for qt in range(NT):
                        if qt + 1 < NT:
                            load_I(qt + 1)
                            load_A(qt + 1)
                            pend["g"] = idx_phase(qt + 1)
                            pend["bis_per_pump"] = 3 if qt < 6 else (2 if qt < 12 else 1)
                            pend["bis"] = 0
                        att_phase(qt)
                        pump(100000, release=True)

        kb.barrier()
        out_t = Trk(multi=True)
        if "E" in stages:
            with ExitStack() as st:
                adae, (SH2, A2, G2) = load_ada(st, "adaE", [SH2, A2, G2])
                wq = sb("wq", [128, 8, 2048], BF16, st=st)
                KT = sb("KT", [128, 2, 128], BF16, st=st)
                fgb = sb("fgb", [128, D], st=st)
                wE = Trk(multi=True)
                with ExitStack() as st2:
                    stg = [sb(f"stgE{i}", [128, 8, 512], F32, st=st2) for i in range(2)]
                    stg_t = [Trk() for _ in range(2)]
                    ld_stg = [kb.stream(f"ldstgE{i}") for i in range(2)]
                    wq_v = wq_d.rearrange("(kc p) n -> p kc n", p=128)
                    for q4 in range(4):
                        i = q4 % 2
                        op("sp", lambda e, i=i, q4=q4: e.dma_start(out=stg[i][:], in_=wq_v[:, :, q4 * 512:(q4 + 1) * 512]),
                           writes=[stg_t[i]], dma=ld_stg[i])
                        op("pool", lambda e, i=i, q4=q4: e.tensor_copy(out=wq[:, :, q4 * 512:(q4 + 1) * 512], in_=stg[i][:]),
                           reads=[stg_t[i]], writes=[wE])
                    for half, skd in enumerate((sk1_d, sk2_d)):
                        op("sp", lambda e, half=half, skd=skd: e.dma_start(out=stg[half][:, 0, 0:128], in_=skd[:, :]),
                           writes=[stg_t[half]], dma=ld_stg[half])
                        op("pe", lambda e, half=half: e.transpose(out=ps[:, half, 0:128], in_=stg[half][:, 0, 0:128], identity=ident_f[:]),
                           reads=[stg_t[half], cT], writes=[psT[half]])
                        op("act", lambda e, half=half: e.activation(out=KT[:, half, :], in_=ps[:, half, 0:128], func=AF.Copy),
                           reads=[psT[half]], writes=[wE])
                    op("sp", lambda e: e.dma_start(out=fgb[:], in_=fg_d[0:1, :].to_broadcast([128, D])), writes=[wE], dma=ld_stg[0])
                    kb.barrier()

                x1t = [sb(f"x1t{i}", [128, D], st=st) for i in range(3)]
                x1t_t = [Trk() for _ in range(3)]
                ld_x1 = [kb.stream(f"ldx1{i}") for i in range(3)]
                ssE = sb("ssE", [128, 8], st=st)
                t_ssE = Trk()
                junkE = sb("junkE", [128, D], BF16, st=st)
                t_junkE = Trk()
                tmp4 = sb("tmp4", [128, D], st=st)
                t_tmp4 = Trk()
                h2b = [sb(f"h2b{i}", [128, D], BF16, st=st) for i in range(2)]
                h2b_t = [Trk() for _ in range(2)]
                h2T = sb("h2T", [128, 8, 128], BF16, st=st)
                t_h2T = Trk()
                qTs = sb("qTs", [128, 16, 128], BF16, st=st)
                t_qTs = Trk()
                Ssb = sb("Ssb", [128, 16, 128], st=st)
                t_Ssb = Trk()
                scr8 = sb("scr8", [128, 2048], st=st)
                t_scr8 = Trk()
                m8 = sb("m8", [128, 16, 16], st=st)
                i8 = sb("i8", [128, 16, 16], U32, st=st)
                i8f = sb("i8f", [128, 16, 16], st=st)
                t_m8, t_i8, t_i8f = Trk(), Trk(), Trk()
                cand = Ssb[:].rearrange("p (h a) t -> p h (a t)", a=2)
                t_cand = t_Ssb
                b8 = sb("b8", [128, 8, 16], st=st)
                c8 = sb("c8", [128, 8, 16], U32, st=st)
                t_b8, t_c8 = Trk(), Trk()
                chi = sb("chi", [128, 128], U32, st=st)
                clo = sb("clo", [128, 128], U32, st=st)
                chif = sb("chif", [128, 8, 16], st=st)
                clof = sb("clof", [128, 8, 16], st=st)
                iab = sb("iab", [128, 2, 128], st=st)
                eidxf = sb("eidxf", [128, 128], st=st)
                t_sm = Trk()
                eidx = [sb(f"eidx{i}", [128, 128], U32, st=st) for i in range(2)]
                eidx_t = [Trk() for _ in range(2)]
                gz = sb("gz", [128, 8, 16], st=st)
                gsum = sb("gsum", [128, 16], st=st)
                t_gz = Trk()
                gates = [sb(f"gates{i}", [128, 128], st=st) for i in range(2)]
                gates_t = [Trk() for _ in range(2)]
                hu = sb("hu", [128, 128], st=st)
                ag = sb("ag", [128, 128], st=st)
                aa = sb("aa", [128, 128], st=st)
                NB = 4
                hu_t = [Trk() for _ in range(128)]
                ag_t = [Trk() for _ in range(128 // NB)]
                aa_t = [Trk() for _ in range(128 // NB)]
                NR = 20
                G = [sb(f"G{i}", [128, 2 * D], BF16, st=st) for i in range(NR)]
                G_t = [Trk() for _ in range(NR)]
                ld_G = [kb.stream(f"ldG{i}") for i in range(NR)]
                dgv = [sb(f"dgv{i}", [128, 128], BF16, st=st) for i in range(4)]
                vs_t = [Trk() for _ in range(4)]
                x2 = sb("x2", [128, D], st=st)
                t_x2 = Trk()
                outt = [sb(f"outt{i}", [128, D], st=st) for i in range(2)]
                outt_t = [Trk() for _ in range(2)]
                st_o = [kb.stream(f"sto{i}") for i in range(2)]

                def load_x1(tt):
                    i = tt % 3
                    op("sp", lambda e: e.dma_start(out=x1t[i][:], in_=x1_s[tt * 128:(tt + 1) * 128, :]),
                       reads=[x1_t], writes=[x1t_t[i]], dma=ld_x1[i])

                def top16(src_fn, n, mout, iout, tm, ti, groups):
                    gm = [Trk() for _ in range(groups)]
                    s2s = (scr8[:, 0:n], scr8[:, 1024:1024 + n])
                    s2t = (t_scr8, Trk())
                    for g0 in range(0, groups, 2):
                        pair = [g for g in (g0, g0 + 1) if g < groups]
                        for g in pair:
                            op("dve", lambda e, g=g: e.max(out=mout[:, g, 0:8], in_=src_fn(g)), reads=[t_Ssb, t_cand], writes=[gm[g]])
                        for k, g in enumerate(pair):
                            op("dve", lambda e, g=g, k=k: e.match_replace(out=s2s[k], in_to_replace=mout[:, g, 0:8], in_values=src_fn(g), imm_value=NEG),
                               reads=[t_Ssb, t_cand, gm[g]], writes=[s2t[k]])
                        for k, g in enumerate(pair):
                            op("dve", lambda e, g=g, k=k: e.max(out=mout[:, g, 8:16], in_=s2s[k]), reads=[s2t[k], gm[g]], writes=[gm[g]])
                        for g in pair:
                            op("dve", lambda e, g=g: e.max_index(out=iout[:, g, 0:8], in_max=mout[:, g, 0:8], in_values=src_fn(g)),
                               reads=[t_Ssb, t_cand, gm[g]], writes=[ti])
                        for g in pair:
                            op("dve", lambda e, g=g: e.max_index(out=iout[:, g, 8:16], in_max=mout[:, g, 8:16], in_values=src_fn(g)),
                               reads=[t_Ssb, t_cand, gm[g]], writes=[ti])
                        yield
                    op("dve", lambda e: e.tensor_copy(out=mout[:, 0, 0:1], in_=mout[:, 0, 0:1]), reads=gm, writes=[tm])

                def front(tt):
                    i = tt % 2
                    x3 = tt % 3
                    op("act", lambda e: e.activation(out=junkE[:], in_=x1t[x3][:], func=AF.Square, accum_out=ssE[:, 0:1]),
                       reads=[x1t_t[x3]], writes=[t_junkE, t_ssE])
                    op("act", lambda e: e.activation(out=ssE[:, 1:2], in_=ssE[:, 0:1], func=AF.Sqrt, bias=eps_t[:], scale=1.0 / D),
                       reads=[t_ssE, cT], writes=[t_ssE])
                    op("dve", lambda e: e.reciprocal(out=ssE[:, 2:3], in_=ssE[:, 1:2]), reads=[t_ssE], writes=[t_ssE])
                    op("dve", lambda e: e.scalar_tensor_tensor(out=tmp4[:], in0=x1t[x3][:], scalar=ssE[:, 2:3], in1=adae[:, A2],
                                                               op0=ALU.mult, op1=ALU.mult), reads=[x1t_t[x3], t_ssE, adaT], writes=[t_tmp4])
                    yield
                    op("dve", lambda e: e.tensor_tensor(out=h2b[i][:], in0=tmp4[:], in1=adae[:, SH2], op=ALU.add),
                       reads=[t_tmp4, adaT], writes=[h2b_t[i]])
                    pbv = ps[:, 7, :].bitcast(BF16)
                    for kc in range(8):
                        op("pe", lambda e, kc=kc: e.transpose(out=pbv[:, kc * 128:(kc + 1) * 128], in_=h2b[i][:, kc * 128:(kc + 1) * 128],
                                                              identity=ident_b[:]), reads=[h2b_t[i], cT], writes=[psT[7]])
                    op("act", lambda e: e.activation(out=h2T[:], in_=pbv.rearrange("p (k t) -> p k t", k=8), func=AF.Copy),
                       reads=[psT[7]], writes=[t_h2T])
                    yield
                    for g in range(4):
                        pb = g % 2
                        for u in range(4):
                            hh = g * 4 + u
                            for kc in range(8):
                                op("pe", lambda e, kc=kc: e.matmul(ps[:, pb, u * 128:(u + 1) * 128], lhsT=wq[:, kc, hh * 128:(hh + 1) * 128],
                                                                   rhs=h2T[:, kc, :], start=(kc == 0), stop=(kc == 7)),
                                   reads=[wE, t_h2T], writes=[psT[pb]])
                        op("act", lambda e: e.activation(out=qTs[:, g * 4:(g + 1) * 4, :].rearrange("p a t -> p (a t)"),
                                                         in_=ps[:, pb, :], func=AF.Copy), reads=[psT[pb]], writes=[t_qTs])
                        yield
                    for g in range(4):
                        pb = 2 + g % 2
                        for u in range(4):
                            hh = g * 4 + u
                            op("pe", lambda e: e.matmul(ps[:, pb, u * 128:(u + 1) * 128], lhsT=qTs[:, hh, :], rhs=KT[:, hh % 2, :],
                                                        start=True, stop=True), reads=[t_qTs, wE], writes=[psT[pb]])
                        op("act", lambda e: e.activation(out=Ssb[:, g * 4:(g + 1) * 4, :].rearrange("p a t -> p (a t)"),
                                                         in_=ps[:, pb, :], func=AF.Copy), reads=[psT[pb]], writes=[t_Ssb])
                        yield
                    yield from top16(lambda g: Ssb[:, g, :], 128, m8, i8, t_m8, t_i8, 16)
                    v4 = m8[:].rearrange("p (h t) k -> p h t k", t=2)
                    op("dve", lambda e: e.tensor_tensor(out=cand.rearrange("p h (a b) -> p h a b", b=16),
                                                        in0=v4[:, :, 0, :].unsqueeze(3).to_broadcast([128, 8, 16, 16]),
                                                        in1=v4[:, :, 1, :].unsqueeze(2).to_broadcast([128, 8, 16, 16]), op=ALU.add),
                       reads=[t_m8], writes=[t_cand])
                    yield
                    yield from top16(lambda g: cand[:, g, :], 256, b8, c8, t_b8, t_c8, 8)
                    c8f = c8[:].rearrange("p h k -> p (h k)")
                    op("dve", lambda e: e.tensor_scalar(out=chi[:], in0=c8f, scalar1=4, scalar2=None, op0=ALU.logical_shift_right),
                       reads=[t_c8], writes=[t_sm])
                    op("dve", lambda e: e.tensor_scalar(out=clo[:], in0=c8f, scalar1=15, scalar2=None, op0=ALU.bitwise_and),
                       reads=[t_c8], writes=[t_sm])
                    op("dve", lambda e: e.tensor_copy(out=chif[:].rearrange("p h k -> p (h k)"), in_=chi[:]), reads=[t_sm], writes=[t_sm])
                    op("dve", lambda e: e.tensor_copy(out=clof[:].rearrange("p h k -> p (h k)"), in_=clo[:]), reads=[t_sm], writes=[t_sm])
                    op("dve", lambda e: e.tensor_copy(out=i8f[:], in_=i8[:]), reads=[t_i8], writes=[t_i8f])
                    yield
                    oh = scr8[:].rearrange("p (h k i) -> p h k i", h=8, k=16)
                    io4 = iota16[:].rearrange("p (k i) -> p k i", i=16).unsqueeze(1).to_broadcast([128, 8, 16, 16])
                    i4 = i8f[:].rearrange("p (h t) k -> p h t k", t=2)
                    for half, cf in enumerate((chif, clof)):
                        op("dve", lambda e: e.tensor_tensor(out=oh, in0=cf[:].unsqueeze(3).to_broadcast([128, 8, 16, 16]), in1=io4, op=ALU.is_equal),
                           reads=[t_sm, cT], writes=[t_scr8])
                        op("dve", lambda e: e.tensor_tensor(out=oh, in0=oh, in1=i4[:, :, half, :].unsqueeze(2).to_broadcast([128, 8, 16, 16]), op=ALU.mult),
                           reads=[t_scr8, t_i8f], writes=[t_scr8])
                        op("dve", lambda e: e.tensor_reduce(out=iab[:, half, :].rearrange("p (h k) -> p h k", k=16), in_=oh,
                                                            axis=mybir.AxisListType.X, op=ALU.add), reads=[t_scr8], writes=[t_sm])
                        yield
                    op("dve", lambda e: e.scalar_tensor_tensor(out=eidxf[:], in0=iab[:, 0, :], scalar=128.0, in1=iab[:, 1, :],
                                                               op0=ALU.mult, op1=ALU.add), reads=[t_sm], writes=[t_sm])
                    op("dve", lambda e: e.tensor_copy(out=eidx[i][:], in_=eidxf[:]), reads=[t_sm], writes=[eidx_t[i]])
                    op("dve", lambda e: e.tensor_tensor(out=gz[:], in0=b8[:], in1=b8[:, :, 0:1].to_broadcast([128, 8, 16]), op=ALU.subtract),
                       reads=[t_b8], writes=[t_gz])
                    op("act", lambda e: e.activation(out=gz[:], in_=gz[:], func=AF.Exp), reads=[t_gz], writes=[t_gz])
                    op("dve", lambda e: e.tensor_reduce(out=gsum[:, 0:8], in_=gz[:], axis=mybir.AxisListType.X, op=ALU.add),
                       reads=[t_gz], writes=[t_gz])
                    op("dve", lambda e: e.reciprocal(out=gsum[:, 8:16], in_=gsum[:, 0:8]), reads=[t_gz], writes=[t_gz])
                    op("dve", lambda e: e.tensor_tensor(out=gates[i][:].rearrange("p (h k) -> p h k", k=16), in0=gz[:],
                                                        in1=gsum[:, 8:16].unsqueeze(2).to_broadcast([128, 8, 16]), op=ALU.mult),
                       reads=[t_gz], writes=[gates_t[i]])
                    yield

                pendE = {"g": None}

                def pumpE(n=1):
                    g = pendE["g"]
                    if g is None:
                        return
                    for _ in range(n):
                        try:
                            next(g)
                        except StopIteration:
                            pendE["g"] = None
                            return

                cnt = {"g": 0, "vs": 0, "jk": 0}
                ring = {}
                jk = [sb(f"jk{i}", [128, D], BF16, st=st) for i in range(2)]
                jk_t = [Trk() for _ in range(2)]

                def gather(tt, hk):
                    i = tt % 2
                    r = cnt["g"] % NR
                    cnt["g"] += 1
                    ring[(tt, hk)] = r
                    op("pool", lambda e: e.indirect_dma_start(out=G[r][:], out_offset=None, in_=uv_s[:, :],
                                                              in_offset=bass.IndirectOffsetOnAxis(ap=eidx[i][:, hk:hk + 1], axis=0)),
                       reads=[eidx_t[i], uv_t], writes=[G_t[r]], dma=ld_G[r])

                def dot(tt, hk):
                    i = tt % 2
                    r = ring[(tt, hk)]
                    bt = hk // NB
                    q = cnt["jk"] % 2
                    cnt["jk"] += 1
                    op("dve", lambda e: e.scalar_tensor_tensor(out=jk[q][:], in0=G[r][:, 0:D], scalar=1.0, in1=h2b[i][:],
                                                               op0=ALU.mult, op1=ALU.mult, accum_out=hu[:, hk:hk + 1]),
                       reads=[G_t[r], h2b_t[i]], writes=[jk_t[q], hu_t[hk]])

                def gate_batch(tt, bt):
                    i = tt % 2
                    cs = slice(bt * NB, (bt + 1) * NB)
                    op("act", lambda e: e.activation(out=ag[:, cs], in_=hu[:, cs], func=AF.Gelu), reads=hu_t[bt * NB:(bt + 1) * NB], writes=[ag_t[bt]])
                    op("dve", lambda e: e.tensor_tensor(out=aa[:, cs], in0=ag[:, cs], in1=gates[i][:, cs], op=ALU.mult),
                       reads=[ag_t[bt], gates_t[i]], writes=[aa_t[bt]])

                def vacc(tt, hk):
                    r = ring.pop((tt, hk))
                    bt = hk // NB
                    r3 = cnt["vs"] % 4
                    cnt["vs"] += 1
                    op("act", lambda e: e.activation(out=dgv[r3][:], in_=ident_b[:], func=AF.Copy, scale=aa[:, hk:hk + 1]),
                       reads=[cT, aa_t[bt]], writes=[vs_t[r3]])
                    for half in range(2):
                        op("pe", lambda e, half=half: e.matmul(ps[:, 4 + half, :], lhsT=dgv[r3][:], rhs=G[r][:, D + half * 512:D + (half + 1) * 512],
                                                               start=(hk == 0), stop=(hk == 127)),
                           reads=[G_t[r], vs_t[r3]], writes=[psT[4 + half]])

                def final(tt):
                    i = tt % 2
                    x3 = tt % 3
                    for half in range(2):
                        hsl = slice(half * 512, (half + 1) * 512)
                        op("dve", lambda e: e.tensor_tensor(out=tmp4[:, hsl], in0=ps[:, 4 + half, :], in1=adae[:, G2][:, hsl], op=ALU.mult),
                           reads=[psT[4 + half], adaT], writes=[t_tmp4])
                    op("pool", lambda e: e.tensor_tensor(out=x2[:], in0=tmp4[:], in1=x1t[x3][:], op=ALU.add),
                       reads=[t_tmp4, x1t_t[x3]], writes=[t_x2])
                    op("act", lambda e: e.activation(out=junkE[:], in_=x2[:], func=AF.Square, accum_out=ssE[:, 4:5]),
                       reads=[t_x2], writes=[t_junkE, t_ssE])
                    op("act", lambda e: e.activation(out=ssE[:, 5:6], in_=ssE[:, 4:5], func=AF.Sqrt, bias=eps_t[:], scale=1.0 / D),
                       reads=[t_ssE, cT], writes=[t_ssE])
                    op("dve", lambda e: e.reciprocal(out=ssE[:, 6:7], in_=ssE[:, 5:6]), reads=[t_ssE], writes=[t_ssE])
                    op("dve", lambda e: e.scalar_tensor_tensor(out=outt[i][:], in0=x2[:], scalar=ssE[:, 6:7], in1=fgb[:],
                                                               op0=ALU.mult, op1=ALU.mult), reads=[t_x2, t_ssE, wE], writes=[outt_t[i]])
                    op("sp", lambda e: e.dma_start(out=out_d[tt * 128:(tt + 1) * 128, :], in_=outt[i][:]),
                       reads=[outt_t[i]], writes=[out_t], dma=st_o[i])

                ntile = int(os.environ.get("ELIM", NT))
                DLY = 1
                LOOK = NR - NB - DLY - 1
                load_x1(0)
                if ntile > 1:
                    load_x1(1)
                for _ in front(0):
                    pass
                for tt in range(ntile):
                    if tt + 2 < ntile:
                        load_x1(tt + 2)
                    if tt + 1 < ntile:
                        pendE["g"] = front(tt + 1)
                    for hk in range(min(LOOK, 128)):
                        gather(tt, hk)
                    for hk in range(128):
                        if hk + LOOK < 128:
                            gather(tt, hk + LOOK)
                        dot(tt, hk)
                        if hk >= DLY and (hk - DLY) % NB == NB - 1:
                            bt = (hk - DLY) // NB
                            gate_batch(tt, bt)
                            for h2 in range(bt * NB, (bt + 1) * NB):
                                vacc(tt, h2)
                        if hk % 3 == 2:
                            pumpE(1)
                    for bt in range((128 - DLY) // NB, 128 // NB):
                        gate_batch(tt, bt)
                        for h2 in range(bt * NB, (bt + 1) * NB):
                            vacc(tt, h2)
                    pumpE(100000)
                    final(tt)
                kb.barrier()

        fin = []
        if "B" in stages:
            fin.append(scr_t)
        fin.append(yag_t)
        fin.append(x1_t)
        fin.append(out_t)
        kb.wait_all("sp", fin)
        print("instructions:", kb.ninst)
    return nc


def _in_maps(inputs, ncores=8):
    consts = make_consts()
    maps = []
    for b in range(ncores):
        m = dict(consts)
        m["x"] = np.ascontiguousarray(inputs["x"][b])
        m["c"] = np.ascontiguousarray(inputs["c"][b:b + 1])
        for k in ("w_ada", "b_ada", "norm1_g", "w_in", "gla_w_gate_up", "gla_b_gate", "gla_norm_g",
                  "dsa_kv_norm_g", "dsa_w_uk", "dsa_w_uv", "w_branch_a", "w_branch_b", "w_out", "norm2_g",
                  "peer_w_q", "peer_sub_keys_1", "peer_sub_keys_2", "peer_u", "peer_v"):
            m[k] = np.ascontiguousarray(inputs[k][0]) if inputs[k].ndim > 1 and inputs[k].shape[0] == 1 else inputs[k]
        for k in ("b_ada", "norm1_g", "gla_b_gate", "gla_norm_g", "dsa_kv_norm_g", "norm2_g"):
            m[k] = np.ascontiguousarray(inputs[k]).reshape(1, -1)
        m["final_norm_g"] = np.ascontiguousarray(inputs["final_norm_g"]).reshape(1, -1)
        maps.append(m)
    return maps


def kernel(**inputs):
    inputs = {k: np.asarray(v) for k, v in inputs.items()}
    nc = build()
    maps = _in_maps(inputs)
    res = run_bass_kernel_spmd(nc, maps, core_ids=list(range(8)))
    return np.stack([r["out"] for r in res.results], axis=0).astype(np.float32)
```
